# Optimizing a Trainium2 kernel written in Bass

```python
import math
import jax, jax.numpy as jnp
from jax import lax
import numpy as np

D_MODEL = 1024
BATCH = 16
SEQ = 4096
DEPTH = 1

ATT_HEADS = 16
ATT_KV_HEADS = 2
ATT_HEAD_DIM = 64
WINDOW = 128
ATT_BLOCK = 128
ROPE_THETA = 10000.0
ML_HEADS = 4
ML_DQK = 128
ML_DV = 256
ML_CHUNK = 64
CONV_WIDTH = 4
MEM_LEN = 256
X_HEADS = 4
X_HEAD_DIM = D_MODEL // X_HEADS
N_GROUPS = 8
EXPERTS_PER_GROUP = 8
N_EXPERTS = N_GROUPS * EXPERTS_PER_GROUP
TOP_K = 2
D_EXPERT = 256
MOE_BLOCK = 128
ALPHA = (2 * DEPTH) ** 0.25
BETA = (8 * DEPTH) ** -0.25
LN_EPS = 1e-5
RMS_EPS = 1e-6

ATT_Q_W = ATT_HEADS * ATT_HEAD_DIM
ATT_KV_W = ATT_KV_HEADS * ATT_HEAD_DIM
ML_QK_W = ML_HEADS * ML_DQK
ML_V_W = ML_HEADS * ML_DV
IN_WIDTHS = (ATT_Q_W, ATT_KV_W, ATT_KV_W, ML_QK_W, ML_QK_W, ML_V_W, ML_V_W, ML_HEADS, ML_HEADS, D_MODEL, D_MODEL)
IN_WIDTH = ATT_Q_W + 2 * ATT_KV_W + 2 * ML_QK_W + 2 * ML_V_W + 2 * ML_HEADS + 2 * D_MODEL

kernel_name = 'hybrid_swa_mlstm_hmoe_deepnorm'


def _split_columns(t, widths):
    parts, start = [], 0
    for w in widths:
        parts.append(t[..., start:start + w])
        start += w
    return parts


def layer_norm(x, g, b):
    xf = x.astype(jnp.float32)
    mu = jnp.mean(xf, -1, keepdims=True)
    var = jnp.mean(jnp.square(xf - mu), -1, keepdims=True)
    return ((xf - mu) * lax.rsqrt(var + LN_EPS) * g.astype(jnp.float32) + b.astype(jnp.float32)).astype(x.dtype)


def rope(x, positions):
    half = x.shape[-1] // 2
    inv = ROPE_THETA ** (-jnp.arange(half, dtype=jnp.float32) / half)
    ang = positions.astype(jnp.float32)[..., None] * inv
    cos = jnp.cos(ang)[:, :, None, :]
    sin = jnp.sin(ang)[:, :, None, :]
    xf = x.astype(jnp.float32)
    x1, x2 = xf[..., :half], xf[..., half:]
    return jnp.concatenate([x1 * cos - x2 * sin, x2 * cos + x1 * sin], -1).astype(x.dtype)


def sliding_window_attention(q, k, v, sinks):
    B, S, Hq, Dh = q.shape
    Hkv = k.shape[2]
    G = Hq // Hkv
    nb = S // ATT_BLOCK
    qb = q.reshape(B, nb, ATT_BLOCK, Hkv, G, Dh)

    def with_prev(t):
        tb = t.reshape(B, nb, ATT_BLOCK, Hkv, Dh)
        prev = jnp.pad(tb[:, :-1], ((0, 0), (1, 0), (0, 0), (0, 0), (0, 0)))
        return jnp.concatenate([prev, tb], axis=2)

    kb, vb = with_prev(k), with_prev(v)
    scores = jnp.einsum('bnqhgd,bnkhd->bnhgqk', qb, kb, preferred_element_type=jnp.float32) * (Dh ** -0.5)
    blk = jnp.arange(nb)[:, None, None]
    qpos = blk * ATT_BLOCK + jnp.arange(ATT_BLOCK)[None, :, None]
    kpos = (blk - 1) * ATT_BLOCK + jnp.arange(2 * ATT_BLOCK)[None, None, :]
    diff = qpos - kpos
    valid = (diff >= 0) & (diff < WINDOW) & (kpos >= 0)
    scores = jnp.where(valid[None, :, None, None], scores, -jnp.inf)
    sink = sinks.astype(jnp.float32).reshape(1, 1, Hkv, G, 1)
    m = jnp.maximum(jnp.max(scores, -1), sink)
    e = jnp.exp(scores - m[..., None])
    den = jnp.sum(e, -1) + jnp.exp(sink - m)
    probs = e / den[..., None]
    out = jnp.einsum('bnhgqk,bnkhd->bnqhgd', probs.astype(v.dtype), vb)
    return out.reshape(B, S, Hq * Dh)


def causal_depthwise_conv(x, w, b):
    out = lax.conv_general_dilated(x, w[:, None, :].astype(x.dtype), window_strides=(1,),
                                   padding=[(CONV_WIDTH - 1, 0)],
                                   dimension_numbers=('NWC', 'WIO', 'NWC'),
                                   feature_group_count=x.shape[-1])
    return out + b.astype(x.dtype)


def mlstm_chunkwise(q, k, v, log_i, log_f):
    B, H, S, dqk = q.shape
    dv = v.shape[-1]
    L = ML_CHUNK
    nc = S // L

    def to_chunks(t):
        return jnp.moveaxis(t.reshape((B, H, nc, L) + t.shape[3:]), 2, 0)

    xs = (to_chunks(q), to_chunks(k), to_chunks(v), to_chunks(log_i), to_chunks(log_f))
    causal = jnp.tril(jnp.ones((L, L), dtype=bool))

    def step(carry, inp):
        C, n, m = carry
        qx, kx, vx, ix, fx = inp
        b = jnp.cumsum(fx, axis=-1)
        d = jnp.where(causal, b[..., :, None] - b[..., None, :] + ix[..., None, :], -jnp.inf)
        inter = b + m[..., None]
        m_t = jnp.maximum(inter, jnp.max(d, -1))
        w_intra = jnp.exp(d - m_t[..., None])
        w_inter = jnp.exp(inter - m_t)
        s = jnp.einsum('bhtd,bhsd->bhts', qx, kx) * w_intra
        num = w_inter[..., None] * jnp.einsum('bhtd,bhde->bhte', qx, C) + jnp.einsum('bhts,bhse->bhte', s, vx)
        den = w_inter * jnp.einsum('bhtd,bhd->bht', qx, n) + jnp.sum(s, -1)
        h = num / jnp.maximum(jnp.abs(den), jnp.exp(-m_t))[..., None]
        b_end = b[..., -1]
        d_end = b_end[..., None] - b + ix
        m_new = jnp.maximum(b_end + m, jnp.max(d_end, -1))
        w_state = jnp.exp(b_end + m - m_new)
        w_k = jnp.exp(d_end - m_new[..., None])
        C_new = w_state[..., None, None] * C + jnp.einsum('bhs,bhsd,bhse->bhde', w_k, kx, vx)
        n_new = w_state[..., None] * n + jnp.einsum('bhs,bhsd->bhd', w_k, kx)
        return (C_new, n_new, m_new), h

    init = (jnp.zeros((B, H, dqk, dv), jnp.float32), jnp.zeros((B, H, dqk), jnp.float32), jnp.zeros((B, H), jnp.float32))
    _, h = lax.scan(step, init, xs)
    return jnp.moveaxis(h, 0, 2).reshape(B, H, S, dv).transpose(0, 2, 1, 3)


def hybrid_mixer(x, positions, w_in, attn_sinks, conv_w, conv_b, b_igate, b_fgate, ml_norm_g,
                 w_att_branch, w_ml_branch, w_mix_out):
    B, S, _ = x.shape
    f32 = jnp.float32
    proj = x @ w_in
    aq, ak, av, mq, mk, mv, mo, mi, mf, ga, gm = _split_columns(proj, IN_WIDTHS)
    aq = rope(aq.reshape(B, S, ATT_HEADS, ATT_HEAD_DIM), positions)
    ak = rope(ak.reshape(B, S, ATT_KV_HEADS, ATT_HEAD_DIM), positions)
    av = av.reshape(B, S, ATT_KV_HEADS, ATT_HEAD_DIM)
    a_out = sliding_window_attention(aq, ak, av, attn_sinks) @ w_att_branch
    qk_conv = jax.nn.silu(causal_depthwise_conv(jnp.concatenate([mq, mk], -1), conv_w, conv_b))
    mq, mk = qk_conv[..., :ML_QK_W], qk_conv[..., ML_QK_W:]

    def heads_first(t, d):
        return t.reshape(B, S, ML_HEADS, d).transpose(0, 2, 1, 3).astype(f32)

    q = heads_first(mq, ML_DQK)
    k = heads_first(mk, ML_DQK) * (ML_DQK ** -0.5)
    v = heads_first(mv, ML_DV)
    log_i = (mi.astype(f32) + b_igate.astype(f32)).transpose(0, 2, 1)
    log_f = jax.nn.log_sigmoid(mf.astype(f32) + b_fgate.astype(f32)).transpose(0, 2, 1)
    hm = mlstm_chunkwise(q, k, v, log_i, log_f)
    hm = hm * lax.rsqrt(jnp.mean(hm * hm, -1, keepdims=True) + RMS_EPS)
    hm = hm.reshape(B, S, ML_V_W) * ml_norm_g.astype(f32)
    hm = (hm * jax.nn.sigmoid(mo.astype(f32))).astype(x.dtype)
    m_out = hm @ w_ml_branch
    y = jax.nn.sigmoid(ga) * a_out + jax.nn.sigmoid(gm) * m_out
    return y @ w_mix_out


def memory_cross_attention(x, mem, w_xq, w_xkv, w_xo):
    B, S, _ = x.shape
    M = mem.shape[1]
    q = (x @ w_xq).reshape(B, S, X_HEADS, X_HEAD_DIM)
    kv = mem @ w_xkv
    k = kv[..., :D_MODEL].reshape(B, M, X_HEADS, X_HEAD_DIM)
    v = kv[..., D_MODEL:].reshape(B, M, X_HEADS, X_HEAD_DIM)
    s = jnp.einsum('bshd,bmhd->bhsm', q, k, preferred_element_type=jnp.float32) * (X_HEAD_DIM ** -0.5)
    p = jax.nn.softmax(s, axis=-1)
    o = jnp.einsum('bhsm,bmhd->bshd', p.astype(v.dtype), v).reshape(B, S, D_MODEL)
    return o @ w_xo


def hierarchical_moe(x, w_router_group, b_router_group, w_router_expert, b_router_expert, w_gate, w_up, w_down):
    B, S, D = x.shape
    N = B * S
    xf = x.reshape(N, D)
    g_logits = (xf @ w_router_group).astype(jnp.float32) + b_router_group.astype(jnp.float32)
    g_prob = jax.nn.softmax(g_logits, axis=-1)
    g_sel = jnp.argmax(g_logits, axis=-1)
    p_group = jnp.take_along_axis(g_prob, g_sel[:, None], axis=-1)
    e_logits = ((xf @ w_router_expert).astype(jnp.float32) + b_router_expert.astype(jnp.float32)).reshape(N, N_GROUPS, EXPERTS_PER_GROUP)
    e_in_group = jnp.take_along_axis(e_logits, g_sel[:, None, None], axis=1)[:, 0]
    top_val, top_idx = lax.top_k(e_in_group, TOP_K)
    weights = p_group * jax.nn.softmax(top_val, axis=-1)
    expert_id = g_sel[:, None] * EXPERTS_PER_GROUP + top_idx
    A = N * TOP_K
    flat_e = expert_id.reshape(A)
    flat_tok = jnp.arange(A, dtype=jnp.int32) // TOP_K
    flat_w = weights.reshape(A)
    order = jnp.argsort(flat_e)
    se, stok, sw = flat_e[order], flat_tok[order], flat_w[order]
    counts = jnp.zeros((N_EXPERTS,), jnp.int32).at[flat_e].add(1)
    padded = ((counts + MOE_BLOCK - 1) // MOE_BLOCK) * MOE_BLOCK
    pad_end = jnp.cumsum(padded)
    pad_start = pad_end - padded
    cnt_start = jnp.cumsum(counts) - counts
    dest = pad_start[se] + (jnp.arange(A, dtype=jnp.int32) - cnt_start[se])
    n_blocks = -(-A // MOE_BLOCK) + N_EXPERTS
    R = n_blocks * MOE_BLOCK
    row_tok = jnp.zeros((R,), jnp.int32).at[dest].set(stok)
    row_w = jnp.zeros((R,), jnp.float32).at[dest].set(sw)
    block_start = jnp.arange(n_blocks, dtype=jnp.int32) * MOE_BLOCK
    block_expert = jnp.minimum(jnp.searchsorted(pad_end, block_start, side='right'), N_EXPERTS - 1)

    def expert_block(args):
        toks, e = args
        xb = xf[toks]
        hb = jax.nn.silu(xb @ w_gate[e]) * (xb @ w_up[e])
        return hb @ w_down[e]

    y_rows = lax.map(expert_block, (row_tok.reshape(n_blocks, MOE_BLOCK), block_expert))
    y_rows = y_rows.reshape(R, D) * row_w[:, None].astype(x.dtype)
    y = jax.ops.segment_sum(y_rows, row_tok, num_segments=N)
    return y.reshape(B, S, D)


def setup_inputs(seed: int = 0) -> dict:
    key = jax.random.key(seed)
    ks = jax.random.split(key, 32)
    nrm = jax.random.normal
    f32 = jnp.float32
    inv = D_MODEL ** -0.5
    positions = jnp.broadcast_to(jnp.arange(SEQ, dtype=jnp.int32)[None, :], (BATCH, SEQ))
    return {
        'x': nrm(ks[0], (BATCH, SEQ, D_MODEL), f32),
        'mem': nrm(ks[1], (BATCH, MEM_LEN, D_MODEL), f32),
        'positions': positions,
        'w_in': nrm(ks[2], (DEPTH, D_MODEL, IN_WIDTH), f32) * inv,
        'attn_sinks': nrm(ks[3], (DEPTH, ATT_HEADS), f32) * 0.5,
        'conv_w': nrm(ks[4], (DEPTH, CONV_WIDTH, 2 * ML_QK_W), f32) * (CONV_WIDTH ** -0.5),
        'conv_b': nrm(ks[5], (DEPTH, 2 * ML_QK_W), f32) * 0.02,
        'b_igate': nrm(ks[6], (DEPTH, ML_HEADS), f32) * 0.1,
        'b_fgate': jnp.linspace(3.0, 6.0, ML_HEADS, dtype=f32)[None, :] + nrm(ks[7], (DEPTH, ML_HEADS), f32) * 0.1,
        'ml_norm_g': 1.0 + nrm(ks[8], (DEPTH, ML_V_W), f32) * 0.02,
        'w_att_branch': nrm(ks[9], (DEPTH, ATT_Q_W, D_MODEL), f32) * (ATT_Q_W ** -0.5),
        'w_ml_branch': nrm(ks[10], (DEPTH, ML_V_W, D_MODEL), f32) * (ML_V_W ** -0.5),
        'w_mix_out': nrm(ks[11], (DEPTH, D_MODEL, D_MODEL), f32) * inv * BETA,
        'ln1_g': 1.0 + nrm(ks[12], (DEPTH, D_MODEL), f32) * 0.02,
        'ln1_b': nrm(ks[13], (DEPTH, D_MODEL), f32) * 0.02,
        'w_xq': nrm(ks[14], (DEPTH, D_MODEL, D_MODEL), f32) * inv,
        'w_xkv': nrm(ks[15], (DEPTH, D_MODEL, 2 * D_MODEL), f32) * inv,
        'w_xo': nrm(ks[16], (DEPTH, D_MODEL, D_MODEL), f32) * inv * BETA,
        'ln2_g': 1.0 + nrm(ks[17], (DEPTH, D_MODEL), f32) * 0.02,
        'ln2_b': nrm(ks[18], (DEPTH, D_MODEL), f32) * 0.02,
        'w_router_group': nrm(ks[19], (DEPTH, D_MODEL, N_GROUPS), f32) * inv,
        'b_router_group': nrm(ks[20], (DEPTH, N_GROUPS), f32) * 0.01,
        'w_router_expert': nrm(ks[21], (DEPTH, D_MODEL, N_EXPERTS), f32) * inv,
        'b_router_expert': nrm(ks[22], (DEPTH, N_EXPERTS), f32) * 0.01,
        'w_gate': nrm(ks[23], (DEPTH, N_EXPERTS, D_MODEL, D_EXPERT), f32) * inv,
        'w_up': nrm(ks[24], (DEPTH, N_EXPERTS, D_MODEL, D_EXPERT), f32) * inv,
        'w_down': nrm(ks[25], (DEPTH, N_EXPERTS, D_EXPERT, D_MODEL), f32) * (D_EXPERT ** -0.5) * BETA,
        'ln3_g': 1.0 + nrm(ks[26], (DEPTH, D_MODEL), f32) * 0.02,
        'ln3_b': nrm(ks[27], (DEPTH, D_MODEL), f32) * 0.02,
    }


def reference(x, mem, positions, w_in, attn_sinks, conv_w, conv_b, b_igate, b_fgate, ml_norm_g,
              w_att_branch, w_ml_branch, w_mix_out, ln1_g, ln1_b, w_xq, w_xkv, w_xo, ln2_g, ln2_b,
              w_router_group, b_router_group, w_router_expert, b_router_expert, w_gate, w_up, w_down,
              ln3_g, ln3_b):
    h = x
    for l in range(DEPTH):
        mix = hybrid_mixer(h, positions, w_in[l], attn_sinks[l], conv_w[l], conv_b[l], b_igate[l], b_fgate[l],
                           ml_norm_g[l], w_att_branch[l], w_ml_branch[l], w_mix_out[l])
        h = layer_norm(ALPHA * h + mix, ln1_g[l], ln1_b[l])
        xa = memory_cross_attention(h, mem, w_xq[l], w_xkv[l], w_xo[l])
        h = layer_norm(ALPHA * h + xa, ln2_g[l], ln2_b[l])
        ff = hierarchical_moe(h, w_router_group[l], b_router_group[l], w_router_expert[l], b_router_expert[l],
                              w_gate[l], w_up[l], w_down[l])
        h = layer_norm(ALPHA * h + ff, ln3_g[l], ln3_b[l])
    return h
```

```python
import math
import contextlib
import numpy as np
import concourse.bass as bass
import concourse.mybir as mybir
from concourse.bass_utils import run_bass_kernel_spmd

F32 = mybir.dt.float32
BF16 = mybir.dt.bfloat16
I32 = mybir.dt.int32
ALU = mybir.AluOpType
AF = mybir.ActivationFunctionType
AX = mybir.AxisListType

D = 1024
NCORES = 8
ALPHA = 2.0 ** 0.25
LN_EPS = 1e-5
RMS_EPS = 1e-6
N_DMA_SEMS = 12
ST = 512
NEXP = 64
MEM = 256

O_AQ, O_AK, O_AV, O_MQ, O_MK, O_MV, O_MO, O_MI, O_MF, O_GA, O_GM = (
    0, 1024, 1152, 1280, 1792, 2304, 3328, 4352, 4356, 4360, 5384)


class Sched:
    COMPUTE = ("pe", "act", "dve", "pool")

    def __init__(self, nc, dma_queues=("sp", "act", "pool")):
        self.nc = nc
        self.ops = {e: [] for e in ("pe", "act", "dve", "pool", "sp")}
        self.last_w = {}
        self.readers = {}
        self.dma_queues = dma_queues
        self.dma_count = {q: 0 for q in dma_queues}
        self.fence_toks = []
        self.fence_pending = set()

    def fence(self):
        toks = []
        for e in self.COMPUTE:
            for i in range(len(self.ops[e]) - 1, -1, -1):
                if self.ops[e][i]["dma"] is None:
                    toks.append(("c", e, i))
                    break
        for q in self.dma_queues:
            n = self.dma_count[q]
            for k in range(max(0, n - N_DMA_SEMS), n):
                toks.append(("d", q, k))
        self.fence_toks = toks
        self.fence_pending = set(self.ops.keys())

    def _fence_deps(self, eng, deps):
        if eng in self.fence_pending:
            self.fence_pending.discard(eng)
            deps = list(deps) + list(self.fence_toks)
        return deps

    def _deps(self, reads, writes):
        deps = []
        for r in reads:
            t = self.last_w.get(r)
            if t is not None:
                deps.append(t)
        for w in writes:
            t = self.last_w.get(w)
            if t is not None:
                deps.append(t)
            deps.extend(self.readers.get(w, ()))
        return deps

    def _commit(self, tok, reads, writes):
        for r in reads:
            self.readers.setdefault(r, []).append(tok)
        for w in writes:
            self.last_w[w] = tok
            self.readers[w] = []

    def op(self, eng, fn, reads=(), writes=()):
        deps = self._fence_deps(eng, self._deps(reads, writes))
        idx = len(self.ops[eng])
        tok = ("c", eng, idx)
        if eng == "pe":
            deps = [d for d in deps if not (d[0] == "c" and d[1] == "pe")]
        self.ops[eng].append({"fn": fn, "deps": deps, "signal": False, "dma": None})
        self._commit(tok, reads, writes)
        return tok

    def dma(self, queue, fn, reads=(), writes=()):
        deps = self._fence_deps(queue, self._deps(reads, writes))
        n = self.dma_count[queue]
        self.dma_count[queue] += 1
        tok = ("d", queue, n)
        if n >= N_DMA_SEMS:
            deps.append(("d", queue, n - N_DMA_SEMS))
        self.ops[queue].append({"fn": fn, "deps": deps, "signal": False, "dma": n})
        self._commit(tok, reads, writes)
        return tok

    def emit(self, final_wait_tokens=()):
        nc = self.nc
        for e, lst in self.ops.items():
            for o in lst:
                for d in o["deps"]:
                    if d[0] == "c":
                        self.ops[d[1]][d[2]]["signal"] = True
        for d in final_wait_tokens:
            if d[0] == "c":
                self.ops[d[1]][d[2]]["signal"] = True
        semval = {}
        for e in self.COMPUTE:
            c = 0
            vals = []
            for o in self.ops[e]:
                if o["signal"] and o["dma"] is None:
                    c += 1
                vals.append(c)
            semval[e] = vals
        with contextlib.ExitStack() as st:
            csem = {e: st.enter_context(nc.semaphore("cs_" + e)) for e in self.COMPUTE}
            dsem = {q: [st.enter_context(nc.semaphore("ds_%s_%d" % (q, i))) for i in range(N_DMA_SEMS)]
                    for q in self.dma_queues}
            block = st.enter_context(nc.Block())

            def need(tok):
                if tok[0] == "c":
                    return ("c", tok[1]), csem[tok[1]], semval[tok[1]][tok[2]]
                q, n = tok[1], tok[2]
                return ("d", q, n % N_DMA_SEMS), dsem[q][n % N_DMA_SEMS], 16 * (n // N_DMA_SEMS + 1)

            def run(e, extra=()):
                def body(engine):
                    waited = {}
                    for o in self.ops[e]:
                        reqs = {}
                        for d in o["deps"]:
                            key, sem, val = need(d)
                            if waited.get(key, 0) >= val:
                                continue
                            if key not in reqs or reqs[key][1] < val:
                                reqs[key] = (sem, val)
                        for key, (sem, val) in reqs.items():
                            engine.wait_ge(sem, val)
                            waited[key] = val
                        inst = o["fn"](engine)
                        if o["dma"] is not None:
                            inst.then_inc(dsem[e][o["dma"] % N_DMA_SEMS], 16)
                        elif o["signal"]:
                            inst.then_inc(csem[e], 1)
                    for d in extra:
                        key, sem, val = need(d)
                        if waited.get(key, 0) < val:
                            engine.wait_ge(sem, val)
                            waited[key] = val
                return body

            block.sync(run("sp", final_wait_tokens))
            block.tensor(run("pe"))
            block.scalar(run("act"))
            block.vector(run("dve"))
            block.gpsimd(run("pool"))


def _tile_block(W, cols):
    cols = np.asarray(cols)
    blk = np.zeros((1024, 512), np.float32)
    ok = cols >= 0
    blk[:, ok] = W[:, cols[ok]]
    return blk.reshape(8, 128, 512).transpose(1, 0, 2)


def win_blocks():
    r = np.arange
    blocks = []
    for g in range(2):
        cols = []
        for c in range(4):
            h0, h1 = 8 * g + c, 8 * g + 4 + c
            cols += list(O_AQ + h0 * 64 + r(64)) + list(O_AQ + h1 * 64 + r(64))
        blocks.append(cols)
    cols = []
    for g in range(2):
        cols += list(O_AK + g * 64 + r(64)) * 2
    cols += list(O_MI + r(4)) + [-1] * 124
    cols += list(O_MF + r(4)) + [-1] * 124
    blocks.append(cols)
    blocks.append(list(O_MQ + r(512)))
    blocks.append(list(O_MK + r(512)))
    blocks.append(list(O_GA + r(512)))
    blocks.append(list(O_GA + 512 + r(512)))
    blocks.append(list(O_GM + r(512)))
    blocks.append(list(O_GM + 512 + r(512)))
    blocks.append(list(O_MV + r(512)))
    blocks.append(list(O_MV + 512 + r(512)))
    blocks.append(list(O_MO + r(512)))
    blocks.append(list(O_MO + 512 + r(512)))
    blocks.append(list(O_AV + r(128)) + [-1] * 384)
    return blocks


B_ATT, B_ML, B_MIX, B_XQ, B_XO, B_XKV = 14, 16, 18, 20, 22, 24
NBLK = 28


def att_row_perm():
    rows = []
    for g in range(2):
        for c in range(4):
            h0, h1 = 8 * g + c, 8 * g + 4 + c
            rows += list(h0 * 64 + np.arange(64)) + list(h1 * 64 + np.arange(64))
    return np.asarray(rows)


def build_wall(w_in, w_att, w_ml, w_mix, w_xq, w_xo, w_xkv):
    wall = np.empty((NBLK, 128, 8, 512), np.float32)
    for i, cols in enumerate(win_blocks()):
        wall[i] = _tile_block(w_in, cols)
    watt = w_att[att_row_perm(), :]
    k = 14
    for W in (watt, w_ml, w_mix, w_xq, w_xo):
        for n in range(2):
            wall[k] = _tile_block(W, list(n * 512 + np.arange(512)))
            k += 1
    for n in range(4):
        wall[k] = _tile_block(w_xkv, list(n * 512 + np.arange(512)))
        k += 1
    return wall.reshape(NBLK, 128, 4096)


def build_consts():
    c = {}
    c["ident"] = np.eye(128, dtype=np.float32)
    Rm = np.zeros((128, 128), np.float32)
    for hh in range(2):
        for d in range(32):
            Rm[hh * 64 + d + 32, hh * 64 + d] = -1.0
            Rm[hh * 64 + d, hh * 64 + d + 32] = 1.0
    c["rm"] = Rm
    k = np.arange(128)[:, None]
    q = np.arange(128)[None, :]
    c["mcur"] = np.tile((k <= q).astype(np.float32), (1, 4))
    c["mprev"] = np.tile((k > q).astype(np.float32), (1, 4))
    c["bigm"] = np.where(k > q, 1e30, 0.0).astype(np.float32)
    c["ustr"] = (k < q).astype(np.float32)
    c["ones"] = np.ones((128, 128), np.float32)
    sel = np.zeros((128, 512), np.float32)
    for h in range(4):
        sel[h, h * 128:(h + 1) * 128] = 1.0
    c["sel"] = sel
    c["iota64"] = np.tile(np.arange(64, dtype=np.float32)[None, :], (128, 1))
    c["blk128"] = np.tile((np.arange(256, dtype=np.float32) * 128.0)[None, :], (128, 1))
    c["pidx"] = np.tile(np.arange(128, dtype=np.float32)[:, None], (1, 8))
    names = ["ident", "rm", "mcur", "mprev", "bigm", "ustr", "ones", "sel", "iota64", "blk128", "pidx"]
    offs = {}
    o = 0
    for n in names:
        offs[n] = (o, c[n].shape[1])
        o += c[n].shape[1]
    arr = np.concatenate([c[n] for n in names], axis=1)
    return arr, offs


_CONST_ARR, _COFF = build_consts()
NCONST = _CONST_ARR.shape[1]


def build(S, NB, stage=3, nblk_moe=None):
    assert S % ST == 0
    NST = S // ST
    NTOK = NB * S
    NTT = NTOK // 128
    A = NTOK * 2
    NBLKM = A // 128 + NEXP if nblk_moe is None else nblk_moe
    R = NBLKM * 128

    nc = bass.Bass("TRN2", target_bir_lowering=False)
    dx = nc.dram_tensor("x", [NB, S, D], F32, kind="ExternalInput").ap()
    dmem = nc.dram_tensor("mem", [NB, MEM, D], F32, kind="ExternalInput").ap()
    dpos = nc.dram_tensor("pos", [NB, S], I32, kind="ExternalInput").ap()
    dwall = nc.dram_tensor("wall", [NBLK, 128, 4096], F32, kind="ExternalInput").ap()
    dconst = nc.dram_tensor("consts", [128, NCONST], F32, kind="ExternalInput").ap()
    dvec = nc.dram_tensor("vecs", [6, D], F32, kind="ExternalInput").ap()
    dpcol = nc.dram_tensor("pcol", [128, 56], F32, kind="ExternalInput").ap()
    dsm = nc.dram_tensor("smalls", [128], F32, kind="ExternalInput").ap()
    dwr = nc.dram_tensor("wr", [128, 8, 72], F32, kind="ExternalInput").ap()
    dwg = nc.dram_tensor("wgu", [NEXP * 128, 4096], F32, kind="ExternalInput").ap()
    dwd = nc.dram_tensor("wd", [NEXP * 128, 2048], F32, kind="ExternalInput").ap()
    dout = nc.dram_tensor("out", [NB, S, D], F32, kind="ExternalOutput").ap()
    WS = nc.dram_tensor("ws", [NBLK, 128, 4096], BF16, kind="Internal").ap()
    H32 = nc.dram_tensor("h32", [NTOK, D], F32, kind="Internal").ap()
    XS = nc.dram_tensor("xs", [R, D], BF16, kind="Internal").ap()
    YS = nc.dram_tensor("ys", [R, D], F32, kind="Internal").ap()
    WGB = nc.dram_tensor("wgb", [NEXP * 128, 4096], BF16, kind="Internal").ap()
    WDB = nc.dram_tensor("wdb", [NEXP * 128, 2048], BF16, kind="Internal").ap()

    sch = Sched(nc)
    es = contextlib.ExitStack()
    AW = 53000
    arena = es.enter_context(nc.sbuf_tensor("arena", [128, AW], F32))
    ptr = [0]

    def carve(shape, dt=F32, parts=128):
        n = int(np.prod(shape[1:]))
        words = n if dt in (F32, I32) else (n + 1) // 2
        words = (words + 7) // 8 * 8
        a = arena[0:parts, ptr[0]:ptr[0] + words]
        ptr[0] += words
        assert ptr[0] <= AW, "arena overflow %d" % ptr[0]
        if dt != F32:
            a = a.bitcast(dt)
        a = a[:, 0:n]
        if len(shape) == 3:
            a = a.rearrange("p (a b) -> p a b", a=shape[1])
        elif len(shape) == 4:
            a = a.rearrange("p (a b c) -> p a b c", a=shape[1], b=shape[2])
        return a

    cst = carve([128, NCONST])
    identb = carve([128, 128], BF16)
    onesb = carve([128, 128], BF16)
    ustrb = carve([128, 128], BF16)
    lnp = carve([128, 4, D])
    pcol = carve([128, 56])
    smb = carve([128, 128])
    xsb = carve([128, 4, D])
    RW = carve([128, NTT, 6])
    posI = carve([128, NTT, 2], I32)
    ohcum = carve([128, 64], BF16)
    stats4 = carve([128, 4, 2, 6])
    mvt4 = carve([128, 4, 2])
    p1_base = ptr[0]
    rmb = carve([128, 128], BF16)
    mcurb = carve([128, 512], BF16)
    mprevb = carve([128, 512], BF16)
    hi16 = carve([1, 16], BF16, parts=1)
    lo32 = carve([1, 16], parts=1)
    NRING = 2
    wring = [carve([128, 8, 512], BF16) for i in range(NRING)]
    fmb = [carve([128, 8, ST], BF16) for i in range(6)]
    xT, qT, sga, sgm, attT, hmT = fmb
    RxT, RqT, Rsga, Rsgm, RattT, RhmT = ["fm%d" % i for i in range(6)]
    qkc, Rqkc = xT, RxT
    yT, RyT = qT, RqT
    h1T, Rh1T = xT, RxT
    qxT, RqxT = attT, RattT
    oxT, RoxT = hmT, RhmT
    kdT = carve([128, 2, 5, 128], BF16)
    vatt = carve([128, 5, 128], BF16)
    pre = carve([128, 8, 3 + ST + 1], BF16)
    vext = carve([128, 4, 4, 264], BF16)
    sigmo = carve([128, 4, D], BF16)
    t32ab = carve([128, 2 * ST]); t32a = t32ab[:, 0:ST]; t32b = t32ab[:, ST:2 * ST]; t32c = carve([128, ST])
    tb16 = carve([128, ST], BF16)
    cosT = carve([128, ST]); sinT = carve([128, ST])
    PT6 = carve([128, 8, 512], BF16)
    rden = carve([128, 512])
    posi = rden.bitcast(I32)
    _o = ptr[0]
    rowi = carve([4, ST], parts=4); rowsp = carve([4, ST], parts=4); rowF = carve([4, ST], parts=4)
    esk = arena[32:34, _o:_o + 1024].bitcast(BF16).rearrange("p (a b) -> p a b", a=4)
    ones2 = arena[32:34, _o + 1024:_o + 1056].bitcast(BF16)
    rowG = carve([4, ST], parts=4)
    rowa = rowi; rowm = rowsp
    ones4 = carve([4, ST], parts=4)
    carry = carve([4, 2], parts=4)
    Gb = carve([128, 4, ST])
    memsb = Gb.rearrange("p a b -> p (a b)").rearrange("p (a b) -> p a b", a=2)
    gprev = carve([128, 4])
    _gbf = Gb.rearrange("p a b -> p (a b)")
    _ro = [0]

    def galias(shape, dt=F32):
        n = int(np.prod(shape[1:]))
        words = n if dt == F32 else (n + 1) // 2
        words = (words + 7) // 8 * 8
        v = _gbf[:, _ro[0]:_ro[0] + words]
        _ro[0] += words
        assert _ro[0] <= 2048
        if dt != F32:
            v = v.bitcast(dt)
        v = v[:, 0:n]
        if len(shape) == 3:
            v = v.rearrange("p (a b) -> p a b", a=shape[1])
        elif len(shape) == 4:
            v = v.rearrange("p (a b c) -> p a b c", a=shape[1], b=shape[2])
        return v
    lg4 = galias([128, 4, 72]); lem4 = galias([128, 4, 64]); top84 = galias([128, 4, 8]); oh4 = galias([128, 4, 2, 64])
    ohb4 = galias([128, 4, 64], BF16); j4 = galias([128, 4, 64]); d4 = galias([128, 4, 8]); pen4 = galias([128, 4, 8])
    rq = galias([128, 12, 4]); ohs = galias([128, 64])
    ROUTE_RES = ["lg4", "lem4", "top84", "oh4", "ohb4", "j4", "d4", "pen4", "rq", "ohs"]
    aT = carve([128, 4, 4]); emt = carve([128, 4, 4])
    S32 = carve([128, 4, 264]); Sbf = carve([128, 4, 264], BF16)
    _mo = _COFF["mcur"][0]; _po = _COFF["mprev"][0]
    Wt4 = cst[:, _mo:_mo + 512].rearrange("p (a b) -> p a b", a=4)
    Gm4 = cst[:, _po:_po + 512].rearrange("p (a b) -> p a b", a=4)
    wint4 = carve([128, 4, 128])
    PTm4 = carve([128, 4, 128], BF16); qtil4 = carve([128, 4, 128], BF16); ktil4 = carve([128, 4, 128], BF16)
    hmtok4 = carve([128, 4, 256], BF16)
    sm4 = carve([128, 16, 4]); sqj = rden[:, 0:256]
    PxT2 = carve([128, 2, ST], BF16); rden2 = t32c
    memT = t32ab.bitcast(BF16).rearrange("p (a b) -> p a b", a=8); KmT = carve([128, 8, MEM], BF16); Vm = carve([128, 2, D], BF16)
    PxT = carve([128, 2, ST], BF16)
    h2Ts = [t32ab.rearrange("p (a b) -> p a b", a=8),
            PT6[:, 0:4, :].rearrange("p a b -> p (a b)").bitcast(F32).rearrange("p (a b) -> p a b", a=8)]
    H2R = [["t32a", "t32b"], [("PTq", i) for i in range(4)]]; wrs = carve([128, 8, 72])
    p1_top = ptr[0]

    ps_t = es.enter_context(nc.psum_tensor("ps", [128, 8, 512], F32))
    psn = [0]

    def bank():
        i = psn[0] % 8
        psn[0] += 1
        return ps_t[:, i, :], ("ps", i)

    def C(name):
        o, w = _COFF[name]
        return cst[:, o:o + w]

    def mm(out, lhsT, rhs, start, stop, r, w):
        return sch.op("pe", lambda e: e.matmul(out, lhsT, rhs, start=start, stop=stop), r, w)

    def tr(out, in_, ident, r, w):
        return sch.op("pe", lambda e: e.transpose(out, in_, ident), r, w)

    def act(out, in_, func, r, w, bias=0.0, scale=1.0, accum_out=None):
        if accum_out is None:
            return sch.op("act", lambda e: e.activation(out, in_, func, bias=bias, scale=scale), r, w)
        return sch.op("act", lambda e: e.activation(out, in_, func, bias=bias, scale=scale, accum_out=accum_out), r, w)

    def tt(eng, out, in0, in1, op, r, w):
        return sch.op(eng, lambda e: e.tensor_tensor(out, in0, in1, op), r, w)

    def ts(eng, out, in0, s1, s2, op0, op1, r, w):
        if op1 is None:
            return sch.op(eng, lambda e: e.tensor_scalar(out, in0, s1, None, op0), r, w)
        return sch.op(eng, lambda e: e.tensor_scalar(out, in0, s1, s2, op0, op1), r, w)

    def stt(out, in0, scalar, in1, op0, op1, r, w):
        return sch.op("dve", lambda e: e.scalar_tensor_tensor(out, in0, scalar, in1, op0, op1), r, w)

    def cp(eng, out, in_, r, w):
        if eng == "act":
            return sch.op("act", lambda e: e.activation(out, in_, AF.Copy), r, w)
        return sch.op(eng, lambda e: e.tensor_copy(out, in_), r, w)

    def red(out, in_, op, r, w):
        return sch.op("dve", lambda e: e.tensor_reduce(out, in_, AX.X, op), r, w)

    def recip(out, in_, r, w):
        return sch.op("dve", lambda e: e.reciprocal(out, in_), r, w)

    def dma(q, out, in_, r, w):
        return sch.dma(q, lambda e: e.dma_start(out, in_), r, w)

    def memset(eng, ap, val, w):
        return sch.op(eng, lambda e: e.memset(ap, val), (), w)

    XSB = [("xsb", i) for i in range(4)]
    gcol = pcol[:, 0:8]; cb = pcol[:, 8:16]; invf = pcol[:, 48:49]
    def cwt(ch, j):
        return pcol[:, 16 + ch * 4 + j:16 + ch * 4 + j + 1]

    dma("sp", cst, dconst, (), ["cst"])
    dma("sp", pcol, dpcol, (), ["pcol"])
    dma("sp", smb, dsm.partition_broadcast(128), (), ["smb"])
    for i in range(4):
        dma("sp", lnp[:, i, :], dvec[i, :].partition_broadcast(128), (), ["lnp"])
    dma("sp", wrs, dwr, (), ["wrs"])
    cp("act", identb, C("ident"), ["cst"], ["identb"])
    cp("act", rmb, C("rm"), ["cst"], ["rmb"])
    cp("act", mcurb, C("mcur"), ["cst"], ["mcurb"])
    cp("act", mprevb, C("mprev"), ["cst"], ["mprevb"])
    cp("act", onesb, C("ones"), ["cst"], ["onesb"])
    cp("act", ustrb, C("ustr"), ["cst"], ["ustrb"])
    memset("dve", ones4, 1.0, ["ones4"])
    memset("dve", ones2, 1.0, ["ones2"])
    memset("dve", ohcum, 0.0, ["ohcum"])
    memset("pool", vext, 1.0, ["vext"])
    act(smb[0:1, 100:116], smb[0:1, 0:16], AF.Exp, ["smb"], ["smbx"])
    cp("dve", hi16, smb[0:1, 100:116], ["smbx"], ["hi16"])
    tt("dve", lo32, smb[0:1, 100:116], hi16, ALU.subtract, ["smbx", "hi16"], ["lo32"])
    esk0 = t32ab[0:1, :].bitcast(BF16).rearrange("p (a b) -> p a b", a=4)
    esk1 = Gb[0:1, 0:2, :].rearrange("p a b -> p (a b)").bitcast(BF16).rearrange("p (a b) -> p a b", a=4)
    for g in range(2):
        for half in range(2):
            for slot in range(4):
                hh = 8 * g + 4 * half + slot
                cp("dve", esk0[0:1, g * 2 + half, slot * 128:(slot + 1) * 128],
                   hi16[0:1, hh:hh + 1].to_broadcast([1, 128]), ["hi16"], ["t32a", "t32b"])
                cp("dve", esk1[0:1, g * 2 + half, slot * 128:(slot + 1) * 128],
                   lo32[0:1, hh:hh + 1].to_broadcast([1, 128]), ["lo32"], ["Gb"])
    dma("sp", esk[0:1, :, :], esk0, ["t32a", "t32b"], ["esk"])
    dma("sp", esk[1:2, :, :], esk1, ["Gb"], ["esk"])
    xsflat = xsb.rearrange("p a b -> p (a b)")
    cast_engs = ["act", "dve", "pool"]
    for b in range(NBLK):
        if b in (B_ML, B_ML + 1):
            dma("sp", xsflat, dwall[b], (), XSB)
            slot = b % NRING
            wr = ("wr", slot)
            dst = wring[slot]
            for kc in range(8):
                sch.op("act", (lambda e, kc=kc, dst=dst: e.activation(
                    dst[:, kc, :], xsflat[:, kc * 512:(kc + 1) * 512], AF.Copy, scale=gcol[:, kc:kc + 1])),
                    XSB + ["pcol"], [wr])
            dma("pool", WS[b], dst.rearrange("p a b -> p (a b)"), [wr], [("ws", b)])
        else:
            sch.dma("pool", (lambda e, b=b: e.dma_start(WS[b], dwall[b], max_dma_last_dim=8192)), (), [("ws", b)])

    moe_units = [0]
    units_per_st = -(-NEXP // (NB * NST))

    def convert_moe_weights():
        for _ in range(units_per_st):
            e_ = moe_units[0]
            if e_ >= NEXP:
                return
            moe_units[0] += 1
            rows = slice(e_ * 128, (e_ + 1) * 128)
            sch.dma("pool", (lambda e, rows=rows: e.dma_start(WGB[rows, :], dwg[rows, :], max_dma_last_dim=8192)), (), ["WGB"])
            sch.dma("pool", (lambda e, rows=rows: e.dma_start(WDB[rows, :], dwd[rows, :], max_dma_last_dim=8192)), (), ["WDB"])

    ringn = [NBLK]

    def wload(b):
        slot = ringn[0] % NRING
        ringn[0] += 1
        dma("sp", wring[slot].rearrange("p a b -> p (a b)"), WS[b], [("ws", b)], [("wr", slot)])
        return wring[slot], ("wr", slot)

    final_tokens = []

    def layer_norm(tti, which, add_eng="pool", xr=None, res=None):
        if xr is None:
            xr = xsb[:, tti, :]
            res = ("xsb", tti)
        stats = stats4[:, tti]; mvt = mvt4[:, tti, :]
        rs_, rm_ = ("stats", tti), ("mvt", tti)
        for hf in range(2):
            sch.op("dve", (lambda e, hf=hf: e.bn_stats(stats[:, hf, :], xr[:, hf * 512:(hf + 1) * 512])), [res], [rs_])
        sch.op("dve", lambda e: e.bn_aggr(mvt, stats.rearrange("p a b -> p (a b)")), [rs_], [rm_])
        ts("dve", mvt[:, 1:2], mvt[:, 1:2], LN_EPS, None, ALU.add, None, [rm_], [rm_])
        act(mvt[:, 1:2], mvt[:, 1:2], AF.Sqrt, [rm_], [rm_])
        recip(mvt[:, 1:2], mvt[:, 1:2], [rm_], [rm_])
        stt(mvt[:, 0:1], mvt[:, 0:1], -1.0, mvt[:, 1:2], ALU.mult, ALU.mult, [rm_], [rm_])
        act(xr, xr, AF.Identity, [res, rm_], [res], bias=mvt[:, 0:1], scale=mvt[:, 1:2])
        tt("pool" if add_eng == "dve" else "dve", xr, xr, lnp[:, 2 * which, :], ALU.mult, [res, "lnp"], [res])
        tt(add_eng, xr, xr, lnp[:, 2 * which + 1, :], ALU.add, [res, "lnp"], [res])

    io = _COFF["ident"][0]
    so = _COFF["sel"][0]
    tile_no = 0
    for b in range(NB):
        memset("dve", carry, 0.0, ["carry"])
        memset("dve", gprev, 0.0, ["gprev"])
        memset("dve", S32, 0.0, ["S32"])
        memset("dve", Sbf, 0.0, ["Sbf"])
        memset("dve", pre[:, :, 0:3], 0.0, ["pre"])
        if stage >= 2:
            for mt in range(2):
                dma("sp", memsb[:, mt, :], dmem[b, mt * 128:(mt + 1) * 128, :], (), ["Gb"] + ROUTE_RES)
            for kc in range(8):
                pb, pr = bank()
                for mt in range(2):
                    tr(pb[:, mt * 128:(mt + 1) * 128], memsb[:, mt, kc * 128:(kc + 1) * 128], C("ident"), ["Gb", "cst"], [pr])
                cp("act", memT[:, kc, :], pb[:, 0:256], [pr], ["t32a", "t32b"])
            for n in range(4):
                wt_, wr_ = wload(B_XKV + n)
                if n < 2:
                    for oc in range(4):
                        pb, pr = bank()
                        for kc in range(8):
                            mm(pb[:, 0:MEM], wt_[:, kc, oc * 128:(oc + 1) * 128], memT[:, kc, :], kc == 0, kc == 7,
                               [wr_, "t32a", "t32b"], [pr])
                        cp("act", KmT[:, n * 4 + oc, :], pb[:, 0:MEM], [pr], ["KmT"])
                else:
                    for mt in range(2):
                        pb, pr = bank()
                        for kc in range(8):
                            mm(pb, memT[:, kc, mt * 128:(mt + 1) * 128], wt_[:, kc, :], kc == 0, kc == 7, [wr_, "t32a", "t32b"], [pr])
                        cp("act", Vm[:, mt, (n - 2) * 512:(n - 1) * 512], pb, [pr], ["Vm"])

        for st in range(NST):
            t0 = st * ST
            first = (st == 0)
            for tti in range(4):
                dma("act", xsb[:, tti, :], dx[b, t0 + tti * 128:t0 + (tti + 1) * 128, :], (), [("xsb", tti)])
            if stage >= 3:
                convert_moe_weights()
            for kc in range(8):
                pb, pr = bank()
                for tti in range(4):
                    tr(pb[:, tti * 128:(tti + 1) * 128], xsb[:, tti, kc * 128:(kc + 1) * 128], C("ident"),
                       [("xsb", tti), "cst"], [pr])
                cp("act" if kc % 2 else "dve", xT[:, kc, :], pb, [pr], [RxT])
            dma("sp", posi, dpos[b, t0:t0 + ST].partition_broadcast(128), (), ["rden"])
            cp("dve", t32a, posi, ["rden"], ["t32a"])
            ts("dve", t32a, t32a, invf, None, ALU.mult, None, ["t32a", "pcol"], ["t32a"])
            for which, dst, dres in ((0, sinT, "sinT"), (1, cosT, "cosT")):
                if which == 1:
                    ts("dve", t32a, t32a, math.pi / 2, None, ALU.add, None, ["t32a"], ["t32a"])
                ts("dve", posi, t32a, 1.0 / (2 * math.pi), None, ALU.mult, None, ["t32a"], ["rden"])
                cp("dve", t32b, posi, ["rden"], ["t32b"])
                stt(t32c, t32b, -6.28125, t32a, ALU.mult, ALU.add, ["t32b", "t32a"], ["t32c"])
                stt(t32c, t32b, -(2 * math.pi - 6.28125), t32c, ALU.mult, ALU.add, ["t32b", "t32c"], ["t32c"])
                ts("dve", t32b, t32c, math.pi, None, ALU.is_gt, None, ["t32c"], ["t32b"])
                stt(t32c, t32b, -2 * math.pi, t32c, ALU.mult, ALU.add, ["t32b", "t32c"], ["t32c"])
                ts("dve", t32b, t32c, -math.pi, None, ALU.is_lt, None, ["t32c"], ["t32b"])
                stt(t32c, t32b, 2 * math.pi, t32c, ALU.mult, ALU.add, ["t32b", "t32c"], ["t32c"])
                ts("dve", t32c, t32c, math.pi, -math.pi, ALU.min, ALU.max, ["t32c"], ["t32c"])
                act(dst, t32c, AF.Sin, ["t32c"], [dres])

            def fm_group(wt_, wr_, j, M=128):
                pb, pr = bank()
                for kc in range(8):
                    mm(pb[0:M, :], wt_[:, kc, j * 128:j * 128 + M], xT[:, kc, :], kc == 0, kc == 7, [wr_, RxT], [pr])
                return pb, pr

            def rope_to(pb, pr, dst, dres):
                cp("act", tb16, pb, [pr], ["tb16"])
                p2, pr2 = bank()
                mm(p2, rmb, tb16, True, True, ["rmb", "tb16"], [pr2])
                tt("pool", t32a, tb16, cosT, ALU.mult, ["tb16", "cosT"], ["t32a"])
                tt("dve", t32b, p2, sinT, ALU.mult, [pr2, "sinT"], ["t32b"])
                tt("dve", dst, t32a, t32b, ALU.add, ["t32a", "t32b"], [dres])

            for g in range(2):
                wt_, wr_ = wload(g)
                for c in range(4):
                    pb, pr = fm_group(wt_, wr_, c)
                    rope_to(pb, pr, qT[:, g * 4 + c, :], RqT)
            wt_, wr_ = wload(2)
            for g in range(2):
                pb, pr = fm_group(wt_, wr_, g)
                rope_to(pb, pr, kdT[:, g, 1:5, :].rearrange("p a b -> p (a b)"), "kdT")
            pb, pr = fm_group(wt_, wr_, 2, M=4)
            act(rowi, pb[0:4, :], AF.Identity, [pr, "pcol"], ["rowi"], bias=pcol[0:4, 49:50])
            pb, pr = fm_group(wt_, wr_, 3, M=4)
            act(rowsp, pb[0:4, :], AF.Identity, [pr, "pcol"], ["rowsp"], bias=pcol[0:4, 50:51])
            act(rowsp, rowsp, AF.Exp, ["rowsp"], ["rowsp"], scale=-1.0)
            act(rowsp, rowsp, AF.Ln, ["rowsp"], ["rowsp"], bias=1.0)
            sch.op("dve", lambda e: e.tensor_tensor_scan(rowF, ones4, rowsp, carry[:, 0:1], ALU.mult, ALU.subtract),
                   ["ones4", "rowsp", "carry"], ["rowF"])
            tt("dve", rowa, rowi, rowF, ALU.subtract, ["rowi", "rowF"], ["rowi"])
            sch.op("dve", lambda e: e.tensor_tensor_scan(rowG, rowa, rowa, carry[:, 1:2], ALU.max, ALU.max),
                   ["rowi", "carry"], ["rowG"])
            cp("dve", carry[:, 0:1], rowF[:, ST - 1:ST], ["rowF"], ["carry"])
            cp("dve", carry[:, 1:2], rowG[:, ST - 1:ST], ["rowG"], ["carry"])
            stt(rowm, rowF, -1.0, rowG, ALU.mult, ALU.subtract, ["rowF", "rowG"], ["rowsp"])
            ts("dve", rowa, rowa, -0.5 * math.log(128.0), None, ALU.add, None, ["rowi"], ["rowi"])
            for which in range(2):
                wt_, wr_ = wload(3 + which)
                for h in range(4):
                    pb, pr = fm_group(wt_, wr_, h)
                    cp("act", pre[:, which * 4 + h, 3:3 + ST], pb, [pr], ["pre"])
            for which, dstt, dres in ((0, sga, Rsga), (1, sgm, Rsgm)):
                for n in range(2):
                    wt_, wr_ = wload(5 + which * 2 + n)
                    for c in range(4):
                        pb, pr = fm_group(wt_, wr_, c)
                        act(dstt[:, n * 4 + c, :], pb, AF.Sigmoid, [pr], [dres])
            for n in range(2):
                wt_, wr_ = wload(9 + n)
                for tti in range(4):
                    pb, pr = bank()
                    for kc in range(8):
                        mm(pb, xT[:, kc, tti * 128:(tti + 1) * 128], wt_[:, kc, :], kc == 0, kc == 7, [wr_, RxT], [pr])
                    cp("act", vext[:, tti, 2 * n:2 * n + 2, 0:256], pb.rearrange("p (a b) -> p a b", a=2), [pr], ["vext"])
            for n in range(2):
                wt_, wr_ = wload(11 + n)
                for tti in range(4):
                    pb, pr = bank()
                    for kc in range(8):
                        mm(pb, xT[:, kc, tti * 128:(tti + 1) * 128], wt_[:, kc, :], kc == 0, kc == 7, [wr_, RxT], [pr])
                    act(sigmo[:, tti, n * 512:(n + 1) * 512], pb, AF.Sigmoid, [pr], ["sigmo"])
            wt_, wr_ = wload(13)
            for tti in range(4):
                pb, pr = bank()
                for kc in range(8):
                    mm(pb[:, 0:128], xT[:, kc, tti * 128:(tti + 1) * 128], wt_[:, kc, 0:128], kc == 0, kc == 7, [wr_, RxT], [pr])
                cp("act", vatt[:, 1 + tti, :], pb[:, 0:128], [pr], ["vatt"])

            def att_A(u_):
                tti, g = u_ // 2, u_ % 2
                js = [1] if (first and tti == 0) else [0, 1]
                PQ = {}
                for j in js:
                    slot = tti + j
                    for half in range(2):
                        q_ = (4 * u_ + 2 * j + half) % 8
                        PQ[(j, half)] = (PT6[:, q_, :], ("PTq", q_))
                        pt_, ptr_ = PQ[(j, half)]
                        pb, pr = bank()
                        mm(pb, kdT[half * 64:(half + 1) * 64, g, slot, :],
                           qT[half * 64:(half + 1) * 64, g * 4:(g + 1) * 4, tti * 128:(tti + 1) * 128],
                           True, True, ["kdT", RqT], [pr])
                        act(pt_, pb, AF.Exp, [pr], [ptr_], scale=0.125)
                        tt("pool", pt_, pt_, (mprevb if j == 0 else mcurb), ALU.mult, [ptr_, "mprevb", "mcurb"], [ptr_])
                return js, PQ

            def att_B(u_, js, PQ):
                tti, g = u_ // 2, u_ % 2
                po, pro = bank()
                pd, prd = bank()
                for half in range(2):
                    hs = slice(half * 64, (half + 1) * 64)
                    for ji, j in enumerate(js):
                        pt_, ptr_ = PQ[(j, half)]
                        mm(po[hs, :], vatt[:, tti + j, g * 64:(g + 1) * 64], pt_, ji == 0, ji == len(js) - 1, ["vatt", ptr_], [pro])
                    for ji, j in enumerate(js):
                        pt_, ptr_ = PQ[(j, half)]
                        mm(pd[hs, :], onesb[:, 0:64], pt_, ji == 0, False, ["onesb", ptr_], [prd])
                    mm(pd[hs, :], ones2, esk[:, g * 2 + half, :], False, True, ["ones2", "esk"], [prd])
                rd_ = rden if u_ % 2 == 0 else t32c
                rr_ = "rden" if u_ % 2 == 0 else "t32c"
                act(rd_, pd, AF.Ln, [prd], [rr_])
                act(rd_, rd_, AF.Exp, [rr_], [rr_], scale=-1.0)
                tt("dve", attT[:, g * 4:(g + 1) * 4, tti * 128:(tti + 1) * 128],
                   po.rearrange("p (a b) -> p a b", a=4), rd_.rearrange("p (a b) -> p a b", a=4), ALU.mult,
                   [pro, rr_], [RattT])

            pend_ = att_A(0)
            for u_ in range(8):
                nxt_ = att_A(u_ + 1) if u_ + 1 < 8 else None
                att_B(u_, *pend_)
                pend_ = nxt_
            cp("pool", kdT[:, :, 0, :], kdT[:, :, 4, :], ["kdT"], ["kdT"])
            cp("pool", vatt[:, 0, :], vatt[:, 4, :], ["vatt"], ["vatt"])

            pb, pr = bank()
            for c in range(4):
                tr(pb[:, c * 4:(c + 1) * 4], rowa[:, c * 128:(c + 1) * 128], cst[0:4, io:io + 4], ["rowi", "cst"], [pr])
                tr(pb[:, 16 + c * 4:16 + (c + 1) * 4], rowm[:, c * 128:(c + 1) * 128], cst[0:4, io:io + 4], ["rowsp", "cst"], [pr])
            cp("dve", aT.rearrange("p a b -> p (a b)"), pb[:, 0:16], [pr], ["aT"])
            act(emt.rearrange("p a b -> p (a b)"), pb[:, 16:32], AF.Exp, [pr], ["emt"])
            for h in range(4):
                pb, pr = bank()
                mm(pb, cst[0:4, so + h * 128:so + (h + 1) * 128], rowG, True, True, ["cst", "rowG"], [pr])
                cp("act", Gb[:, h, :], pb, [pr], ["Gb"] + (ROUTE_RES if h == 0 else []))
            for ch in range(8):
                tc_, tr_ = (t32a, "t32a") if ch % 2 == 0 else (t32b, "t32b")
                ts("dve", tc_, pre[:, ch, 3:3 + ST], cwt(ch, 3), cb[:, ch:ch + 1], ALU.mult, ALU.add, ["pre", "pcol"], [tr_])
                for j in range(3):
                    stt(tc_, pre[:, ch, j:j + ST], cwt(ch, j), tc_, ALU.mult, ALU.add, ["pre", "pcol", tr_], [tr_])
                act(qkc[:, ch, :], tc_, AF.Silu, [tr_], [Rqkc])
            cp("pool", pre[:, :, 0:3], pre[:, :, ST:ST + 3], ["pre"], ["pre"])
            for n in range(2):
                wt_, wr_ = wload(B_ATT + n)
                for oc in range(4):
                    pb, pr = bank()
                    for c in range(8):
                        mm(pb, wt_[:, c, oc * 128:(oc + 1) * 128], attT[:, c, :], c == 0, c == 7, [wr_, RattT], [pr])
                    tt("dve", sga[:, n * 4 + oc, :], pb, sga[:, n * 4 + oc, :], ALU.mult, [pr, Rsga], [Rsga])
            H4 = range(4)

            def fbank(i):
                return ps_t[:, i, :], ("ps", i)

            def ml_P(c):
                cs = slice(c * 128, (c + 1) * 128)
                gps = [gprev[:, h:h + 1] if c == 0 else Gb[:, h, c * 128 - 1:c * 128] for h in H4]
                gpr = "gprev" if c == 0 else "Gb"
                gcs = [Gb[:, h, c * 128 + 127:c * 128 + 128] for h in H4]
                for h in H4:
                    tt("pool", Gm4[:, h, :], Gb[:, h, cs], C("bigm"), ALU.add, ["Gb", "cst"], [("Gm", h)])
                for h in H4:
                    act(Wt4[:, h, :], Gm4[:, h, :], AF.Exp, [("Gm", h), "aT"], [("Wt", h)], bias=aT[:, c, h:h + 1], scale=-1.0)
                pS, prS = fbank(4)
                for h in H4:
                    mm(pS[:, h * 128:(h + 1) * 128], qkc[:, 4 + h, cs], qkc[:, h, cs], True, True, [Rqkc], [prS])
                tt("dve", PTm4.rearrange("p a b -> p (a b)"), pS, Wt4.rearrange("p a b -> p (a b)"), ALU.mult,
                   [prS] + [("Wt", h) for h in H4], ["PTm4"])
                for h in H4:
                    act(wint4[:, h, :], Gb[:, h, cs], AF.Exp, ["Gb", gpr], [("wint", h)], bias=gps[h], scale=-1.0)
                tt("dve", qtil4, qkc[:, 0:4, cs], wint4, ALU.mult, [Rqkc] + [("wint", h) for h in H4], ["qtil4"])
                gp4 = gprev if c == 0 else Gb[:, :, c * 128 - 1]
                gc4 = Gb[:, :, c * 128 + 127]
                tt("pool", sm4[:, 8, :], gc4, aT[:, c, :], ALU.subtract, ["Gb", "aT"], ["sm89"])
                tt("pool", sm4[:, 9, :], gc4, gp4, ALU.subtract, ["Gb", gpr, "sm89"], ["sm89"])
                act(sm4[:, 8:10, :].rearrange("p a b -> p (a b)"), sm4[:, 8:10, :].rearrange("p a b -> p (a b)"), AF.Exp,
                    ["sm89"], ["sm89"], scale=-1.0)
                pK, prK = fbank(4)
                pKb = pK.bitcast(BF16)
                for h in H4:
                    tr(pKb[:, h * 128:(h + 1) * 128], qkc[:, 4 + h, cs], identb, [Rqkc, "identb"], [prK])
                tt("dve", ktil4, pKb[:, 0:512].rearrange("p (a b) -> p a b", a=4), sm4[:, 8, :].unsqueeze(2).to_broadcast([128, 4, 128]),
                   ALU.mult, [prK, "sm89"], ["ktil4"])
                if c > 0:
                    cp("pool", Sbf[:, :, 0:257], S32[:, :, 0:257], ["S32"], ["Sbf"])
                for h in H4:
                    pD, prD = fbank(5 if h % 2 == 0 else 7)
                    mm(pD[:, 0:257], ktil4[:, h, :], vext[:, c, h, 0:257], True, True, ["ktil4", "vext"], [prD])
                    stt(S32[:, h, 0:257], S32[:, h, 0:257], sm4[:, 9, h:h + 1], pD[:, 0:257], ALU.mult, ALU.add,
                        ["S32", "sm89", prD], ["S32"])

            def ml_Q(c):
                pNs = []
                for h in H4:
                    pN, prN = fbank(h)
                    pNs.append((pN, prN))
                    mm(pN[:, 0:257], qtil4[:, h, :], Sbf[:, h, 0:257], True, False, ["qtil4", "Sbf"], [prN])
                    mm(pN[:, 0:257], PTm4[:, h, :], vext[:, c, h, 0:257], False, True, ["PTm4", "vext"], [prN])
                return pNs

            def ml_R(c, pNs):
                cs = slice(c * 128, (c + 1) * 128)
                act(sm4[:, 0, :], ps_t[:, 0:4, 256], AF.Abs, [pNs[h][1] for h in H4], ["sm0"])
                tt("dve", sm4[:, 0, :], sm4[:, 0, :], emt[:, c, :], ALU.max, ["sm0", "emt"], ["sm0m"])
                recip(sm4[:, 1, :], sm4[:, 0, :], ["sm0m"], ["sm1"])
                for h in H4:
                    act(sqj, pNs[h][0][:, 0:256], AF.Square, [pNs[h][1], "sm1"], ["rden", ("sm2", h)], scale=sm4[:, 1, h:h + 1],
                        accum_out=sm4[:, 2, h:h + 1])
                ts("dve", sm4[:, 3, :], sm4[:, 2, :], 1.0 / 256.0, RMS_EPS, ALU.mult, ALU.add, [("sm2", h) for h in H4], ["sm3"])
                act(sm4[:, 4, :], sm4[:, 3, :], AF.Sqrt, ["sm3"], ["sm4"])
                recip(sm4[:, 5, :], sm4[:, 4, :], ["sm4"], ["sm5"])
                tt("dve", sm4[:, 6, :], sm4[:, 5, :], sm4[:, 1, :], ALU.mult, ["sm5", "sm1"], ["sm6"])
                for h in H4:
                    stt(hmtok4[:, h, :], pNs[h][0][:, 0:256], sm4[:, 6, h:h + 1], sigmo[:, c, h * 256:(h + 1) * 256], ALU.mult, ALU.mult,
                        [pNs[h][1], "sm6", "sigmo"], [("hmtok", h)])
                pT_, prT = fbank(6)
                pTb = pT_.bitcast(BF16)
                for h in H4:
                    for hf in range(2):
                        tr(pTb[:, (2 * h + hf) * 128:(2 * h + hf + 1) * 128], hmtok4[:, h, hf * 128:(hf + 1) * 128], identb,
                           [("hmtok", h), "identb"], [prT])
                cp("act", hmT[:, :, cs], pTb.rearrange("p (a b) -> p a b", a=8), [prT], [RhmT])

            ml_P(0)
            for c in range(4):
                pNs_ = ml_Q(c)
                if c + 1 < 4:
                    ml_P(c + 1)
                else:
                    cp("pool", Sbf[:, :, 0:257], S32[:, :, 0:257], ["S32"], ["Sbf"])
                ml_R(c, pNs_)
            for h in range(4):
                cp("pool", gprev[:, h:h + 1], Gb[:, h, ST - 1:ST], ["Gb"], ["gprev"])

            for n in range(2):
                wt_, wr_ = wload(B_ML + n)
                for oc in range(4):
                    pb, pr = bank()
                    for c in range(8):
                        mm(pb, wt_[:, c, oc * 128:(oc + 1) * 128], hmT[:, c, :], c == 0, c == 7, [wr_, RhmT], [pr])
                    tt("dve", t32c, pb, sgm[:, n * 4 + oc, :], ALU.mult, [pr, Rsgm], ["t32c"])
                    tt("dve", yT[:, n * 4 + oc, :], t32c, sga[:, n * 4 + oc, :], ALU.add, ["t32c", Rsga], [RyT])
            wmix = [wload(B_MIX + n) for n in range(2)]
            for tti in range(4):
                for n in range(2):
                    wt_, wr_ = wmix[n]
                    pb, pr = bank()
                    for c in range(8):
                        mm(pb, yT[:, c, tti * 128:(tti + 1) * 128], wt_[:, c, :], c == 0, c == 7, [wr_, RyT], [pr])
                    stt(xsb[:, tti, n * 512:(n + 1) * 512], xsb[:, tti, n * 512:(n + 1) * 512], ALPHA, pb, ALU.mult, ALU.add,
                        [("xsb", tti), pr], [("xsb", tti)])
                layer_norm(tti, 0)

            if stage >= 2:
                for tti in range(4):
                    for hb in range(2):
                        pb, pr = bank()
                        for k4 in range(4):
                            kc = hb * 4 + k4
                            tr(pb[:, k4 * 128:(k4 + 1) * 128], xsb[:, tti, kc * 128:(kc + 1) * 128], C("ident"),
                               [("xsb", tti), "cst"], [pr])
                        cp("act" if hb else "dve", h1T[:, hb * 4:(hb + 1) * 4, tti * 128:(tti + 1) * 128],
                           pb.rearrange("p (a b) -> p a b", a=4), [pr], [Rh1T])
                for n in range(2):
                    wt_, wr_ = wload(B_XQ + n)
                    for oc in range(4):
                        pb, pr = bank()
                        for c in range(8):
                            mm(pb, wt_[:, c, oc * 128:(oc + 1) * 128], h1T[:, c, :], c == 0, c == 7, [wr_, Rh1T], [pr])
                        cp("act", qxT[:, n * 4 + oc, :], pb, [pr], [RqxT])
                PXs = [PxT, PxT2]
                RDs = [rden, rden2]

                def xa_A(hd):
                    P_ = PXs[hd % 2]
                    for mt in range(2):
                        pb, pr = bank()
                        for dc in range(2):
                            mm(pb, KmT[:, 2 * hd + dc, mt * 128:(mt + 1) * 128], qxT[:, 2 * hd + dc, :], dc == 0, dc == 1,
                               ["KmT", RqxT], [pr])
                        act(P_[:, mt, :], pb, AF.Exp, [pr], [("PxT", hd % 2, mt)], scale=1.0 / 16.0)

                def xa_B(hd):
                    P_ = PXs[hd % 2]
                    rd = RDs[hd % 2]
                    rr = "t32c" if hd % 2 else "rden"
                    pd, prd = bank()
                    for mt in range(2):
                        mm(pd, onesb, P_[:, mt, :], mt == 0, mt == 1, ["onesb", ("PxT", hd % 2, mt)], [prd])
                    act(rd, pd, AF.Ln, [prd], [rr])
                    act(rd, rd, AF.Exp, [rr], [rr], scale=-1.0)
                    for dc in range(2):
                        po, pro = bank()
                        for mt in range(2):
                            mm(po, Vm[:, mt, (2 * hd + dc) * 128:(2 * hd + dc + 1) * 128], P_[:, mt, :], mt == 0, mt == 1,
                               ["Vm", ("PxT", hd % 2, mt)], [pro])
                        tt("dve", oxT[:, 2 * hd + dc, :], po, rd, ALU.mult, [pro, rr], [RoxT])

                xa_A(0)
                for hd in range(4):
                    if hd + 1 < 4:
                        xa_A(hd + 1)
                    xa_B(hd)
                wxo = [wload(B_XO + n) for n in range(2)]
                for tti in range(4):
                    for n in range(2):
                        wt_, wr_ = wxo[n]
                        pb, pr = bank()
                        for c in range(8):
                            mm(pb, oxT[:, c, tti * 128:(tti + 1) * 128], wt_[:, c, :], c == 0, c == 7, [wr_, RoxT], [pr])
                        stt(xsb[:, tti, n * 512:(n + 1) * 512], xsb[:, tti, n * 512:(n + 1) * 512], ALPHA, pb, ALU.mult, ALU.add,
                            [("xsb", tti), pr], [("xsb", tti)])
                    layer_norm(tti, 1)

            if stage < 3:
                for tti in range(4):
                    tk = dma("pool", dout[b, t0 + tti * 128:t0 + (tti + 1) * 128, :], xsb[:, tti, :], [("xsb", tti)], ["dout"])
                    final_tokens.append(tk)
            else:
                T4 = slice(tile_no, tile_no + 4)
                for tti in range(4):
                    tno = tile_no + tti
                    res = ("xsb", tti)
                    h2T = h2Ts[tti % 2]; hres = H2R[tti % 2]
                    dma("pool", H32[tno * 128:(tno + 1) * 128, :], xsb[:, tti, :], [res], ["H32"])
                    for hb in range(2):
                        pb, pr = bank()
                        for k4 in range(4):
                            kc = hb * 4 + k4
                            tr(pb[:, k4 * 128:(k4 + 1) * 128], xsb[:, tti, kc * 128:(kc + 1) * 128], C("ident"), [res, "cst"], [pr])
                        cp("act", h2T[:, hb * 4:(hb + 1) * 4, :], pb.rearrange("p (a b) -> p a b", a=4), [pr], hres)
                    pb, pr = bank()
                    for kc in range(8):
                        mm(pb[:, 0:72], h2T[:, kc, :], wrs[:, kc, :], kc == 0, kc == 7, hres + ["wrs"], [pr])
                    tt("dve", lg4[:, tti, :], pb[:, 0:72], smb[:, 24:96], ALU.add, [pr, "smb"], ["lg4"])
                G8 = lg4[:, :, 0:8]
                red(rq[:, 0, :], G8, ALU.max, ["lg4"], ["rq0"])
                tt("dve", d4, G8, rq[:, 0, :].unsqueeze(2).to_broadcast([128, 4, 8]), ALU.subtract, ["lg4", "rq0"], ["d4"])
                ts("dve", pen4, d4, 0.0, None, ALU.is_equal, None, ["d4"], ["pen4"])
                ts("dve", pen4, pen4, 1e30, -1e30, ALU.mult, ALU.add, ["pen4"], ["pen4"])
                act(d4, d4, AF.Exp, ["d4"], ["d4"])
                red(rq[:, 1, :], d4, ALU.add, ["d4"], ["rq1"])
                recip(rq[:, 2, :], rq[:, 1, :], ["rq1"], ["rq2"])
                tt("dve", lem4.rearrange("p t (g e) -> p t g e", g=8), lg4[:, :, 8:72].rearrange("p t (g e) -> p t g e", g=8),
                   pen4.unsqueeze(3).to_broadcast([128, 4, 8, 8]), ALU.add, ["lg4", "pen4"], ["lem4"])
                for tti in range(4):
                    sch.op("dve", (lambda e, tti=tti: e.max(top84[:, tti, :], lem4[:, tti, :])), ["lem4"], ["top84"])
                for s_ in range(2):
                    tt("dve", oh4[:, :, s_, :], lem4, top84[:, :, s_:s_ + 1].to_broadcast([128, 4, 64]), ALU.is_equal,
                       ["lem4", "top84"], [("oh4", s_)])
                    tt("dve", j4, oh4[:, :, s_, :], C("iota64").unsqueeze(1).to_broadcast([128, 4, 64]), ALU.mult,
                       [("oh4", s_), "cst"], ["j4"])
                    red(RW[:, T4, 2 + s_], j4, ALU.add, ["j4"], ["RW"])
                tt("dve", rq[:, 3, :], top84[:, :, 1], top84[:, :, 0], ALU.subtract, ["top84"], ["rq3"])
                act(rq[:, 3, :], rq[:, 3, :], AF.Exp, ["rq3"], ["rq3"])
                ts("dve", rq[:, 3, :], rq[:, 3, :], 1.0, None, ALU.add, None, ["rq3"], ["rq3"])
                recip(rq[:, 4, :], rq[:, 3, :], ["rq3"], ["rq4"])
                tt("dve", RW[:, T4, 0], rq[:, 4, :], rq[:, 2, :], ALU.mult, ["rq4", "rq2"], ["RW"])
                tt("dve", RW[:, T4, 1], rq[:, 2, :], RW[:, T4, 0], ALU.subtract, ["rq2", "RW"], ["RW"])
                tt("dve", ohb4, oh4[:, :, 0, :], oh4[:, :, 1, :], ALU.add, [("oh4", 0), ("oh4", 1)], ["ohb4"])
                pR, prR = bank()
                for tti in range(4):
                    o_ = pR[:, tti * 64:(tti + 1) * 64]
                    mm(o_, onesb, ohcum, True, False, ["onesb", "ohcum"], [prR])
                    for t2 in range(tti):
                        mm(o_, onesb, ohb4[:, t2, :], False, False, ["onesb", "ohb4"], [prR])
                    mm(o_, ustrb, ohb4[:, tti, :], False, True, ["ustrb", "ohb4"], [prR])
                for s_ in range(2):
                    tt("dve", j4, pR[:, 0:256].rearrange("p (a b) -> p a b", a=4), oh4[:, :, s_, :], ALU.mult, [prR, ("oh4", s_)], ["j4"])
                    red(RW[:, T4, 4 + s_], j4, ALU.add, ["j4"], ["RW"])
                red(ohs, ohb4.rearrange("p t e -> p e t"), ALU.add, ["ohb4"], ["ohs"])
                tt("dve", ohcum, ohcum, ohs, ALU.add, ["ohcum", "ohs"], ["ohcum"])
            tile_no += 4

    if stage >= 3:
        sch.fence()
        ptr[0] = p1_base
        cnt = carve([128, 64]); cnti = carve([128, 64], I32); pend = carve([128, 64]); pstart = carve([128, 64])
        bef = carve([128, 256]); bei = carve([128, 256], I32)
        p2_mark = ptr[0]
        NWG, NWD, DEP = 4, 6, 3
        wg16 = [carve([128, 8, 512], BF16) for i in range(NWG)]
        wd16 = [carve([128, 2, D], BF16) for i in range(NWD)]
        xblk = [carve([128, D], BF16) for i in range(DEP)]
        xTb = [carve([128, 8, 128], BF16) for i in range(DEP)]
        sgl = [carve([128, 256]) for i in range(DEP)]
        actb = [carve([128, 256], BF16) for i in range(DEP)]
        actT = [carve([128, 2, 128], BF16) for i in range(DEP)]
        ysb = [carve([128, D]) for i in range(DEP)]
        x16s = [carve([128, D], BF16) for i in range(4)]
        hls = [carve([128, D]) for i in range(4)]
        pb, pr = bank()
        mm(pb[:, 0:64], onesb, ohcum, True, True, ["onesb", "ohcum"], [pr])
        cp("dve", cnt, pb[:, 0:64], [pr], ["cnt"])
        ts("dve", cnti, cnt, 127.0, None, ALU.add, None, ["cnt"], ["cnti"])
        ts("dve", cnti, cnti, 7, None, ALU.arith_shift_right, None, ["cnti"], ["cnti"])
        ts("dve", cnti, cnti, 7, None, ALU.logical_shift_left, None, ["cnti"], ["cnti"])
        cp("dve", cnt, cnti, ["cnti"], ["cnt"])
        sch.op("dve", lambda e: e.tensor_tensor_scan(pend, C("ones")[:, 0:64], cnt, 0.0, ALU.mult, ALU.add),
               ["cst", "cnt"], ["pend"])
        tt("dve", pstart, pend, cnt, ALU.subtract, ["pend", "cnt"], ["pstart"])
        ohall = carve([128, NTT, 64])
        posf = carve([128, NTT])
        for s_ in range(2):
            tt("dve", ohall, C("iota64").unsqueeze(1).to_broadcast([128, NTT, 64]),
               RW[:, :, 2 + s_:3 + s_].to_broadcast([128, NTT, 64]), ALU.is_equal, ["cst", "RW"], ["ohall"])
            tt("dve", ohall, ohall, pstart.unsqueeze(1).to_broadcast([128, NTT, 64]), ALU.mult, ["ohall", "pstart"], ["ohall"])
            red(posf, ohall, ALU.add, ["ohall"], ["posf"])
            tt("dve", posf, posf, RW[:, :, 4 + s_], ALU.add, ["posf", "RW"], ["posf"])
            cp("dve", posI[:, :, s_], posf, ["posf"], ["posI"])
        memset("dve", bef, 0.0, ["bef"])
        for e_ in range(NEXP):
            stt(bef, C("blk128"), pend[:, e_:e_ + 1], bef, ALU.is_ge, ALU.add, ["cst", "pend", "bef"], ["bef"])
        ts("dve", bef, bef, 128.0, None, ALU.mult, None, ["bef"], ["bef"])
        ts("dve", bef, bef, C("pidx")[:, 0:1], None, ALU.add, None, ["bef", "cst"], ["bef"])
        cp("dve", bei, bef, ["bef"], ["bei"])
        for tno in range(NTT):
            hl = hls[tno % 4]; x16 = x16s[tno % 4]
            dma("sp", hl, H32[tno * 128:(tno + 1) * 128, :], ["H32"], [("hl", tno % 4)])
            cp("act", x16, hl, [("hl", tno % 4)], [("x16", tno % 4)])
            for s_ in range(2):
                sch.dma("pool", (lambda e, tno=tno, s_=s_, x16=x16: e.indirect_dma_start(
                    out=XS, out_offset=bass.IndirectOffsetOnAxis(ap=posI[:, tno, s_:s_ + 1], axis=0),
                    in_=x16, in_offset=None)), [("x16", tno % 4), "posI"], ["XS"])
        sch.fence()
        bc_ = {}

        def bcreg(e):
            if "r" not in bc_:
                bc_["r"] = e.alloc_register("bcreg")
                e.reg_mov(bc_["r"], NEXP * 128 - 1)
            return bc_["r"]

        def moe_A(blk):
            g_, d_, p_ = blk % NWG, blk % NWD, blk % DEP
            sch.dma("pool", (lambda e: e.indirect_dma_start(
                out=wg16[g_].rearrange("p a b -> p (a b)"), out_offset=None, in_=WGB,
                in_offset=bass.IndirectOffsetOnAxis(ap=bei[:, blk:blk + 1], axis=0),
                bounds_check=bcreg(e), oob_is_err=False)), ["bei", "WGB"], [("wg16", g_)])
            sch.dma("pool", (lambda e: e.indirect_dma_start(
                out=wd16[d_].rearrange("p a b -> p (a b)"), out_offset=None, in_=WDB,
                in_offset=bass.IndirectOffsetOnAxis(ap=bei[:, blk:blk + 1], axis=0),
                bounds_check=bcreg(e), oob_is_err=False)), ["bei", "WDB"], [("wd16", d_)])
            dma("act", xblk[p_], XS[blk * 128:(blk + 1) * 128, :], ["XS"], [("xblk", p_)])
            pb, pr = bank()
            pbb = pb.bitcast(BF16)
            for kc in range(8):
                tr(pbb[:, kc * 128:(kc + 1) * 128], xblk[p_][:, kc * 128:(kc + 1) * 128], identb, [("xblk", p_), "identb"], [pr])
            cp("dve", xTb[p_].rearrange("p a b -> p (a b)"), pbb, [pr], [("xTb", p_)])

        def moe_B(blk):
            g_, p_ = blk % NWG, blk % DEP
            pH, prH = bank()
            for kc in range(8):
                mm(pH, xTb[p_][:, kc, :], wg16[g_][:, kc, :], kc == 0, kc == 7, [("xTb", p_), ("wg16", g_)], [prH])
            act(sgl[p_], pH[:, 0:256], AF.Silu, [prH], [("sgl", p_)])
            tt("dve", actb[p_], pH[:, 256:512], sgl[p_], ALU.mult, [prH, ("sgl", p_)], [("actb", p_)])

        def moe_C(blk):
            p_ = blk % DEP
            pb, pr = bank()
            pbb = pb.bitcast(BF16)
            for fc in range(2):
                tr(pbb[:, fc * 128:(fc + 1) * 128], actb[p_][:, fc * 128:(fc + 1) * 128], identb, [("actb", p_), "identb"], [pr])
            cp("dve", actT[p_].rearrange("p a b -> p (a b)"), pbb[:, 0:256], [pr], [("actT", p_)])

        def moe_D(blk):
            d_, p_ = blk % NWD, blk % DEP
            for n in range(2):
                pY, prY = bank()
                for fc in range(2):
                    mm(pY, actT[p_][:, fc, :], wd16[d_][:, fc, n * 512:(n + 1) * 512], fc == 0, fc == 1,
                       [("actT", p_), ("wd16", d_)], [prY])
                cp("act" if n == 0 else "dve", ysb[p_][:, n * 512:(n + 1) * 512], pY, [prY], [("ysb", p_)])
            dma("sp", YS[blk * 128:(blk + 1) * 128, :], ysb[p_], [("ysb", p_)], ["YS"])

        for step in range(NBLKM + 3):
            if step < NBLKM:
                moe_A(step)
            if 0 <= step - 1 < NBLKM:
                moe_B(step - 1)
            if 0 <= step - 2 < NBLKM:
                moe_C(step - 2)
            if 0 <= step - 3 < NBLKM:
                moe_D(step - 3)
        sch.fence()
        ptr[0] = p2_mark
        y0s = [carve([128, D]) for i in range(4)]; y1s = [carve([128, D]) for i in range(4)]
        hb3 = [carve([128, D]) for i in range(6)]
        for i in range(2):
            dma("sp", lnp[:, i, :], dvec[4 + i, :].partition_broadcast(128), (), ["lnp"])
        def p3_fetch(tno):
            k4 = tno % 4
            dma("act", hb3[tno % 6], H32[tno * 128:(tno + 1) * 128, :], ["H32"], [("hb3", tno % 6)])
            for s_, yy, yr in ((0, y0s[k4], ("y0", k4)), (1, y1s[k4], ("y1", k4))):
                sch.dma("pool", (lambda e, s_=s_, yy=yy: e.indirect_dma_start(
                    out=yy, out_offset=None, in_=YS,
                    in_offset=bass.IndirectOffsetOnAxis(ap=posI[:, tno, s_:s_ + 1], axis=0))), ["YS", "posI"], [yr])

        for t_ in range(min(3, NTT)):
            p3_fetch(t_)
        for tno in range(NTT):
            b_ = (tno * 128) // S
            tok0 = tno * 128 - b_ * S
            hx = hb3[tno % 6]
            res = ("hb3", tno % 6)
            if tno + 3 < NTT:
                p3_fetch(tno + 3)
            k4 = tno % 4
            y0 = y0s[k4]; y1 = y1s[k4]
            act(y0, y0, AF.Copy, [("y0", k4), "RW"], [("y0", k4)], scale=RW[:, tno, 0:1])
            stt(y0, y1, RW[:, tno, 1:2], y0, ALU.mult, ALU.add, [("y1", k4), "RW", ("y0", k4)], [("y0", k4)])
            stt(hx, hx, ALPHA, y0, ALU.mult, ALU.add, [res, ("y0", k4)], [res])
            layer_norm(tno % 4, 0, add_eng="dve", xr=hx, res=res)
            tk = dma("sp", dout[b_, tok0:tok0 + 128, :], hx, [res], ["dout"])
            final_tokens.append(tk)

    sch.emit(final_tokens)
    es.close()
    return nc


def make_in_maps(inp, S, NB, ncores):
    f = lambda a: np.ascontiguousarray(np.asarray(a, dtype=np.float32))
    wall = build_wall(f(inp["w_in"][0]), f(inp["w_att_branch"][0]), f(inp["w_ml_branch"][0]), f(inp["w_mix_out"][0]),
                      f(inp["w_xq"][0]), f(inp["w_xo"][0]), f(inp["w_xkv"][0]))
    vecs = np.stack([f(inp[k][0]) for k in ("ln1_g", "ln1_b", "ln2_g", "ln2_b", "ln3_g", "ln3_b")])
    pcol = np.zeros((128, 56), np.float32)
    pcol[:, 0:8] = f(inp["ml_norm_g"][0]).reshape(8, 128).T
    pcol[:, 8:16] = f(inp["conv_b"][0]).reshape(8, 128).T
    cwv = f(inp["conv_w"][0])
    for ch in range(8):
        for j in range(4):
            pcol[:, 16 + ch * 4 + j] = cwv[j, ch * 128:(ch + 1) * 128]
    pcol[:, 48] = (10000.0 ** (-(np.arange(128) % 32).astype(np.float32) / np.float32(32))).astype(np.float32)
    pcol[0:4, 49] = f(inp["b_igate"][0])
    pcol[0:4, 50] = f(inp["b_fgate"][0])
    smalls = np.zeros((128,), np.float32)
    smalls[0:16] = f(inp["attn_sinks"][0])
    smalls[24:32] = f(inp["b_router_group"][0])
    smalls[32:96] = f(inp["b_router_expert"][0])
    wr = np.concatenate([f(inp["w_router_group"][0]), f(inp["w_router_expert"][0])], axis=1)
    wr = np.ascontiguousarray(wr.reshape(8, 128, 72).transpose(1, 0, 2))
    wgu = np.concatenate([f(inp["w_gate"][0]), f(inp["w_up"][0])], axis=2)
    wgu = np.ascontiguousarray(wgu.reshape(NEXP, 8, 128, 512).transpose(0, 2, 1, 3)).reshape(NEXP * 128, 4096)
    wd = f(inp["w_down"][0])
    wd = np.ascontiguousarray(wd.reshape(NEXP, 2, 128, D).transpose(0, 2, 1, 3)).reshape(NEXP * 128, 2048)
    x = f(inp["x"]); mem = f(inp["mem"]); pos = np.ascontiguousarray(np.asarray(inp["positions"], dtype=np.int32))
    maps = []
    for c in range(ncores):
        maps.append({
            "x": x[c * NB:(c + 1) * NB], "mem": mem[c * NB:(c + 1) * NB], "pos": pos[c * NB:(c + 1) * NB],
            "wall": wall, "consts": _CONST_ARR, "vecs": vecs, "pcol": pcol, "smalls": smalls,
            "wr": wr, "wgu": wgu, "wd": wd,
        })
    return maps


_NC_CACHE = {}


def kernel(**inp):
    B, S = inp["x"].shape[0], inp["x"].shape[1]
    NB = B // NCORES
    key = (S, NB)
    if key not in _NC_CACHE:
        _NC_CACHE[key] = build(S, NB)
    nc = _NC_CACHE[key]
    maps = make_in_maps(inp, S, NB, NCORES)
    res = run_bass_kernel_spmd(nc, maps, core_ids=list(range(NCORES)))
    return np.concatenate([r["out"] for r in res.results], axis=0).astype(np.float32)
```

```python
import math
import contextlib
import numpy as np
import concourse.bass as bass
import concourse.mybir as mybir
from concourse.bass_utils import run_bass_kernel_spmd

F32 = mybir.dt.float32
BF16 = mybir.dt.bfloat16
I32 = mybir.dt.int32
ALU = mybir.AluOpType
AF = mybir.ActivationFunctionType
AX = mybir.AxisListType

D = 1024
NCORES = 8
ALPHA = 2.0 ** 0.25
LN_EPS = 1e-5
RMS_EPS = 1e-6
N_DMA_SEMS = 12
ST = 512
NEXP = 64
MEM = 256

O_AQ, O_AK, O_AV, O_MQ, O_MK, O_MV, O_MO, O_MI, O_MF, O_GA, O_GM = (
    0, 1024, 1152, 1280, 1792, 2304, 3328, 4352, 4356, 4360, 5384)


class Sched:
    COMPUTE = ("pe", "act", "dve", "pool")

    def __init__(self, nc, dma_queues=("sp", "act", "pool")):
        self.nc = nc
        self.ops = {e: [] for e in ("pe", "act", "dve", "pool", "sp")}
        self.last_w = {}
        self.readers = {}
        self.dma_queues = dma_queues
        self.dma_count = {q: 0 for q in dma_queues}
        self.fence_toks = []
        self.fence_pending = set()

    def fence(self):
        toks = []
        for e in self.COMPUTE:
            for i in range(len(self.ops[e]) - 1, -1, -1):
                if self.ops[e][i]["dma"] is None:
                    toks.append(("c", e, i))
                    break
        for q in self.dma_queues:
            n = self.dma_count[q]
            for k in range(max(0, n - N_DMA_SEMS), n):
                toks.append(("d", q, k))
        self.fence_toks = toks
        self.fence_pending = set(self.ops.keys())

    def _fence_deps(self, eng, deps):
        if eng in self.fence_pending:
            self.fence_pending.discard(eng)
            deps = list(deps) + list(self.fence_toks)
        return deps

    def _deps(self, reads, writes):
        deps = []
        for r in reads:
            t = self.last_w.get(r)
            if t is not None:
                deps.append(t)
        for w in writes:
            t = self.last_w.get(w)
            if t is not None:
                deps.append(t)
            deps.extend(self.readers.get(w, ()))
        return deps

    def _commit(self, tok, reads, writes):
        for r in reads:
            self.readers.setdefault(r, []).append(tok)
        for w in writes:
            self.last_w[w] = tok
            self.readers[w] = []

    def op(self, eng, fn, reads=(), writes=()):
        deps = self._fence_deps(eng, self._deps(reads, writes))
        idx = len(self.ops[eng])
        tok = ("c", eng, idx)
        if eng == "pe":
            deps = [d for d in deps if not (d[0] == "c" and d[1] == "pe")]
        self.ops[eng].append({"fn": fn, "deps": deps, "signal": False, "dma": None})
        self._commit(tok, reads, writes)
        return tok

    def dma(self, queue, fn, reads=(), writes=()):
        deps = self._fence_deps(queue, self._deps(reads, writes))
        n = self.dma_count[queue]
        self.dma_count[queue] += 1
        tok = ("d", queue, n)
        if n >= N_DMA_SEMS:
            deps.append(("d", queue, n - N_DMA_SEMS))
        self.ops[queue].append({"fn": fn, "deps": deps, "signal": False, "dma": n})
        self._commit(tok, reads, writes)
        return tok

    def emit(self, final_wait_tokens=()):
        nc = self.nc
        for e, lst in self.ops.items():
            for o in lst:
                for d in o["deps"]:
                    if d[0] == "c":
                        self.ops[d[1]][d[2]]["signal"] = True
        for d in final_wait_tokens:
            if d[0] == "c":
                self.ops[d[1]][d[2]]["signal"] = True
        semval = {}
        for e in self.COMPUTE:
            c = 0
            vals = []
            for o in self.ops[e]:
                if o["signal"] and o["dma"] is None:
                    c += 1
                vals.append(c)
            semval[e] = vals
        with contextlib.ExitStack() as st:
            csem = {e: st.enter_context(nc.semaphore("cs_" + e)) for e in self.COMPUTE}
            dsem = {q: [st.enter_context(nc.semaphore("ds_%s_%d" % (q, i))) for i in range(N_DMA_SEMS)]
                    for q in self.dma_queues}
            block = st.enter_context(nc.Block())

            def need(tok):
                if tok[0] == "c":
                    return ("c", tok[1]), csem[tok[1]], semval[tok[1]][tok[2]]
                q, n = tok[1], tok[2]
                return ("d", q, n % N_DMA_SEMS), dsem[q][n % N_DMA_SEMS], 16 * (n // N_DMA_SEMS + 1)

            def run(e, extra=()):
                def body(engine):
                    waited = {}
                    for o in self.ops[e]:
                        reqs = {}
                        for d in o["deps"]:
                            key, sem, val = need(d)
                            if waited.get(key, 0) >= val:
                                continue
                            if key not in reqs or reqs[key][1] < val:
                                reqs[key] = (sem, val)
                        for key, (sem, val) in reqs.items():
                            engine.wait_ge(sem, val)
                            waited[key] = val
                        inst = o["fn"](engine)
                        if o["dma"] is not None:
                            inst.then_inc(dsem[e][o["dma"] % N_DMA_SEMS], 16)
                        elif o["signal"]:
                            inst.then_inc(csem[e], 1)
                    for d in extra:
                        key, sem, val = need(d)
                        if waited.get(key, 0) < val:
                            engine.wait_ge(sem, val)
                            waited[key] = val
                return body

            block.sync(run("sp", final_wait_tokens))
            block.tensor(run("pe"))
            block.scalar(run("act"))
            block.vector(run("dve"))
            block.gpsimd(run("pool"))


def _tile_block(W, cols):
    cols = np.asarray(cols)
    blk = np.zeros((1024, 512), np.float32)
    ok = cols >= 0
    blk[:, ok] = W[:, cols[ok]]
    return blk.reshape(8, 128, 512).transpose(1, 0, 2)


def win_blocks():
    r = np.arange
    blocks = []
    for g in range(2):
        cols = []
        for c in range(4):
            h0, h1 = 8 * g + c, 8 * g + 4 + c
            cols += list(O_AQ + h0 * 64 + r(64)) + list(O_AQ + h1 * 64 + r(64))
        blocks.append(cols)
    cols = []
    for g in range(2):
        cols += list(O_AK + g * 64 + r(64)) * 2
    cols += list(O_MI + r(4)) + [-1] * 124
    cols += list(O_MF + r(4)) + [-1] * 124
    blocks.append(cols)
    blocks.append(list(O_MQ + r(512)))
    blocks.append(list(O_MK + r(512)))
    blocks.append(list(O_GA + r(512)))
    blocks.append(list(O_GA + 512 + r(512)))
    blocks.append(list(O_GM + r(512)))
    blocks.append(list(O_GM + 512 + r(512)))
    blocks.append(list(O_MV + r(512)))
    blocks.append(list(O_MV + 512 + r(512)))
    blocks.append(list(O_MO + r(512)))
    blocks.append(list(O_MO + 512 + r(512)))
    blocks.append(list(O_AV + r(128)) + [-1] * 384)
    return blocks


B_ATT, B_ML, B_MIX, B_XQ, B_XO, B_XKV = 14, 16, 18, 20, 22, 24
NBLK = 28


def att_row_perm():
    rows = []
    for g in range(2):
        for c in range(4):
            h0, h1 = 8 * g + c, 8 * g + 4 + c
            rows += list(h0 * 64 + np.arange(64)) + list(h1 * 64 + np.arange(64))
    return np.asarray(rows)


def build_wall(w_in, w_att, w_ml, w_mix, w_xq, w_xo, w_xkv):
    wall = np.empty((NBLK, 128, 8, 512), np.float32)
    for i, cols in enumerate(win_blocks()):
        wall[i] = _tile_block(w_in, cols)
    watt = w_att[att_row_perm(), :]
    k = 14
    for W in (watt, w_ml, w_mix, w_xq, w_xo):
        for n in range(2):
            wall[k] = _tile_block(W, list(n * 512 + np.arange(512)))
            k += 1
    for n in range(4):
        wall[k] = _tile_block(w_xkv, list(n * 512 + np.arange(512)))
        k += 1
    return wall.reshape(NBLK, 128, 4096)


def build_consts():
    c = {}
    c["ident"] = np.eye(128, dtype=np.float32)
    Rm = np.zeros((128, 128), np.float32)
    for hh in range(2):
        for d in range(32):
            Rm[hh * 64 + d + 32, hh * 64 + d] = -1.0
            Rm[hh * 64 + d, hh * 64 + d + 32] = 1.0
    c["rm"] = Rm
    k = np.arange(128)[:, None]
    q = np.arange(128)[None, :]
    c["mcur"] = np.tile((k <= q).astype(np.float32), (1, 4))
    c["mprev"] = np.tile((k > q).astype(np.float32), (1, 4))
    c["bigm"] = np.where(k > q, 1e30, 0.0).astype(np.float32)
    c["ustr"] = (k < q).astype(np.float32)
    c["ones"] = np.ones((128, 128), np.float32)
    sel = np.zeros((128, 512), np.float32)
    for h in range(4):
        sel[h, h * 128:(h + 1) * 128] = 1.0
    c["sel"] = sel
    c["iota64"] = np.tile(np.arange(64, dtype=np.float32)[None, :], (128, 1))
    c["blk128"] = np.tile((np.arange(256, dtype=np.float32) * 128.0)[None, :], (128, 1))
    c["pidx"] = np.tile(np.arange(128, dtype=np.float32)[:, None], (1, 8))
    names = ["ident", "rm", "mcur", "mprev", "bigm", "ustr", "ones", "sel", "iota64", "blk128", "pidx"]
    offs = {}
    o = 0
    for n in names:
        offs[n] = (o, c[n].shape[1])
        o += c[n].shape[1]
    arr = np.concatenate([c[n] for n in names], axis=1)
    return arr, offs


_CONST_ARR, _COFF = build_consts()
NCONST = _CONST_ARR.shape[1]


def build(S, NB, stage=3, nblk_moe=None):
    assert S % ST == 0
    NST = S // ST
    NTOK = NB * S
    NTT = NTOK // 128
    A = NTOK * 2
    NBLKM = A // 128 + NEXP if nblk_moe is None else nblk_moe
    R = NBLKM * 128

    nc = bass.Bass("TRN2", target_bir_lowering=False)
    dx = nc.dram_tensor("x", [NB, S, D], F32, kind="ExternalInput").ap()
    dmem = nc.dram_tensor("mem", [NB, MEM, D], F32, kind="ExternalInput").ap()
    dpos = nc.dram_tensor("pos", [NB, S], I32, kind="ExternalInput").ap()
    dwall = nc.dram_tensor("wall", [NBLK, 128, 4096], F32, kind="ExternalInput").ap()
    dconst = nc.dram_tensor("consts", [128, NCONST], F32, kind="ExternalInput").ap()
    dvec = nc.dram_tensor("vecs", [6, D], F32, kind="ExternalInput").ap()
    dpcol = nc.dram_tensor("pcol", [128, 56], F32, kind="ExternalInput").ap()
    dsm = nc.dram_tensor("smalls", [128], F32, kind="ExternalInput").ap()
    dwr = nc.dram_tensor("wr", [128, 8, 72], F32, kind="ExternalInput").ap()
    dwg = nc.dram_tensor("wgu", [NEXP * 128, 4096], F32, kind="ExternalInput").ap()
    dwd = nc.dram_tensor("wd", [NEXP * 128, 2048], F32, kind="ExternalInput").ap()
    dout = nc.dram_tensor("out", [NB, S, D], F32, kind="ExternalOutput").ap()
    WS = nc.dram_tensor("ws", [NBLK, 128, 4096], BF16, kind="Internal").ap()
    H32 = nc.dram_tensor("h32", [NTOK, D], F32, kind="Internal").ap()
    XS = nc.dram_tensor("xs", [R, D], BF16, kind="Internal").ap()
    YS = nc.dram_tensor("ys", [R, D], BF16, kind="Internal").ap()
    WGB = nc.dram_tensor("wgb", [NEXP * 128, 4096], BF16, kind="Internal").ap()
    WDB = nc.dram_tensor("wdb", [NEXP * 128, 2048], BF16, kind="Internal").ap()

    sch = Sched(nc)
    es = contextlib.ExitStack()
    AW = 53000
    arena = es.enter_context(nc.sbuf_tensor("arena", [128, AW], F32))
    ptr = [0]

    def carve(shape, dt=F32, parts=128):
        n = int(np.prod(shape[1:]))
        words = n if dt in (F32, I32) else (n + 1) // 2
        words = (words + 7) // 8 * 8
        a = arena[0:parts, ptr[0]:ptr[0] + words]
        ptr[0] += words
        assert ptr[0] <= AW, "arena overflow %d" % ptr[0]
        if dt != F32:
            a = a.bitcast(dt)
        a = a[:, 0:n]
        if len(shape) == 3:
            a = a.rearrange("p (a b) -> p a b", a=shape[1])
        elif len(shape) == 4:
            a = a.rearrange("p (a b c) -> p a b c", a=shape[1], b=shape[2])
        return a

    cst = carve([128, NCONST])
    identb = carve([128, 128], BF16)
    onesb = carve([128, 128], BF16)
    ustrb = carve([128, 128], BF16)
    lnp = carve([128, 4, D])
    pcol = carve([128, 56])
    smb = carve([128, 128])
    xsb = carve([128, 4, D])
    RW = carve([128, NTT, 6])
    posI = carve([128, NTT, 2], I32)
    ohcum = carve([128, 64], BF16)
    stats4 = carve([128, 4, 2, 6])
    mvt4 = carve([128, 4, 2])
    p1_base = ptr[0]
    rmb = carve([128, 128], BF16)
    mcurb = carve([128, 512], BF16)
    mprevb = carve([128, 512], BF16)
    hi16 = carve([1, 16], BF16, parts=1)
    lo32 = carve([1, 16], parts=1)
    NRING = 2
    wring = [carve([128, 8, 512], BF16) for i in range(NRING)]
    fmb = [carve([128, 8, ST], BF16) for i in range(6)]
    xT, qT, sga, sgm, attT, hmT = fmb
    RxT, RqT, Rsga, Rsgm, RattT, RhmT = ["fm%d" % i for i in range(6)]
    qkc, Rqkc = xT, RxT
    yT, RyT = qT, RqT
    h1T, Rh1T = xT, RxT
    qxT, RqxT = attT, RattT
    oxT, RoxT = hmT, RhmT
    kdT = carve([128, 2, 5, 128], BF16)
    vatt = carve([128, 5, 128], BF16)
    pre = carve([128, 8, 3 + ST + 1], BF16)
    vext = carve([128, 4, 4, 264], BF16)
    sigmo = carve([128, 4, D], BF16)
    t32ab = carve([128, 2 * ST]); t32a = t32ab[:, 0:ST]; t32b = t32ab[:, ST:2 * ST]; t32c = carve([128, ST])
    tb16 = carve([128, ST], BF16)
    cosT = carve([128, ST]); sinT = carve([128, ST])
    PT6 = carve([128, 8, 512], BF16)
    rden = carve([128, 512])
    posi = rden.bitcast(I32)
    _o = ptr[0]
    rowi = carve([4, ST], parts=4); rowsp = carve([4, ST], parts=4); rowF = carve([4, ST], parts=4)
    esk = arena[32:34, _o:_o + 1024].bitcast(BF16).rearrange("p (a b) -> p a b", a=4)
    ones2 = arena[32:34, _o + 1024:_o + 1056].bitcast(BF16)
    rowG = carve([4, ST], parts=4)
    rowa = rowi; rowm = rowsp
    ones4 = carve([4, ST], parts=4)
    carry = carve([4, 2], parts=4)
    Gb = carve([128, 4, ST])
    memsb = Gb.rearrange("p a b -> p (a b)").rearrange("p (a b) -> p a b", a=2)
    gprev = carve([128, 4])
    _gbf = Gb.rearrange("p a b -> p (a b)")
    _ro = [0]

    def galias(shape, dt=F32):
        n = int(np.prod(shape[1:]))
        words = n if dt == F32 else (n + 1) // 2
        words = (words + 7) // 8 * 8
        v = _gbf[:, _ro[0]:_ro[0] + words]
        _ro[0] += words
        assert _ro[0] <= 2048
        if dt != F32:
            v = v.bitcast(dt)
        v = v[:, 0:n]
        if len(shape) == 3:
            v = v.rearrange("p (a b) -> p a b", a=shape[1])
        elif len(shape) == 4:
            v = v.rearrange("p (a b c) -> p a b c", a=shape[1], b=shape[2])
        return v
    lg4 = galias([128, 4, 72]); lem4 = galias([128, 4, 64]); top84 = galias([128, 4, 8]); oh4 = galias([128, 4, 2, 64])
    ohb4 = galias([128, 4, 64], BF16); j4 = galias([128, 4, 64]); d4 = galias([128, 4, 8]); pen4 = galias([128, 4, 8])
    rq = galias([128, 12, 4]); ohs = galias([128, 64])
    ROUTE_RES = ["lg4", "lem4", "top84", "oh4", "ohb4", "j4", "d4", "pen4", "rq", "ohs"]
    aT = carve([128, 4, 4]); emt = carve([128, 4, 4])
    S32 = carve([128, 4, 264]); Sbf = carve([128, 4, 264], BF16)
    _mo = _COFF["mcur"][0]; _po = _COFF["mprev"][0]
    Wt4 = cst[:, _mo:_mo + 512].rearrange("p (a b) -> p a b", a=4)
    Gm4 = cst[:, _po:_po + 512].rearrange("p (a b) -> p a b", a=4)
    wint4 = carve([128, 4, 128])
    PTm4 = carve([128, 4, 128], BF16); qtil4 = carve([128, 4, 128], BF16); ktil4 = carve([128, 4, 128], BF16)
    hmtok4 = carve([128, 4, 256], BF16)
    sm4 = carve([128, 16, 4]); sqj = rden[:, 0:256]
    PxT2 = carve([128, 2, ST], BF16); rden2 = t32c
    memT = t32ab.bitcast(BF16).rearrange("p (a b) -> p a b", a=8); KmT = carve([128, 8, MEM], BF16); Vm = carve([128, 2, D], BF16)
    PxT = carve([128, 2, ST], BF16)
    h2Ts = [t32ab.rearrange("p (a b) -> p a b", a=8),
            PT6[:, 0:4, :].rearrange("p a b -> p (a b)").bitcast(F32).rearrange("p (a b) -> p a b", a=8)]
    H2R = [["t32a", "t32b"], [("PTq", i) for i in range(4)]]; wrs = carve([128, 8, 72])
    p1_top = ptr[0]

    ps_t = es.enter_context(nc.psum_tensor("ps", [128, 8, 512], F32))
    psn = [0]

    def bank():
        i = psn[0] % 8
        psn[0] += 1
        return ps_t[:, i, :], ("ps", i)

    def C(name):
        o, w = _COFF[name]
        return cst[:, o:o + w]

    def mm(out, lhsT, rhs, start, stop, r, w):
        return sch.op("pe", lambda e: e.matmul(out, lhsT, rhs, start=start, stop=stop), r, w)

    def tr(out, in_, ident, r, w):
        return sch.op("pe", lambda e: e.transpose(out, in_, ident), r, w)

    def act(out, in_, func, r, w, bias=0.0, scale=1.0, accum_out=None):
        if accum_out is None:
            return sch.op("act", lambda e: e.activation(out, in_, func, bias=bias, scale=scale), r, w)
        return sch.op("act", lambda e: e.activation(out, in_, func, bias=bias, scale=scale, accum_out=accum_out), r, w)

    def tt(eng, out, in0, in1, op, r, w):
        return sch.op(eng, lambda e: e.tensor_tensor(out, in0, in1, op), r, w)

    def ts(eng, out, in0, s1, s2, op0, op1, r, w):
        if op1 is None:
            return sch.op(eng, lambda e: e.tensor_scalar(out, in0, s1, None, op0), r, w)
        return sch.op(eng, lambda e: e.tensor_scalar(out, in0, s1, s2, op0, op1), r, w)

    def stt(out, in0, scalar, in1, op0, op1, r, w):
        return sch.op("dve", lambda e: e.scalar_tensor_tensor(out, in0, scalar, in1, op0, op1), r, w)

    def cp(eng, out, in_, r, w):
        if eng == "act":
            return sch.op("act", lambda e: e.activation(out, in_, AF.Copy), r, w)
        return sch.op(eng, lambda e: e.tensor_copy(out, in_), r, w)

    def red(out, in_, op, r, w):
        return sch.op("dve", lambda e: e.tensor_reduce(out, in_, AX.X, op), r, w)

    def recip(out, in_, r, w):
        return sch.op("dve", lambda e: e.reciprocal(out, in_), r, w)

    def dma(q, out, in_, r, w):
        return sch.dma(q, lambda e: e.dma_start(out, in_), r, w)

    def memset(eng, ap, val, w):
        return sch.op(eng, lambda e: e.memset(ap, val), (), w)

    XSB = [("xsb", i) for i in range(4)]
    gcol = pcol[:, 0:8]; cb = pcol[:, 8:16]; invf = pcol[:, 48:49]
    def cwt(ch, j):
        return pcol[:, 16 + ch * 4 + j:16 + ch * 4 + j + 1]

    dma("sp", cst, dconst, (), ["cst"])
    dma("sp", pcol, dpcol, (), ["pcol"])
    dma("sp", smb, dsm.partition_broadcast(128), (), ["smb"])
    for i in range(4):
        dma("sp", lnp[:, i, :], dvec[i, :].partition_broadcast(128), (), ["lnp"])
    dma("sp", wrs, dwr, (), ["wrs"])
    cp("act", identb, C("ident"), ["cst"], ["identb"])
    cp("act", rmb, C("rm"), ["cst"], ["rmb"])
    cp("act", mcurb, C("mcur"), ["cst"], ["mcurb"])
    cp("act", mprevb, C("mprev"), ["cst"], ["mprevb"])
    cp("act", onesb, C("ones"), ["cst"], ["onesb"])
    cp("act", ustrb, C("ustr"), ["cst"], ["ustrb"])
    memset("dve", ones4, 1.0, ["ones4"])
    memset("dve", ones2, 1.0, ["ones2"])
    memset("dve", ohcum, 0.0, ["ohcum"])
    memset("pool", vext, 1.0, ["vext"])
    act(smb[0:1, 100:116], smb[0:1, 0:16], AF.Exp, ["smb"], ["smbx"])
    cp("dve", hi16, smb[0:1, 100:116], ["smbx"], ["hi16"])
    tt("dve", lo32, smb[0:1, 100:116], hi16, ALU.subtract, ["smbx", "hi16"], ["lo32"])
    esk0 = t32ab[0:1, :].bitcast(BF16).rearrange("p (a b) -> p a b", a=4)
    esk1 = Gb[0:1, 0:2, :].rearrange("p a b -> p (a b)").bitcast(BF16).rearrange("p (a b) -> p a b", a=4)
    for g in range(2):
        for half in range(2):
            for slot in range(4):
                hh = 8 * g + 4 * half + slot
                cp("dve", esk0[0:1, g * 2 + half, slot * 128:(slot + 1) * 128],
                   hi16[0:1, hh:hh + 1].to_broadcast([1, 128]), ["hi16"], ["t32a", "t32b"])
                cp("dve", esk1[0:1, g * 2 + half, slot * 128:(slot + 1) * 128],
                   lo32[0:1, hh:hh + 1].to_broadcast([1, 128]), ["lo32"], ["Gb"])
    dma("sp", esk[0:1, :, :], esk0, ["t32a", "t32b"], ["esk"])
    dma("sp", esk[1:2, :, :], esk1, ["Gb"], ["esk"])
    xsflat = xsb.rearrange("p a b -> p (a b)")
    cast_engs = ["act", "dve", "pool"]
    for b in range(NBLK):
        if b in (B_ML, B_ML + 1):
            dma("sp", xsflat, dwall[b], (), XSB)
            slot = b % NRING
            wr = ("wr", slot)
            dst = wring[slot]
            for kc in range(8):
                sch.op("act", (lambda e, kc=kc, dst=dst: e.activation(
                    dst[:, kc, :], xsflat[:, kc * 512:(kc + 1) * 512], AF.Copy, scale=gcol[:, kc:kc + 1])),
                    XSB + ["pcol"], [wr])
            dma("pool", WS[b], dst.rearrange("p a b -> p (a b)"), [wr], [("ws", b)])
        else:
            sch.dma("pool", (lambda e, b=b: e.dma_start(WS[b], dwall[b], max_dma_last_dim=8192)), (), [("ws", b)])

    moe_units = [0]
    units_per_st = -(-NEXP // (NB * NST))

    def convert_moe_weights():
        for _ in range(units_per_st):
            e_ = moe_units[0]
            if e_ >= NEXP:
                return
            moe_units[0] += 1
            rows = slice(e_ * 128, (e_ + 1) * 128)
            sch.dma("pool", (lambda e, rows=rows: e.dma_start(WGB[rows, :], dwg[rows, :], max_dma_last_dim=8192)), (), ["WGB"])
            sch.dma("pool", (lambda e, rows=rows: e.dma_start(WDB[rows, :], dwd[rows, :], max_dma_last_dim=8192)), (), ["WDB"])

    ringn = [NBLK]

    def wload(b):
        slot = ringn[0] % NRING
        ringn[0] += 1
        dma("sp", wring[slot].rearrange("p a b -> p (a b)"), WS[b], [("ws", b)], [("wr", slot)])
        return wring[slot], ("wr", slot)

    final_tokens = []

    def layer_norm(tti, which, add_eng="pool"):
        xr = xsb[:, tti, :]
        res = ("xsb", tti)
        stats = stats4[:, tti]; mvt = mvt4[:, tti, :]
        rs_, rm_ = ("stats", tti), ("mvt", tti)
        for hf in range(2):
            sch.op("dve", (lambda e, hf=hf: e.bn_stats(stats[:, hf, :], xsb[:, tti, hf * 512:(hf + 1) * 512])), [res], [rs_])
        sch.op("dve", lambda e: e.bn_aggr(mvt, stats.rearrange("p a b -> p (a b)")), [rs_], [rm_])
        ts("dve", mvt[:, 1:2], mvt[:, 1:2], LN_EPS, None, ALU.add, None, [rm_], [rm_])
        act(mvt[:, 1:2], mvt[:, 1:2], AF.Sqrt, [rm_], [rm_])
        recip(mvt[:, 1:2], mvt[:, 1:2], [rm_], [rm_])
        stt(mvt[:, 0:1], mvt[:, 0:1], -1.0, mvt[:, 1:2], ALU.mult, ALU.mult, [rm_], [rm_])
        act(xr, xr, AF.Identity, [res, rm_], [res], bias=mvt[:, 0:1], scale=mvt[:, 1:2])
        tt("pool" if add_eng == "dve" else "dve", xr, xr, lnp[:, 2 * which, :], ALU.mult, [res, "lnp"], [res])
        tt(add_eng, xr, xr, lnp[:, 2 * which + 1, :], ALU.add, [res, "lnp"], [res])

    io = _COFF["ident"][0]
    so = _COFF["sel"][0]
    tile_no = 0
    for b in range(NB):
        memset("dve", carry, 0.0, ["carry"])
        memset("dve", gprev, 0.0, ["gprev"])
        memset("dve", S32, 0.0, ["S32"])
        memset("dve", Sbf, 0.0, ["Sbf"])
        memset("dve", pre[:, :, 0:3], 0.0, ["pre"])
        if stage >= 2:
            for mt in range(2):
                dma("sp", memsb[:, mt, :], dmem[b, mt * 128:(mt + 1) * 128, :], (), ["Gb"] + ROUTE_RES)
            for kc in range(8):
                pb, pr = bank()
                for mt in range(2):
                    tr(pb[:, mt * 128:(mt + 1) * 128], memsb[:, mt, kc * 128:(kc + 1) * 128], C("ident"), ["Gb", "cst"], [pr])
                cp("act", memT[:, kc, :], pb[:, 0:256], [pr], ["t32a", "t32b"])
            for n in range(4):
                wt_, wr_ = wload(B_XKV + n)
                if n < 2:
                    for oc in range(4):
                        pb, pr = bank()
                        for kc in range(8):
                            mm(pb[:, 0:MEM], wt_[:, kc, oc * 128:(oc + 1) * 128], memT[:, kc, :], kc == 0, kc == 7,
                               [wr_, "t32a", "t32b"], [pr])
                        cp("act", KmT[:, n * 4 + oc, :], pb[:, 0:MEM], [pr], ["KmT"])
                else:
                    for mt in range(2):
                        pb, pr = bank()
                        for kc in range(8):
                            mm(pb, memT[:, kc, mt * 128:(mt + 1) * 128], wt_[:, kc, :], kc == 0, kc == 7, [wr_, "t32a", "t32b"], [pr])
                        cp("act", Vm[:, mt, (n - 2) * 512:(n - 1) * 512], pb, [pr], ["Vm"])

        for st in range(NST):
            t0 = st * ST
            first = (st == 0)
            for tti in range(4):
                dma("act", xsb[:, tti, :], dx[b, t0 + tti * 128:t0 + (tti + 1) * 128, :], (), [("xsb", tti)])
            if stage >= 3:
                convert_moe_weights()
            for kc in range(8):
                pb, pr = bank()
                for tti in range(4):
                    tr(pb[:, tti * 128:(tti + 1) * 128], xsb[:, tti, kc * 128:(kc + 1) * 128], C("ident"),
                       [("xsb", tti), "cst"], [pr])
                cp("act" if kc % 2 else "dve", xT[:, kc, :], pb, [pr], [RxT])
            dma("sp", posi, dpos[b, t0:t0 + ST].partition_broadcast(128), (), ["rden"])
            cp("dve", t32a, posi, ["rden"], ["t32a"])
            ts("dve", t32a, t32a, invf, None, ALU.mult, None, ["t32a", "pcol"], ["t32a"])
            for which, dst, dres in ((0, sinT, "sinT"), (1, cosT, "cosT")):
                if which == 1:
                    ts("dve", t32a, t32a, math.pi / 2, None, ALU.add, None, ["t32a"], ["t32a"])
                ts("dve", posi, t32a, 1.0 / (2 * math.pi), None, ALU.mult, None, ["t32a"], ["rden"])
                cp("dve", t32b, posi, ["rden"], ["t32b"])
                stt(t32c, t32b, -6.28125, t32a, ALU.mult, ALU.add, ["t32b", "t32a"], ["t32c"])
                stt(t32c, t32b, -(2 * math.pi - 6.28125), t32c, ALU.mult, ALU.add, ["t32b", "t32c"], ["t32c"])
                ts("dve", t32b, t32c, math.pi, None, ALU.is_gt, None, ["t32c"], ["t32b"])
                stt(t32c, t32b, -2 * math.pi, t32c, ALU.mult, ALU.add, ["t32b", "t32c"], ["t32c"])
                ts("dve", t32b, t32c, -math.pi, None, ALU.is_lt, None, ["t32c"], ["t32b"])
                stt(t32c, t32b, 2 * math.pi, t32c, ALU.mult, ALU.add, ["t32b", "t32c"], ["t32c"])
                ts("dve", t32c, t32c, math.pi, -math.pi, ALU.min, ALU.max, ["t32c"], ["t32c"])
                act(dst, t32c, AF.Sin, ["t32c"], [dres])

            def fm_group(wt_, wr_, j, M=128):
                pb, pr = bank()
                for kc in range(8):
                    mm(pb[0:M, :], wt_[:, kc, j * 128:j * 128 + M], xT[:, kc, :], kc == 0, kc == 7, [wr_, RxT], [pr])
                return pb, pr

            def rope_to(pb, pr, dst, dres):
                cp("act", tb16, pb, [pr], ["tb16"])
                p2, pr2 = bank()
                mm(p2, rmb, tb16, True, True, ["rmb", "tb16"], [pr2])
                tt("pool", t32a, tb16, cosT, ALU.mult, ["tb16", "cosT"], ["t32a"])
                tt("dve", t32b, p2, sinT, ALU.mult, [pr2, "sinT"], ["t32b"])
                tt("dve", dst, t32a, t32b, ALU.add, ["t32a", "t32b"], [dres])

            for g in range(2):
                wt_, wr_ = wload(g)
                for c in range(4):
                    pb, pr = fm_group(wt_, wr_, c)
                    rope_to(pb, pr, qT[:, g * 4 + c, :], RqT)
            wt_, wr_ = wload(2)
            for g in range(2):
                pb, pr = fm_group(wt_, wr_, g)
                rope_to(pb, pr, kdT[:, g, 1:5, :].rearrange("p a b -> p (a b)"), "kdT")
            pb, pr = fm_group(wt_, wr_, 2, M=4)
            act(rowi, pb[0:4, :], AF.Identity, [pr, "pcol"], ["rowi"], bias=pcol[0:4, 49:50])
            pb, pr = fm_group(wt_, wr_, 3, M=4)
            act(rowsp, pb[0:4, :], AF.Identity, [pr, "pcol"], ["rowsp"], bias=pcol[0:4, 50:51])
            act(rowsp, rowsp, AF.Exp, ["rowsp"], ["rowsp"], scale=-1.0)
            act(rowsp, rowsp, AF.Ln, ["rowsp"], ["rowsp"], bias=1.0)
            sch.op("dve", lambda e: e.tensor_tensor_scan(rowF, ones4, rowsp, carry[:, 0:1], ALU.mult, ALU.subtract),
                   ["ones4", "rowsp", "carry"], ["rowF"])
            tt("dve", rowa, rowi, rowF, ALU.subtract, ["rowi", "rowF"], ["rowi"])
            sch.op("dve", lambda e: e.tensor_tensor_scan(rowG, rowa, rowa, carry[:, 1:2], ALU.max, ALU.max),
                   ["rowi", "carry"], ["rowG"])
            cp("dve", carry[:, 0:1], rowF[:, ST - 1:ST], ["rowF"], ["carry"])
            cp("dve", carry[:, 1:2], rowG[:, ST - 1:ST], ["rowG"], ["carry"])
            stt(rowm, rowF, -1.0, rowG, ALU.mult, ALU.subtract, ["rowF", "rowG"], ["rowsp"])
            ts("dve", rowa, rowa, -0.5 * math.log(128.0), None, ALU.add, None, ["rowi"], ["rowi"])
            for which in range(2):
                wt_, wr_ = wload(3 + which)
                for h in range(4):
                    pb, pr = fm_group(wt_, wr_, h)
                    cp("act", pre[:, which * 4 + h, 3:3 + ST], pb, [pr], ["pre"])
            for which, dstt, dres in ((0, sga, Rsga), (1, sgm, Rsgm)):
                for n in range(2):
                    wt_, wr_ = wload(5 + which * 2 + n)
                    for c in range(4):
                        pb, pr = fm_group(wt_, wr_, c)
                        act(dstt[:, n * 4 + c, :], pb, AF.Sigmoid, [pr], [dres])
            for n in range(2):
                wt_, wr_ = wload(9 + n)
                for tti in range(4):
                    pb, pr = bank()
                    for kc in range(8):
                        mm(pb, xT[:, kc, tti * 128:(tti + 1) * 128], wt_[:, kc, :], kc == 0, kc == 7, [wr_, RxT], [pr])
                    cp("act", vext[:, tti, 2 * n:2 * n + 2, 0:256], pb.rearrange("p (a b) -> p a b", a=2), [pr], ["vext"])
            for n in range(2):
                wt_, wr_ = wload(11 + n)
                for tti in range(4):
                    pb, pr = bank()
                    for kc in range(8):
                        mm(pb, xT[:, kc, tti * 128:(tti + 1) * 128], wt_[:, kc, :], kc == 0, kc == 7, [wr_, RxT], [pr])
                    act(sigmo[:, tti, n * 512:(n + 1) * 512], pb, AF.Sigmoid, [pr], ["sigmo"])
            wt_, wr_ = wload(13)
            for tti in range(4):
                pb, pr = bank()
                for kc in range(8):
                    mm(pb[:, 0:128], xT[:, kc, tti * 128:(tti + 1) * 128], wt_[:, kc, 0:128], kc == 0, kc == 7, [wr_, RxT], [pr])
                cp("act", vatt[:, 1 + tti, :], pb[:, 0:128], [pr], ["vatt"])

            def att_A(u_):
                tti, g = u_ // 2, u_ % 2
                js = [1] if (first and tti == 0) else [0, 1]
                PQ = {}
                for j in js:
                    slot = tti + j
                    for half in range(2):
                        q_ = (4 * u_ + 2 * j + half) % 8
                        PQ[(j, half)] = (PT6[:, q_, :], ("PTq", q_))
                        pt_, ptr_ = PQ[(j, half)]
                        pb, pr = bank()
                        mm(pb, kdT[half * 64:(half + 1) * 64, g, slot, :],
                           qT[half * 64:(half + 1) * 64, g * 4:(g + 1) * 4, tti * 128:(tti + 1) * 128],
                           True, True, ["kdT", RqT], [pr])
                        act(pt_, pb, AF.Exp, [pr], [ptr_], scale=0.125)
                        tt("pool", pt_, pt_, (mprevb if j == 0 else mcurb), ALU.mult, [ptr_, "mprevb", "mcurb"], [ptr_])
                return js, PQ

            def att_B(u_, js, PQ):
                tti, g = u_ // 2, u_ % 2
                po, pro = bank()
                pd, prd = bank()
                for half in range(2):
                    hs = slice(half * 64, (half + 1) * 64)
                    for ji, j in enumerate(js):
                        pt_, ptr_ = PQ[(j, half)]
                        mm(po[hs, :], vatt[:, tti + j, g * 64:(g + 1) * 64], pt_, ji == 0, ji == len(js) - 1, ["vatt", ptr_], [pro])
                    for ji, j in enumerate(js):
                        pt_, ptr_ = PQ[(j, half)]
                        mm(pd[hs, :], onesb[:, 0:64], pt_, ji == 0, False, ["onesb", ptr_], [prd])
                    mm(pd[hs, :], ones2, esk[:, g * 2 + half, :], False, True, ["ones2", "esk"], [prd])
                rd_ = rden if u_ % 2 == 0 else t32c
                rr_ = "rden" if u_ % 2 == 0 else "t32c"
                act(rd_, pd, AF.Ln, [prd], [rr_])
                act(rd_, rd_, AF.Exp, [rr_], [rr_], scale=-1.0)
                tt("dve", attT[:, g * 4:(g + 1) * 4, tti * 128:(tti + 1) * 128],
                   po.rearrange("p (a b) -> p a b", a=4), rd_.rearrange("p (a b) -> p a b", a=4), ALU.mult,
                   [pro, rr_], [RattT])

            pend_ = att_A(0)
            for u_ in range(8):
                nxt_ = att_A(u_ + 1) if u_ + 1 < 8 else None
                att_B(u_, *pend_)
                pend_ = nxt_
            cp("pool", kdT[:, :, 0, :], kdT[:, :, 4, :], ["kdT"], ["kdT"])
            cp("pool", vatt[:, 0, :], vatt[:, 4, :], ["vatt"], ["vatt"])

            pb, pr = bank()
            for c in range(4):
                tr(pb[:, c * 4:(c + 1) * 4], rowa[:, c * 128:(c + 1) * 128], cst[0:4, io:io + 4], ["rowi", "cst"], [pr])
                tr(pb[:, 16 + c * 4:16 + (c + 1) * 4], rowm[:, c * 128:(c + 1) * 128], cst[0:4, io:io + 4], ["rowsp", "cst"], [pr])
            cp("dve", aT.rearrange("p a b -> p (a b)"), pb[:, 0:16], [pr], ["aT"])
            act(emt.rearrange("p a b -> p (a b)"), pb[:, 16:32], AF.Exp, [pr], ["emt"])
            for h in range(4):
                pb, pr = bank()
                mm(pb, cst[0:4, so + h * 128:so + (h + 1) * 128], rowG, True, True, ["cst", "rowG"], [pr])
                cp("act", Gb[:, h, :], pb, [pr], ["Gb"] + (ROUTE_RES if h == 0 else []))
            for ch in range(8):
                tc_, tr_ = (t32a, "t32a") if ch % 2 == 0 else (t32b, "t32b")
                ts("dve", tc_, pre[:, ch, 3:3 + ST], cwt(ch, 3), cb[:, ch:ch + 1], ALU.mult, ALU.add, ["pre", "pcol"], [tr_])
                for j in range(3):
                    stt(tc_, pre[:, ch, j:j + ST], cwt(ch, j), tc_, ALU.mult, ALU.add, ["pre", "pcol", tr_], [tr_])
                act(qkc[:, ch, :], tc_, AF.Silu, [tr_], [Rqkc])
            cp("pool", pre[:, :, 0:3], pre[:, :, ST:ST + 3], ["pre"], ["pre"])
            for n in range(2):
                wt_, wr_ = wload(B_ATT + n)
                for oc in range(4):
                    pb, pr = bank()
                    for c in range(8):
                        mm(pb, wt_[:, c, oc * 128:(oc + 1) * 128], attT[:, c, :], c == 0, c == 7, [wr_, RattT], [pr])
                    tt("dve", sga[:, n * 4 + oc, :], pb, sga[:, n * 4 + oc, :], ALU.mult, [pr, Rsga], [Rsga])
            H4 = range(4)

            def fbank(i):
                return ps_t[:, i, :], ("ps", i)

            def ml_P(c):
                cs = slice(c * 128, (c + 1) * 128)
                gps = [gprev[:, h:h + 1] if c == 0 else Gb[:, h, c * 128 - 1:c * 128] for h in H4]
                gpr = "gprev" if c == 0 else "Gb"
                gcs = [Gb[:, h, c * 128 + 127:c * 128 + 128] for h in H4]
                for h in H4:
                    tt("pool", Gm4[:, h, :], Gb[:, h, cs], C("bigm"), ALU.add, ["Gb", "cst"], [("Gm", h)])
                for h in H4:
                    act(Wt4[:, h, :], Gm4[:, h, :], AF.Exp, [("Gm", h), "aT"], [("Wt", h)], bias=aT[:, c, h:h + 1], scale=-1.0)
                pS, prS = fbank(4)
                for h in H4:
                    mm(pS[:, h * 128:(h + 1) * 128], qkc[:, 4 + h, cs], qkc[:, h, cs], True, True, [Rqkc], [prS])
                tt("dve", PTm4.rearrange("p a b -> p (a b)"), pS, Wt4.rearrange("p a b -> p (a b)"), ALU.mult,
                   [prS] + [("Wt", h) for h in H4], ["PTm4"])
                for h in H4:
                    act(wint4[:, h, :], Gb[:, h, cs], AF.Exp, ["Gb", gpr], [("wint", h)], bias=gps[h], scale=-1.0)
                tt("dve", qtil4, qkc[:, 0:4, cs], wint4, ALU.mult, [Rqkc] + [("wint", h) for h in H4], ["qtil4"])
                gp4 = gprev if c == 0 else Gb[:, :, c * 128 - 1]
                gc4 = Gb[:, :, c * 128 + 127]
                tt("pool", sm4[:, 8, :], gc4, aT[:, c, :], ALU.subtract, ["Gb", "aT"], ["sm89"])
                tt("pool", sm4[:, 9, :], gc4, gp4, ALU.subtract, ["Gb", gpr, "sm89"], ["sm89"])
                act(sm4[:, 8:10, :].rearrange("p a b -> p (a b)"), sm4[:, 8:10, :].rearrange("p a b -> p (a b)"), AF.Exp,
                    ["sm89"], ["sm89"], scale=-1.0)
                pK, prK = fbank(4)
                pKb = pK.bitcast(BF16)
                for h in H4:
                    tr(pKb[:, h * 128:(h + 1) * 128], qkc[:, 4 + h, cs], identb, [Rqkc, "identb"], [prK])
                tt("dve", ktil4, pKb[:, 0:512].rearrange("p (a b) -> p a b", a=4), sm4[:, 8, :].unsqueeze(2).to_broadcast([128, 4, 128]),
                   ALU.mult, [prK, "sm89"], ["ktil4"])
                if c > 0:
                    cp("pool", Sbf[:, :, 0:257], S32[:, :, 0:257], ["S32"], ["Sbf"])
                for h in H4:
                    pD, prD = fbank(5 if h % 2 == 0 else 7)
                    mm(pD[:, 0:257], ktil4[:, h, :], vext[:, c, h, 0:257], True, True, ["ktil4", "vext"], [prD])
                    stt(S32[:, h, 0:257], S32[:, h, 0:257], sm4[:, 9, h:h + 1], pD[:, 0:257], ALU.mult, ALU.add,
                        ["S32", "sm89", prD], ["S32"])

            def ml_Q(c):
                pNs = []
                for h in H4:
                    pN, prN = fbank(h)
                    pNs.append((pN, prN))
                    mm(pN[:, 0:257], qtil4[:, h, :], Sbf[:, h, 0:257], True, False, ["qtil4", "Sbf"], [prN])
                    mm(pN[:, 0:257], PTm4[:, h, :], vext[:, c, h, 0:257], False, True, ["PTm4", "vext"], [prN])
                return pNs

            def ml_R(c, pNs):
                cs = slice(c * 128, (c + 1) * 128)
                act(sm4[:, 0, :], ps_t[:, 0:4, 256], AF.Abs, [pNs[h][1] for h in H4], ["sm0"])
                tt("dve", sm4[:, 0, :], sm4[:, 0, :], emt[:, c, :], ALU.max, ["sm0", "emt"], ["sm0m"])
                recip(sm4[:, 1, :], sm4[:, 0, :], ["sm0m"], ["sm1"])
                for h in H4:
                    act(sqj, pNs[h][0][:, 0:256], AF.Square, [pNs[h][1], "sm1"], ["rden", ("sm2", h)], scale=sm4[:, 1, h:h + 1],
                        accum_out=sm4[:, 2, h:h + 1])
                ts("dve", sm4[:, 3, :], sm4[:, 2, :], 1.0 / 256.0, RMS_EPS, ALU.mult, ALU.add, [("sm2", h) for h in H4], ["sm3"])
                act(sm4[:, 4, :], sm4[:, 3, :], AF.Sqrt, ["sm3"], ["sm4"])
                recip(sm4[:, 5, :], sm4[:, 4, :], ["sm4"], ["sm5"])
                tt("dve", sm4[:, 6, :], sm4[:, 5, :], sm4[:, 1, :], ALU.mult, ["sm5", "sm1"], ["sm6"])
                for h in H4:
                    stt(hmtok4[:, h, :], pNs[h][0][:, 0:256], sm4[:, 6, h:h + 1], sigmo[:, c, h * 256:(h + 1) * 256], ALU.mult, ALU.mult,
                        [pNs[h][1], "sm6", "sigmo"], [("hmtok", h)])
                pT_, prT = fbank(6)
                pTb = pT_.bitcast(BF16)
                for h in H4:
                    for hf in range(2):
                        tr(pTb[:, (2 * h + hf) * 128:(2 * h + hf + 1) * 128], hmtok4[:, h, hf * 128:(hf + 1) * 128], identb,
                           [("hmtok", h), "identb"], [prT])
                cp("act", hmT[:, :, cs], pTb.rearrange("p (a b) -> p a b", a=8), [prT], [RhmT])

            ml_P(0)
            for c in range(4):
                pNs_ = ml_Q(c)
                if c + 1 < 4:
                    ml_P(c + 1)
                else:
                    cp("pool", Sbf[:, :, 0:257], S32[:, :, 0:257], ["S32"], ["Sbf"])
                ml_R(c, pNs_)
            for h in range(4):
                cp("pool", gprev[:, h:h + 1], Gb[:, h, ST - 1:ST], ["Gb"], ["gprev"])

            for n in range(2):
                wt_, wr_ = wload(B_ML + n)
                for oc in range(4):
                    pb, pr = bank()
                    for c in range(8):
                        mm(pb, wt_[:, c, oc * 128:(oc + 1) * 128], hmT[:, c, :], c == 0, c == 7, [wr_, RhmT], [pr])
                    tt("dve", t32c, pb, sgm[:, n * 4 + oc, :], ALU.mult, [pr, Rsgm], ["t32c"])
                    tt("dve", yT[:, n * 4 + oc, :], t32c, sga[:, n * 4 + oc, :], ALU.add, ["t32c", Rsga], [RyT])
            wmix = [wload(B_MIX + n) for n in range(2)]
            for tti in range(4):
                for n in range(2):
                    wt_, wr_ = wmix[n]
                    pb, pr = bank()
                    for c in range(8):
                        mm(pb, yT[:, c, tti * 128:(tti + 1) * 128], wt_[:, c, :], c == 0, c == 7, [wr_, RyT], [pr])
                    stt(xsb[:, tti, n * 512:(n + 1) * 512], xsb[:, tti, n * 512:(n + 1) * 512], ALPHA, pb, ALU.mult, ALU.add,
                        [("xsb", tti), pr], [("xsb", tti)])
                layer_norm(tti, 0)

            if stage >= 2:
                for tti in range(4):
                    for hb in range(2):
                        pb, pr = bank()
                        for k4 in range(4):
                            kc = hb * 4 + k4
                            tr(pb[:, k4 * 128:(k4 + 1) * 128], xsb[:, tti, kc * 128:(kc + 1) * 128], C("ident"),
                               [("xsb", tti), "cst"], [pr])
                        cp("act" if hb else "dve", h1T[:, hb * 4:(hb + 1) * 4, tti * 128:(tti + 1) * 128],
                           pb.rearrange("p (a b) -> p a b", a=4), [pr], [Rh1T])
                for n in range(2):
                    wt_, wr_ = wload(B_XQ + n)
                    for oc in range(4):
                        pb, pr = bank()
                        for c in range(8):
                            mm(pb, wt_[:, c, oc * 128:(oc + 1) * 128], h1T[:, c, :], c == 0, c == 7, [wr_, Rh1T], [pr])
                        cp("act", qxT[:, n * 4 + oc, :], pb, [pr], [RqxT])
                PXs = [PxT, PxT2]
                RDs = [rden, rden2]

                def xa_A(hd):
                    P_ = PXs[hd % 2]
                    for mt in range(2):
                        pb, pr = bank()
                        for dc in range(2):
                            mm(pb, KmT[:, 2 * hd + dc, mt * 128:(mt + 1) * 128], qxT[:, 2 * hd + dc, :], dc == 0, dc == 1,
                               ["KmT", RqxT], [pr])
                        act(P_[:, mt, :], pb, AF.Exp, [pr], [("PxT", hd % 2, mt)], scale=1.0 / 16.0)

                def xa_B(hd):
                    P_ = PXs[hd % 2]
                    rd = RDs[hd % 2]
                    rr = "t32c" if hd % 2 else "rden"
                    pd, prd = bank()
                    for mt in range(2):
                        mm(pd, onesb, P_[:, mt, :], mt == 0, mt == 1, ["onesb", ("PxT", hd % 2, mt)], [prd])
                    act(rd, pd, AF.Ln, [prd], [rr])
                    act(rd, rd, AF.Exp, [rr], [rr], scale=-1.0)
                    for dc in range(2):
                        po, pro = bank()
                        for mt in range(2):
                            mm(po, Vm[:, mt, (2 * hd + dc) * 128:(2 * hd + dc + 1) * 128], P_[:, mt, :], mt == 0, mt == 1,
                               ["Vm", ("PxT", hd % 2, mt)], [pro])
                        tt("dve", oxT[:, 2 * hd + dc, :], po, rd, ALU.mult, [pro, rr], [RoxT])

                xa_A(0)
                for hd in range(4):
                    if hd + 1 < 4:
                        xa_A(hd + 1)
                    xa_B(hd)
                wxo = [wload(B_XO + n) for n in range(2)]
                for tti in range(4):
                    for n in range(2):
                        wt_, wr_ = wxo[n]
                        pb, pr = bank()
                        for c in range(8):
                            mm(pb, oxT[:, c, tti * 128:(tti + 1) * 128], wt_[:, c, :], c == 0, c == 7, [wr_, RoxT], [pr])
                        stt(xsb[:, tti, n * 512:(n + 1) * 512], xsb[:, tti, n * 512:(n + 1) * 512], ALPHA, pb, ALU.mult, ALU.add,
                            [("xsb", tti), pr], [("xsb", tti)])
                    layer_norm(tti, 1)

            if stage < 3:
                for tti in range(4):
                    tk = dma("pool", dout[b, t0 + tti * 128:t0 + (tti + 1) * 128, :], xsb[:, tti, :], [("xsb", tti)], ["dout"])
                    final_tokens.append(tk)
            else:
                T4 = slice(tile_no, tile_no + 4)
                for tti in range(4):
                    tno = tile_no + tti
                    res = ("xsb", tti)
                    h2T = h2Ts[tti % 2]; hres = H2R[tti % 2]
                    dma("pool", H32[tno * 128:(tno + 1) * 128, :], xsb[:, tti, :], [res], ["H32"])
                    for hb in range(2):
                        pb, pr = bank()
                        for k4 in range(4):
                            kc = hb * 4 + k4
                            tr(pb[:, k4 * 128:(k4 + 1) * 128], xsb[:, tti, kc * 128:(kc + 1) * 128], C("ident"), [res, "cst"], [pr])
                        cp("act", h2T[:, hb * 4:(hb + 1) * 4, :], pb.rearrange("p (a b) -> p a b", a=4), [pr], hres)
                    pb, pr = bank()
                    for kc in range(8):
                        mm(pb[:, 0:72], h2T[:, kc, :], wrs[:, kc, :], kc == 0, kc == 7, hres + ["wrs"], [pr])
                    tt("dve", lg4[:, tti, :], pb[:, 0:72], smb[:, 24:96], ALU.add, [pr, "smb"], ["lg4"])
                G8 = lg4[:, :, 0:8]
                red(rq[:, 0, :], G8, ALU.max, ["lg4"], ["rq0"])
                tt("dve", d4, G8, rq[:, 0, :].unsqueeze(2).to_broadcast([128, 4, 8]), ALU.subtract, ["lg4", "rq0"], ["d4"])
                ts("dve", pen4, d4, 0.0, None, ALU.is_equal, None, ["d4"], ["pen4"])
                ts("dve", pen4, pen4, 1e30, -1e30, ALU.mult, ALU.add, ["pen4"], ["pen4"])
                act(d4, d4, AF.Exp, ["d4"], ["d4"])
                red(rq[:, 1, :], d4, ALU.add, ["d4"], ["rq1"])
                recip(rq[:, 2, :], rq[:, 1, :], ["rq1"], ["rq2"])
                tt("dve", lem4.rearrange("p t (g e) -> p t g e", g=8), lg4[:, :, 8:72].rearrange("p t (g e) -> p t g e", g=8),
                   pen4.unsqueeze(3).to_broadcast([128, 4, 8, 8]), ALU.add, ["lg4", "pen4"], ["lem4"])
                for tti in range(4):
                    sch.op("dve", (lambda e, tti=tti: e.max(top84[:, tti, :], lem4[:, tti, :])), ["lem4"], ["top84"])
                for s_ in range(2):
                    tt("dve", oh4[:, :, s_, :], lem4, top84[:, :, s_:s_ + 1].to_broadcast([128, 4, 64]), ALU.is_equal,
                       ["lem4", "top84"], [("oh4", s_)])
                    tt("dve", j4, oh4[:, :, s_, :], C("iota64").unsqueeze(1).to_broadcast([128, 4, 64]), ALU.mult,
                       [("oh4", s_), "cst"], ["j4"])
                    red(RW[:, T4, 2 + s_], j4, ALU.add, ["j4"], ["RW"])
                tt("dve", rq[:, 3, :], top84[:, :, 1], top84[:, :, 0], ALU.subtract, ["top84"], ["rq3"])
                act(rq[:, 3, :], rq[:, 3, :], AF.Exp, ["rq3"], ["rq3"])
                ts("dve", rq[:, 3, :], rq[:, 3, :], 1.0, None, ALU.add, None, ["rq3"], ["rq3"])
                recip(rq[:, 4, :], rq[:, 3, :], ["rq3"], ["rq4"])
                tt("dve", RW[:, T4, 0], rq[:, 4, :], rq[:, 2, :], ALU.mult, ["rq4", "rq2"], ["RW"])
                tt("dve", RW[:, T4, 1], rq[:, 2, :], RW[:, T4, 0], ALU.subtract, ["rq2", "RW"], ["RW"])
                tt("dve", ohb4, oh4[:, :, 0, :], oh4[:, :, 1, :], ALU.add, [("oh4", 0), ("oh4", 1)], ["ohb4"])
                pR, prR = bank()
                for tti in range(4):
                    o_ = pR[:, tti * 64:(tti + 1) * 64]
                    mm(o_, onesb, ohcum, True, False, ["onesb", "ohcum"], [prR])
                    for t2 in range(tti):
                        mm(o_, onesb, ohb4[:, t2, :], False, False, ["onesb", "ohb4"], [prR])
                    mm(o_, ustrb, ohb4[:, tti, :], False, True, ["ustrb", "ohb4"], [prR])
                for s_ in range(2):
                    tt("dve", j4, pR[:, 0:256].rearrange("p (a b) -> p a b", a=4), oh4[:, :, s_, :], ALU.mult, [prR, ("oh4", s_)], ["j4"])
                    red(RW[:, T4, 4 + s_], j4, ALU.add, ["j4"], ["RW"])
                red(ohs, ohb4.rearrange("p t e -> p e t"), ALU.add, ["ohb4"], ["ohs"])
                tt("dve", ohcum, ohcum, ohs, ALU.add, ["ohcum", "ohs"], ["ohcum"])
            tile_no += 4

    if stage >= 3:
        sch.fence()
        ptr[0] = p1_base
        cnt = carve([128, 64]); cnti = carve([128, 64], I32); pend = carve([128, 64]); pstart = carve([128, 64])
        bef = carve([128, 256]); bei = carve([128, 256], I32)
        NWG, NWD, DEP = 4, 6, 3
        wg16 = [carve([128, 8, 512], BF16) for i in range(NWG)]
        wd16 = [carve([128, 2, D], BF16) for i in range(NWD)]
        xblk = [carve([128, D], BF16) for i in range(DEP)]
        xTb = [carve([128, 8, 128], BF16) for i in range(DEP)]
        sgl = [carve([128, 256]) for i in range(DEP)]
        actb = [carve([128, 256], BF16) for i in range(DEP)]
        actT = [carve([128, 2, 128], BF16) for i in range(DEP)]
        ysb = [carve([128, D], BF16) for i in range(DEP)]
        y0s = [carve([128, D], BF16) for i in range(3)]; y1s = [carve([128, D], BF16) for i in range(3)]
        yfs = [carve([128, D]) for i in range(2)]
        x16s = [carve([128, D], BF16) for i in range(4)]
        hls = [carve([128, D]) for i in range(4)]
        pb, pr = bank()
        mm(pb[:, 0:64], onesb, ohcum, True, True, ["onesb", "ohcum"], [pr])
        cp("dve", cnt, pb[:, 0:64], [pr], ["cnt"])
        ts("dve", cnti, cnt, 127.0, None, ALU.add, None, ["cnt"], ["cnti"])
        ts("dve", cnti, cnti, 7, None, ALU.arith_shift_right, None, ["cnti"], ["cnti"])
        ts("dve", cnti, cnti, 7, None, ALU.logical_shift_left, None, ["cnti"], ["cnti"])
        cp("dve", cnt, cnti, ["cnti"], ["cnt"])
        sch.op("dve", lambda e: e.tensor_tensor_scan(pend, C("ones")[:, 0:64], cnt, 0.0, ALU.mult, ALU.add),
               ["cst", "cnt"], ["pend"])
        tt("dve", pstart, pend, cnt, ALU.subtract, ["pend", "cnt"], ["pstart"])
        ohall = carve([128, NTT, 64])
        posf = carve([128, NTT])
        for s_ in range(2):
            tt("dve", ohall, C("iota64").unsqueeze(1).to_broadcast([128, NTT, 64]),
               RW[:, :, 2 + s_:3 + s_].to_broadcast([128, NTT, 64]), ALU.is_equal, ["cst", "RW"], ["ohall"])
            tt("dve", ohall, ohall, pstart.unsqueeze(1).to_broadcast([128, NTT, 64]), ALU.mult, ["ohall", "pstart"], ["ohall"])
            red(posf, ohall, ALU.add, ["ohall"], ["posf"])
            tt("dve", posf, posf, RW[:, :, 4 + s_], ALU.add, ["posf", "RW"], ["posf"])
            cp("dve", posI[:, :, s_], posf, ["posf"], ["posI"])
        memset("dve", bef, 0.0, ["bef"])
        for e_ in range(NEXP):
            stt(bef, C("blk128"), pend[:, e_:e_ + 1], bef, ALU.is_ge, ALU.add, ["cst", "pend", "bef"], ["bef"])
        ts("dve", bef, bef, 128.0, None, ALU.mult, None, ["bef"], ["bef"])
        ts("dve", bef, bef, C("pidx")[:, 0:1], None, ALU.add, None, ["bef", "cst"], ["bef"])
        cp("dve", bei, bef, ["bef"], ["bei"])
        for tno in range(NTT):
            hl = hls[tno % 4]; x16 = x16s[tno % 4]
            dma("sp", hl, H32[tno * 128:(tno + 1) * 128, :], ["H32"], [("hl", tno % 4)])
            cp("act", x16, hl, [("hl", tno % 4)], [("x16", tno % 4)])
            for s_ in range(2):
                sch.dma("pool", (lambda e, tno=tno, s_=s_, x16=x16: e.indirect_dma_start(
                    out=XS, out_offset=bass.IndirectOffsetOnAxis(ap=posI[:, tno, s_:s_ + 1], axis=0),
                    in_=x16, in_offset=None)), [("x16", tno % 4), "posI"], ["XS"])
        sch.fence()
        bc_ = {}

        def bcreg(e):
            if "r" not in bc_:
                bc_["r"] = e.alloc_register("bcreg")
                e.reg_mov(bc_["r"], NEXP * 128 - 1)
            return bc_["r"]

        def moe_A(blk):
            g_, d_, p_ = blk % NWG, blk % NWD, blk % DEP
            sch.dma("pool", (lambda e: e.indirect_dma_start(
                out=wg16[g_].rearrange("p a b -> p (a b)"), out_offset=None, in_=WGB,
                in_offset=bass.IndirectOffsetOnAxis(ap=bei[:, blk:blk + 1], axis=0),
                bounds_check=bcreg(e), oob_is_err=False)), ["bei", "WGB"], [("wg16", g_)])
            sch.dma("pool", (lambda e: e.indirect_dma_start(
                out=wd16[d_].rearrange("p a b -> p (a b)"), out_offset=None, in_=WDB,
                in_offset=bass.IndirectOffsetOnAxis(ap=bei[:, blk:blk + 1], axis=0),
                bounds_check=bcreg(e), oob_is_err=False)), ["bei", "WDB"], [("wd16", d_)])
            dma("act", xblk[p_], XS[blk * 128:(blk + 1) * 128, :], ["XS"], [("xblk", p_)])
            pb, pr = bank()
            pbb = pb.bitcast(BF16)
            for kc in range(8):
                tr(pbb[:, kc * 128:(kc + 1) * 128], xblk[p_][:, kc * 128:(kc + 1) * 128], identb, [("xblk", p_), "identb"], [pr])
            cp("dve", xTb[p_].rearrange("p a b -> p (a b)"), pbb, [pr], [("xTb", p_)])

        def moe_B(blk):
            g_, p_ = blk % NWG, blk % DEP
            pH, prH = bank()
            for kc in range(8):
                mm(pH, xTb[p_][:, kc, :], wg16[g_][:, kc, :], kc == 0, kc == 7, [("xTb", p_), ("wg16", g_)], [prH])
            act(sgl[p_], pH[:, 0:256], AF.Silu, [prH], [("sgl", p_)])
            tt("dve", actb[p_], pH[:, 256:512], sgl[p_], ALU.mult, [prH, ("sgl", p_)], [("actb", p_)])

        def moe_C(blk):
            p_ = blk % DEP
            pb, pr = bank()
            pbb = pb.bitcast(BF16)
            for fc in range(2):
                tr(pbb[:, fc * 128:(fc + 1) * 128], actb[p_][:, fc * 128:(fc + 1) * 128], identb, [("actb", p_), "identb"], [pr])
            cp("dve", actT[p_].rearrange("p a b -> p (a b)"), pbb[:, 0:256], [pr], [("actT", p_)])

        def moe_D(blk):
            d_, p_ = blk % NWD, blk % DEP
            for n in range(2):
                pY, prY = bank()
                for fc in range(2):
                    mm(pY, actT[p_][:, fc, :], wd16[d_][:, fc, n * 512:(n + 1) * 512], fc == 0, fc == 1,
                       [("actT", p_), ("wd16", d_)], [prY])
                cp("act" if n == 0 else "dve", ysb[p_][:, n * 512:(n + 1) * 512], pY, [prY], [("ysb", p_)])
            dma("sp", YS[blk * 128:(blk + 1) * 128, :], ysb[p_], [("ysb", p_)], ["YS"])

        for step in range(NBLKM + 3):
            if step < NBLKM:
                moe_A(step)
            if 0 <= step - 1 < NBLKM:
                moe_B(step - 1)
            if 0 <= step - 2 < NBLKM:
                moe_C(step - 2)
            if 0 <= step - 3 < NBLKM:
                moe_D(step - 3)
        sch.fence()
        for i in range(2):
            dma("sp", lnp[:, i, :], dvec[4 + i, :].partition_broadcast(128), (), ["lnp"])
        def p3_fetch(tno):
            tti = tno % 4
            k3 = tno % 3
            dma("act", xsb[:, tti, :], H32[tno * 128:(tno + 1) * 128, :], ["H32"], [("xsb", tti)])
            for s_, yy, yr in ((0, y0s[k3], ("y0", k3)), (1, y1s[k3], ("y1", k3))):
                sch.dma("pool", (lambda e, s_=s_, yy=yy: e.indirect_dma_start(
                    out=yy, out_offset=None, in_=YS,
                    in_offset=bass.IndirectOffsetOnAxis(ap=posI[:, tno, s_:s_ + 1], axis=0))), ["YS", "posI"], [yr])

        p3_fetch(0)
        if NTT > 1:
            p3_fetch(1)
        for tno in range(NTT):
            tti = tno % 4
            b_ = (tno * 128) // S
            tok0 = tno * 128 - b_ * S
            res = ("xsb", tti)
            if tno + 2 < NTT:
                p3_fetch(tno + 2)
            k3 = tno % 3
            y0 = y0s[k3]; y1 = y1s[k3]
            yf = yfs[tno % 2]; yfr = ("yf", tno % 2)
            act(yf, y0, AF.Copy, [("y0", k3), "RW"], [yfr], scale=RW[:, tno, 0:1])
            stt(yf, y1, RW[:, tno, 1:2], yf, ALU.mult, ALU.add, [("y1", k3), "RW", yfr], [yfr])
            stt(xsb[:, tti, :], xsb[:, tti, :], ALPHA, yf, ALU.mult, ALU.add, [res, yfr], [res])
            layer_norm(tti, 0, add_eng="dve")
            tk = dma("sp", dout[b_, tok0:tok0 + 128, :], xsb[:, tti, :], [res], ["dout"])
            final_tokens.append(tk)

    sch.emit(final_tokens)
    es.close()
    return nc


def make_in_maps(inp, S, NB, ncores):
    f = lambda a: np.ascontiguousarray(np.asarray(a, dtype=np.float32))
    wall = build_wall(f(inp["w_in"][0]), f(inp["w_att_branch"][0]), f(inp["w_ml_branch"][0]), f(inp["w_mix_out"][0]),
                      f(inp["w_xq"][0]), f(inp["w_xo"][0]), f(inp["w_xkv"][0]))
    vecs = np.stack([f(inp[k][0]) for k in ("ln1_g", "ln1_b", "ln2_g", "ln2_b", "ln3_g", "ln3_b")])
    pcol = np.zeros((128, 56), np.float32)
    pcol[:, 0:8] = f(inp["ml_norm_g"][0]).reshape(8, 128).T
    pcol[:, 8:16] = f(inp["conv_b"][0]).reshape(8, 128).T
    cwv = f(inp["conv_w"][0])
    for ch in range(8):
        for j in range(4):
            pcol[:, 16 + ch * 4 + j] = cwv[j, ch * 128:(ch + 1) * 128]
    pcol[:, 48] = (10000.0 ** (-(np.arange(128) % 32).astype(np.float32) / np.float32(32))).astype(np.float32)
    pcol[0:4, 49] = f(inp["b_igate"][0])
    pcol[0:4, 50] = f(inp["b_fgate"][0])
    smalls = np.zeros((128,), np.float32)
    smalls[0:16] = f(inp["attn_sinks"][0])
    smalls[24:32] = f(inp["b_router_group"][0])
    smalls[32:96] = f(inp["b_router_expert"][0])
    wr = np.concatenate([f(inp["w_router_group"][0]), f(inp["w_router_expert"][0])], axis=1)
    wr = np.ascontiguousarray(wr.reshape(8, 128, 72).transpose(1, 0, 2))
    wgu = np.concatenate([f(inp["w_gate"][0]), f(inp["w_up"][0])], axis=2)
    wgu = np.ascontiguousarray(wgu.reshape(NEXP, 8, 128, 512).transpose(0, 2, 1, 3)).reshape(NEXP * 128, 4096)
    wd = f(inp["w_down"][0])
    wd = np.ascontiguousarray(wd.reshape(NEXP, 2, 128, D).transpose(0, 2, 1, 3)).reshape(NEXP * 128, 2048)
    x = f(inp["x"]); mem = f(inp["mem"]); pos = np.ascontiguousarray(np.asarray(inp["positions"], dtype=np.int32))
    maps = []
    for c in range(ncores):
        maps.append({
            "x": x[c * NB:(c + 1) * NB], "mem": mem[c * NB:(c + 1) * NB], "pos": pos[c * NB:(c + 1) * NB],
            "wall": wall, "consts": _CONST_ARR, "vecs": vecs, "pcol": pcol, "smalls": smalls,
            "wr": wr, "wgu": wgu, "wd": wd,
        })
    return maps


_NC_CACHE = {}


def kernel(**inp):
    B, S = inp["x"].shape[0], inp["x"].shape[1]
    NB = B // NCORES
    key = (S, NB)
    if key not in _NC_CACHE:
        _NC_CACHE[key] = build(S, NB)
    nc = _NC_CACHE[key]
    maps = make_in_maps(inp, S, NB, NCORES)
    res = run_bass_kernel_spmd(nc, maps, core_ids=list(range(NCORES)))
    return np.concatenate([r["out"] for r in res.results], axis=0).astype(np.float32)
```

```python
import math
import contextlib
import numpy as np
import concourse.bass as bass
import concourse.mybir as mybir
from concourse.bass_utils import run_bass_kernel_spmd

F32 = mybir.dt.float32
BF16 = mybir.dt.bfloat16
I32 = mybir.dt.int32
ALU = mybir.AluOpType
AF = mybir.ActivationFunctionType
AX = mybir.AxisListType

D = 1024
NCORES = 8
ALPHA = 2.0 ** 0.25
LN_EPS = 1e-5
RMS_EPS = 1e-6
N_DMA_SEMS = 12
ST = 512
NEXP = 64
MEM = 256

O_AQ, O_AK, O_AV, O_MQ, O_MK, O_MV, O_MO, O_MI, O_MF, O_GA, O_GM = (
    0, 1024, 1152, 1280, 1792, 2304, 3328, 4352, 4356, 4360, 5384)


class Sched:
    COMPUTE = ("pe", "act", "dve", "pool")

    def __init__(self, nc, dma_queues=("sp", "act", "pool")):
        self.nc = nc
        self.ops = {e: [] for e in ("pe", "act", "dve", "pool", "sp")}
        self.last_w = {}
        self.readers = {}
        self.dma_queues = dma_queues
        self.dma_count = {q: 0 for q in dma_queues}
        self.fence_toks = []
        self.fence_pending = set()

    def fence(self):
        toks = []
        for e in self.COMPUTE:
            for i in range(len(self.ops[e]) - 1, -1, -1):
                if self.ops[e][i]["dma"] is None:
                    toks.append(("c", e, i))
                    break
        for q in self.dma_queues:
            n = self.dma_count[q]
            for k in range(max(0, n - N_DMA_SEMS), n):
                toks.append(("d", q, k))
        self.fence_toks = toks
        self.fence_pending = set(self.ops.keys())

    def _fence_deps(self, eng, deps):
        if eng in self.fence_pending:
            self.fence_pending.discard(eng)
            deps = list(deps) + list(self.fence_toks)
        return deps

    def _deps(self, reads, writes):
        deps = []
        for r in reads:
            t = self.last_w.get(r)
            if t is not None:
                deps.append(t)
        for w in writes:
            t = self.last_w.get(w)
            if t is not None:
                deps.append(t)
            deps.extend(self.readers.get(w, ()))
        return deps

    def _commit(self, tok, reads, writes):
        for r in reads:
            self.readers.setdefault(r, []).append(tok)
        for w in writes:
            self.last_w[w] = tok
            self.readers[w] = []

    def op(self, eng, fn, reads=(), writes=()):
        deps = self._fence_deps(eng, self._deps(reads, writes))
        idx = len(self.ops[eng])
        tok = ("c", eng, idx)
        if eng == "pe":
            deps = [d for d in deps if not (d[0] == "c" and d[1] == "pe")]
        self.ops[eng].append({"fn": fn, "deps": deps, "signal": False, "dma": None})
        self._commit(tok, reads, writes)
        return tok

    def dma(self, queue, fn, reads=(), writes=()):
        deps = self._fence_deps(queue, self._deps(reads, writes))
        n = self.dma_count[queue]
        self.dma_count[queue] += 1
        tok = ("d", queue, n)
        if n >= N_DMA_SEMS:
            deps.append(("d", queue, n - N_DMA_SEMS))
        self.ops[queue].append({"fn": fn, "deps": deps, "signal": False, "dma": n})
        self._commit(tok, reads, writes)
        return tok

    def emit(self, final_wait_tokens=()):
        nc = self.nc
        for e, lst in self.ops.items():
            for o in lst:
                for d in o["deps"]:
                    if d[0] == "c":
                        self.ops[d[1]][d[2]]["signal"] = True
        for d in final_wait_tokens:
            if d[0] == "c":
                self.ops[d[1]][d[2]]["signal"] = True
        semval = {}
        for e in self.COMPUTE:
            c = 0
            vals = []
            for o in self.ops[e]:
                if o["signal"] and o["dma"] is None:
                    c += 1
                vals.append(c)
            semval[e] = vals
        with contextlib.ExitStack() as st:
            csem = {e: st.enter_context(nc.semaphore("cs_" + e)) for e in self.COMPUTE}
            dsem = {q: [st.enter_context(nc.semaphore("ds_%s_%d" % (q, i))) for i in range(N_DMA_SEMS)]
                    for q in self.dma_queues}
            block = st.enter_context(nc.Block())

            def need(tok):
                if tok[0] == "c":
                    return ("c", tok[1]), csem[tok[1]], semval[tok[1]][tok[2]]
                q, n = tok[1], tok[2]
                return ("d", q, n % N_DMA_SEMS), dsem[q][n % N_DMA_SEMS], 16 * (n // N_DMA_SEMS + 1)

            def run(e, extra=()):
                def body(engine):
                    waited = {}
                    for o in self.ops[e]:
                        reqs = {}
                        for d in o["deps"]:
                            key, sem, val = need(d)
                            if waited.get(key, 0) >= val:
                                continue
                            if key not in reqs or reqs[key][1] < val:
                                reqs[key] = (sem, val)
                        for key, (sem, val) in reqs.items():
                            engine.wait_ge(sem, val)
                            waited[key] = val
                        inst = o["fn"](engine)
                        if o["dma"] is not None:
                            inst.then_inc(dsem[e][o["dma"] % N_DMA_SEMS], 16)
                        elif o["signal"]:
                            inst.then_inc(csem[e], 1)
                    for d in extra:
                        key, sem, val = need(d)
                        if waited.get(key, 0) < val:
                            engine.wait_ge(sem, val)
                            waited[key] = val
                return body

            block.sync(run("sp", final_wait_tokens))
            block.tensor(run("pe"))
            block.scalar(run("act"))
            block.vector(run("dve"))
            block.gpsimd(run("pool"))


def _tile_block(W, cols):
    cols = np.asarray(cols)
    blk = np.zeros((1024, 512), np.float32)
    ok = cols >= 0
    blk[:, ok] = W[:, cols[ok]]
    return blk.reshape(8, 128, 512).transpose(1, 0, 2)


def win_blocks():
    r = np.arange
    blocks = []
    for g in range(2):
        cols = []
        for c in range(4):
            h0, h1 = 8 * g + c, 8 * g + 4 + c
            cols += list(O_AQ + h0 * 64 + r(64)) + list(O_AQ + h1 * 64 + r(64))
        blocks.append(cols)
    cols = []
    for g in range(2):
        cols += list(O_AK + g * 64 + r(64)) * 2
    cols += list(O_MI + r(4)) + [-1] * 124
    cols += list(O_MF + r(4)) + [-1] * 124
    blocks.append(cols)
    blocks.append(list(O_MQ + r(512)))
    blocks.append(list(O_MK + r(512)))
    blocks.append(list(O_GA + r(512)))
    blocks.append(list(O_GA + 512 + r(512)))
    blocks.append(list(O_GM + r(512)))
    blocks.append(list(O_GM + 512 + r(512)))
    blocks.append(list(O_MV + r(512)))
    blocks.append(list(O_MV + 512 + r(512)))
    blocks.append(list(O_MO + r(512)))
    blocks.append(list(O_MO + 512 + r(512)))
    blocks.append(list(O_AV + r(128)) + [-1] * 384)
    return blocks


B_ATT, B_ML, B_MIX, B_XQ, B_XO, B_XKV = 14, 16, 18, 20, 22, 24
NBLK = 28


def att_row_perm():
    rows = []
    for g in range(2):
        for c in range(4):
            h0, h1 = 8 * g + c, 8 * g + 4 + c
            rows += list(h0 * 64 + np.arange(64)) + list(h1 * 64 + np.arange(64))
    return np.asarray(rows)


def build_wall(w_in, w_att, w_ml, w_mix, w_xq, w_xo, w_xkv):
    wall = np.empty((NBLK, 128, 8, 512), np.float32)
    for i, cols in enumerate(win_blocks()):
        wall[i] = _tile_block(w_in, cols)
    watt = w_att[att_row_perm(), :]
    k = 14
    for W in (watt, w_ml, w_mix, w_xq, w_xo):
        for n in range(2):
            wall[k] = _tile_block(W, list(n * 512 + np.arange(512)))
            k += 1
    for n in range(4):
        wall[k] = _tile_block(w_xkv, list(n * 512 + np.arange(512)))
        k += 1
    return wall.reshape(NBLK, 128, 4096)


def build_consts():
    c = {}
    c["ident"] = np.eye(128, dtype=np.float32)
    Rm = np.zeros((128, 128), np.float32)
    for hh in range(2):
        for d in range(32):
            Rm[hh * 64 + d + 32, hh * 64 + d] = -1.0
            Rm[hh * 64 + d, hh * 64 + d + 32] = 1.0
    c["rm"] = Rm
    k = np.arange(128)[:, None]
    q = np.arange(128)[None, :]
    c["mcur"] = np.tile((k <= q).astype(np.float32), (1, 4))
    c["mprev"] = np.tile((k > q).astype(np.float32), (1, 4))
    c["bigm"] = np.where(k > q, 1e30, 0.0).astype(np.float32)
    c["ustr"] = (k < q).astype(np.float32)
    c["ones"] = np.ones((128, 128), np.float32)
    sel = np.zeros((128, 512), np.float32)
    for h in range(4):
        sel[h, h * 128:(h + 1) * 128] = 1.0
    c["sel"] = sel
    c["iota64"] = np.tile(np.arange(64, dtype=np.float32)[None, :], (128, 1))
    c["blk128"] = np.tile((np.arange(256, dtype=np.float32) * 128.0)[None, :], (128, 1))
    c["pidx"] = np.tile(np.arange(128, dtype=np.float32)[:, None], (1, 8))
    names = ["ident", "rm", "mcur", "mprev", "bigm", "ustr", "ones", "sel", "iota64", "blk128", "pidx"]
    offs = {}
    o = 0
    for n in names:
        offs[n] = (o, c[n].shape[1])
        o += c[n].shape[1]
    arr = np.concatenate([c[n] for n in names], axis=1)
    return arr, offs


_CONST_ARR, _COFF = build_consts()
NCONST = _CONST_ARR.shape[1]


def build(S, NB, stage=3, nblk_moe=None):
    assert S % ST == 0
    NST = S // ST
    NTOK = NB * S
    NTT = NTOK // 128
    A = NTOK * 2
    NBLKM = A // 128 + NEXP if nblk_moe is None else nblk_moe
    R = NBLKM * 128

    nc = bass.Bass("TRN2", target_bir_lowering=False)
    dx = nc.dram_tensor("x", [NB, S, D], F32, kind="ExternalInput").ap()
    dmem = nc.dram_tensor("mem", [NB, MEM, D], F32, kind="ExternalInput").ap()
    dpos = nc.dram_tensor("pos", [NB, S], I32, kind="ExternalInput").ap()
    dwall = nc.dram_tensor("wall", [NBLK, 128, 4096], F32, kind="ExternalInput").ap()
    dconst = nc.dram_tensor("consts", [128, NCONST], F32, kind="ExternalInput").ap()
    dvec = nc.dram_tensor("vecs", [6, D], F32, kind="ExternalInput").ap()
    dpcol = nc.dram_tensor("pcol", [128, 56], F32, kind="ExternalInput").ap()
    dsm = nc.dram_tensor("smalls", [128], F32, kind="ExternalInput").ap()
    dwr = nc.dram_tensor("wr", [128, 8, 72], F32, kind="ExternalInput").ap()
    dwg = nc.dram_tensor("wgu", [NEXP * 128, 4096], F32, kind="ExternalInput").ap()
    dwd = nc.dram_tensor("wd", [NEXP * 128, 2048], F32, kind="ExternalInput").ap()
    dout = nc.dram_tensor("out", [NB, S, D], F32, kind="ExternalOutput").ap()
    WS = nc.dram_tensor("ws", [NBLK, 128, 4096], BF16, kind="Internal").ap()
    H32 = nc.dram_tensor("h32", [NTOK, D], F32, kind="Internal").ap()
    XS = nc.dram_tensor("xs", [R, D], BF16, kind="Internal").ap()
    YS = nc.dram_tensor("ys", [R, D], BF16, kind="Internal").ap()
    WGB = nc.dram_tensor("wgb", [NEXP * 128, 4096], BF16, kind="Internal").ap()
    WDB = nc.dram_tensor("wdb", [NEXP * 128, 2048], BF16, kind="Internal").ap()

    sch = Sched(nc)
    es = contextlib.ExitStack()
    AW = 53000
    arena = es.enter_context(nc.sbuf_tensor("arena", [128, AW], F32))
    ptr = [0]

    def carve(shape, dt=F32, parts=128):
        n = int(np.prod(shape[1:]))
        words = n if dt in (F32, I32) else (n + 1) // 2
        words = (words + 7) // 8 * 8
        a = arena[0:parts, ptr[0]:ptr[0] + words]
        ptr[0] += words
        assert ptr[0] <= AW, "arena overflow %d" % ptr[0]
        if dt != F32:
            a = a.bitcast(dt)
        a = a[:, 0:n]
        if len(shape) == 3:
            a = a.rearrange("p (a b) -> p a b", a=shape[1])
        elif len(shape) == 4:
            a = a.rearrange("p (a b c) -> p a b c", a=shape[1], b=shape[2])
        return a

    cst = carve([128, NCONST])
    identb = carve([128, 128], BF16)
    onesb = carve([128, 128], BF16)
    ustrb = carve([128, 128], BF16)
    lnp = carve([128, 4, D])
    pcol = carve([128, 56])
    smb = carve([128, 128])
    xsb = carve([128, 4, D])
    RW = carve([128, NTT, 6])
    posI = carve([128, NTT, 2], I32)
    ohcum = carve([128, 64], BF16)
    stats4 = carve([128, 4, 2, 6])
    mvt4 = carve([128, 4, 2])
    p1_base = ptr[0]
    rmb = carve([128, 128], BF16)
    mcurb = carve([128, 512], BF16)
    mprevb = carve([128, 512], BF16)
    hi16 = carve([1, 16], BF16, parts=1)
    lo32 = carve([1, 16], parts=1)
    NRING = 2
    wring = [carve([128, 8, 512], BF16) for i in range(NRING)]
    fmb = [carve([128, 8, ST], BF16) for i in range(6)]
    xT, qT, sga, sgm, attT, hmT = fmb
    RxT, RqT, Rsga, Rsgm, RattT, RhmT = ["fm%d" % i for i in range(6)]
    qkc, Rqkc = xT, RxT
    yT, RyT = qT, RqT
    h1T, Rh1T = xT, RxT
    qxT, RqxT = attT, RattT
    oxT, RoxT = hmT, RhmT
    kdT = carve([128, 2, 5, 128], BF16)
    vatt = carve([128, 5, 128], BF16)
    pre = carve([128, 8, 3 + ST + 1], BF16)
    vext = carve([128, 4, 4, 264], BF16)
    sigmo = carve([128, 4, D], BF16)
    t32ab = carve([128, 2 * ST]); t32a = t32ab[:, 0:ST]; t32b = t32ab[:, ST:2 * ST]; t32c = carve([128, ST])
    tb16 = carve([128, ST], BF16)
    cosT = carve([128, ST]); sinT = carve([128, ST])
    PT6 = carve([128, 8, 512], BF16)
    rden = carve([128, 512])
    posi = rden.bitcast(I32)
    _o = ptr[0]
    rowi = carve([4, ST], parts=4); rowsp = carve([4, ST], parts=4); rowF = carve([4, ST], parts=4)
    esk = arena[32:34, _o:_o + 1024].bitcast(BF16).rearrange("p (a b) -> p a b", a=4)
    ones2 = arena[32:34, _o + 1024:_o + 1056].bitcast(BF16)
    rowG = carve([4, ST], parts=4)
    rowa = rowi; rowm = rowsp
    ones4 = carve([4, ST], parts=4)
    carry = carve([4, 2], parts=4)
    Gb = carve([128, 4, ST])
    memsb = Gb.rearrange("p a b -> p (a b)").rearrange("p (a b) -> p a b", a=2)
    gprev = carve([128, 4])
    _gbf = Gb.rearrange("p a b -> p (a b)")
    _ro = [0]

    def galias(shape, dt=F32):
        n = int(np.prod(shape[1:]))
        words = n if dt == F32 else (n + 1) // 2
        words = (words + 7) // 8 * 8
        v = _gbf[:, _ro[0]:_ro[0] + words]
        _ro[0] += words
        assert _ro[0] <= 2048
        if dt != F32:
            v = v.bitcast(dt)
        v = v[:, 0:n]
        if len(shape) == 3:
            v = v.rearrange("p (a b) -> p a b", a=shape[1])
        elif len(shape) == 4:
            v = v.rearrange("p (a b c) -> p a b c", a=shape[1], b=shape[2])
        return v
    lg4 = galias([128, 4, 72]); lem4 = galias([128, 4, 64]); top84 = galias([128, 4, 8]); oh4 = galias([128, 4, 2, 64])
    ohb4 = galias([128, 4, 64], BF16); j4 = galias([128, 4, 64]); d4 = galias([128, 4, 8]); pen4 = galias([128, 4, 8])
    rq = galias([128, 12, 4]); ohs = galias([128, 64])
    ROUTE_RES = ["lg4", "lem4", "top84", "oh4", "ohb4", "j4", "d4", "pen4", "rq", "ohs"]
    aT = carve([128, 4, 4]); emt = carve([128, 4, 4])
    S32 = carve([128, 4, 264]); Sbf = carve([128, 4, 264], BF16)
    _mo = _COFF["mcur"][0]; _po = _COFF["mprev"][0]
    Wt4 = cst[:, _mo:_mo + 512].rearrange("p (a b) -> p a b", a=4)
    Gm4 = cst[:, _po:_po + 512].rearrange("p (a b) -> p a b", a=4)
    wint4 = carve([128, 4, 128])
    PTm4 = carve([128, 4, 128], BF16); qtil4 = carve([128, 4, 128], BF16); ktil4 = carve([128, 4, 128], BF16)
    hmtok4 = carve([128, 4, 256], BF16)
    sm4 = carve([128, 16, 4]); sqj = rden[:, 0:256]
    PxT2 = carve([128, 2, ST], BF16); rden2 = t32c
    memT = t32ab.bitcast(BF16).rearrange("p (a b) -> p a b", a=8); KmT = carve([128, 8, MEM], BF16); Vm = carve([128, 2, D], BF16)
    PxT = carve([128, 2, ST], BF16)
    h2Ts = [t32ab.rearrange("p (a b) -> p a b", a=8),
            PT6[:, 0:4, :].rearrange("p a b -> p (a b)").bitcast(F32).rearrange("p (a b) -> p a b", a=8)]
    H2R = [["t32a", "t32b"], [("PTq", i) for i in range(4)]]; wrs = carve([128, 8, 72])
    p1_top = ptr[0]

    ps_t = es.enter_context(nc.psum_tensor("ps", [128, 8, 512], F32))
    psn = [0]

    def bank():
        i = psn[0] % 8
        psn[0] += 1
        return ps_t[:, i, :], ("ps", i)

    def C(name):
        o, w = _COFF[name]
        return cst[:, o:o + w]

    def mm(out, lhsT, rhs, start, stop, r, w):
        return sch.op("pe", lambda e: e.matmul(out, lhsT, rhs, start=start, stop=stop), r, w)

    def tr(out, in_, ident, r, w):
        return sch.op("pe", lambda e: e.transpose(out, in_, ident), r, w)

    def act(out, in_, func, r, w, bias=0.0, scale=1.0, accum_out=None):
        if accum_out is None:
            return sch.op("act", lambda e: e.activation(out, in_, func, bias=bias, scale=scale), r, w)
        return sch.op("act", lambda e: e.activation(out, in_, func, bias=bias, scale=scale, accum_out=accum_out), r, w)

    def tt(eng, out, in0, in1, op, r, w):
        return sch.op(eng, lambda e: e.tensor_tensor(out, in0, in1, op), r, w)

    def ts(eng, out, in0, s1, s2, op0, op1, r, w):
        if op1 is None:
            return sch.op(eng, lambda e: e.tensor_scalar(out, in0, s1, None, op0), r, w)
        return sch.op(eng, lambda e: e.tensor_scalar(out, in0, s1, s2, op0, op1), r, w)

    def stt(out, in0, scalar, in1, op0, op1, r, w):
        return sch.op("dve", lambda e: e.scalar_tensor_tensor(out, in0, scalar, in1, op0, op1), r, w)

    def cp(eng, out, in_, r, w):
        if eng == "act":
            return sch.op("act", lambda e: e.activation(out, in_, AF.Copy), r, w)
        return sch.op(eng, lambda e: e.tensor_copy(out, in_), r, w)

    def red(out, in_, op, r, w):
        return sch.op("dve", lambda e: e.tensor_reduce(out, in_, AX.X, op), r, w)

    def recip(out, in_, r, w):
        return sch.op("dve", lambda e: e.reciprocal(out, in_), r, w)

    def dma(q, out, in_, r, w):
        return sch.dma(q, lambda e: e.dma_start(out, in_), r, w)

    def memset(eng, ap, val, w):
        return sch.op(eng, lambda e: e.memset(ap, val), (), w)

    XSB = [("xsb", i) for i in range(4)]
    gcol = pcol[:, 0:8]; cb = pcol[:, 8:16]; invf = pcol[:, 48:49]
    def cwt(ch, j):
        return pcol[:, 16 + ch * 4 + j:16 + ch * 4 + j + 1]

    dma("sp", cst, dconst, (), ["cst"])
    dma("sp", pcol, dpcol, (), ["pcol"])
    dma("sp", smb, dsm.partition_broadcast(128), (), ["smb"])
    for i in range(4):
        dma("sp", lnp[:, i, :], dvec[i, :].partition_broadcast(128), (), ["lnp"])
    dma("sp", wrs, dwr, (), ["wrs"])
    cp("act", identb, C("ident"), ["cst"], ["identb"])
    cp("act", rmb, C("rm"), ["cst"], ["rmb"])
    cp("act", mcurb, C("mcur"), ["cst"], ["mcurb"])
    cp("act", mprevb, C("mprev"), ["cst"], ["mprevb"])
    cp("act", onesb, C("ones"), ["cst"], ["onesb"])
    cp("act", ustrb, C("ustr"), ["cst"], ["ustrb"])
    memset("dve", ones4, 1.0, ["ones4"])
    memset("dve", ones2, 1.0, ["ones2"])
    memset("dve", ohcum, 0.0, ["ohcum"])
    memset("pool", vext, 1.0, ["vext"])
    act(smb[0:1, 100:116], smb[0:1, 0:16], AF.Exp, ["smb"], ["smbx"])
    cp("dve", hi16, smb[0:1, 100:116], ["smbx"], ["hi16"])
    tt("dve", lo32, smb[0:1, 100:116], hi16, ALU.subtract, ["smbx", "hi16"], ["lo32"])
    esk0 = t32ab[0:1, :].bitcast(BF16).rearrange("p (a b) -> p a b", a=4)
    esk1 = Gb[0:1, 0:2, :].rearrange("p a b -> p (a b)").bitcast(BF16).rearrange("p (a b) -> p a b", a=4)
    for g in range(2):
        for half in range(2):
            for slot in range(4):
                hh = 8 * g + 4 * half + slot
                cp("dve", esk0[0:1, g * 2 + half, slot * 128:(slot + 1) * 128],
                   hi16[0:1, hh:hh + 1].to_broadcast([1, 128]), ["hi16"], ["t32a", "t32b"])
                cp("dve", esk1[0:1, g * 2 + half, slot * 128:(slot + 1) * 128],
                   lo32[0:1, hh:hh + 1].to_broadcast([1, 128]), ["lo32"], ["Gb"])
    dma("sp", esk[0:1, :, :], esk0, ["t32a", "t32b"], ["esk"])
    dma("sp", esk[1:2, :, :], esk1, ["Gb"], ["esk"])
    xsflat = xsb.rearrange("p a b -> p (a b)")
    cast_engs = ["act", "dve", "pool"]
    def convert_block(b):
        if b in (B_ML, B_ML + 1):
            dma("sp", xsflat, dwall[b], (), XSB)
            slot = b % NRING
            wr = ("wr", slot)
            dst = wring[slot]
            for kc in range(8):
                sch.op("act", (lambda e, kc=kc, dst=dst: e.activation(
                    dst[:, kc, :], xsflat[:, kc * 512:(kc + 1) * 512], AF.Copy, scale=gcol[:, kc:kc + 1])),
                    XSB + ["pcol"], [wr])
            dma("pool", WS[b], dst.rearrange("p a b -> p (a b)"), [wr], [("ws", b)])
        else:
            sch.dma("pool", (lambda e, b=b: e.dma_start(WS[b], dwall[b], max_dma_last_dim=8192)), (), [("ws", b)])

    early = [B_ML, B_ML + 1] + list(range(B_XKV, B_XKV + 4)) + list(range(0, 14))
    late = [b for b in range(NBLK) if b not in early]
    for b in early:
        convert_block(b)
    late_pending = [True]

    moe_units = [0]
    units_per_st = -(-NEXP // (NB * NST))

    def convert_moe_weights():
        for _ in range(units_per_st):
            e_ = moe_units[0]
            if e_ >= NEXP:
                return
            moe_units[0] += 1
            rows = slice(e_ * 128, (e_ + 1) * 128)
            sch.dma("pool", (lambda e, rows=rows: e.dma_start(WGB[rows, :], dwg[rows, :], max_dma_last_dim=8192)), (), ["WGB"])
            sch.dma("pool", (lambda e, rows=rows: e.dma_start(WDB[rows, :], dwd[rows, :], max_dma_last_dim=8192)), (), ["WDB"])

    ringn = [NBLK]

    def wload(b):
        slot = ringn[0] % NRING
        ringn[0] += 1
        dma("sp", wring[slot].rearrange("p a b -> p (a b)"), WS[b], [("ws", b)], [("wr", slot)])
        return wring[slot], ("wr", slot)

    final_tokens = []

    def layer_norm(tti, which, add_eng="pool"):
        xr = xsb[:, tti, :]
        res = ("xsb", tti)
        stats = stats4[:, tti]; mvt = mvt4[:, tti, :]
        rs_, rm_ = ("stats", tti), ("mvt", tti)
        for hf in range(2):
            sch.op("dve", (lambda e, hf=hf: e.bn_stats(stats[:, hf, :], xsb[:, tti, hf * 512:(hf + 1) * 512])), [res], [rs_])
        sch.op("dve", lambda e: e.bn_aggr(mvt, stats.rearrange("p a b -> p (a b)")), [rs_], [rm_])
        ts("dve", mvt[:, 1:2], mvt[:, 1:2], LN_EPS, None, ALU.add, None, [rm_], [rm_])
        act(mvt[:, 1:2], mvt[:, 1:2], AF.Sqrt, [rm_], [rm_])
        recip(mvt[:, 1:2], mvt[:, 1:2], [rm_], [rm_])
        stt(mvt[:, 0:1], mvt[:, 0:1], -1.0, mvt[:, 1:2], ALU.mult, ALU.mult, [rm_], [rm_])
        act(xr, xr, AF.Identity, [res, rm_], [res], bias=mvt[:, 0:1], scale=mvt[:, 1:2])
        tt("pool" if add_eng == "dve" else "dve", xr, xr, lnp[:, 2 * which, :], ALU.mult, [res, "lnp"], [res])
        tt(add_eng, xr, xr, lnp[:, 2 * which + 1, :], ALU.add, [res, "lnp"], [res])

    io = _COFF["ident"][0]
    so = _COFF["sel"][0]
    tile_no = 0
    for b in range(NB):
        memset("dve", carry, 0.0, ["carry"])
        memset("dve", gprev, 0.0, ["gprev"])
        memset("dve", S32, 0.0, ["S32"])
        memset("dve", Sbf, 0.0, ["Sbf"])
        memset("dve", pre[:, :, 0:3], 0.0, ["pre"])
        if stage >= 2:
            for mt in range(2):
                dma("sp", memsb[:, mt, :], dmem[b, mt * 128:(mt + 1) * 128, :], (), ["Gb"] + ROUTE_RES)
            for kc in range(8):
                pb, pr = bank()
                for mt in range(2):
                    tr(pb[:, mt * 128:(mt + 1) * 128], memsb[:, mt, kc * 128:(kc + 1) * 128], C("ident"), ["Gb", "cst"], [pr])
                cp("act", memT[:, kc, :], pb[:, 0:256], [pr], ["t32a", "t32b"])
            for n in range(4):
                wt_, wr_ = wload(B_XKV + n)
                if n < 2:
                    for oc in range(4):
                        pb, pr = bank()
                        for kc in range(8):
                            mm(pb[:, 0:MEM], wt_[:, kc, oc * 128:(oc + 1) * 128], memT[:, kc, :], kc == 0, kc == 7,
                               [wr_, "t32a", "t32b"], [pr])
                        cp("act", KmT[:, n * 4 + oc, :], pb[:, 0:MEM], [pr], ["KmT"])
                else:
                    for mt in range(2):
                        pb, pr = bank()
                        for kc in range(8):
                            mm(pb, memT[:, kc, mt * 128:(mt + 1) * 128], wt_[:, kc, :], kc == 0, kc == 7, [wr_, "t32a", "t32b"], [pr])
                        cp("act", Vm[:, mt, (n - 2) * 512:(n - 1) * 512], pb, [pr], ["Vm"])

        for st in range(NST):
            t0 = st * ST
            first = (st == 0)
            for tti in range(4):
                dma("act", xsb[:, tti, :], dx[b, t0 + tti * 128:t0 + (tti + 1) * 128, :], (), [("xsb", tti)])
            if stage >= 3:
                convert_moe_weights()
            for kc in range(8):
                pb, pr = bank()
                for tti in range(4):
                    tr(pb[:, tti * 128:(tti + 1) * 128], xsb[:, tti, kc * 128:(kc + 1) * 128], C("ident"),
                       [("xsb", tti), "cst"], [pr])
                cp("act" if kc % 2 else "dve", xT[:, kc, :], pb, [pr], [RxT])
            dma("sp", posi, dpos[b, t0:t0 + ST].partition_broadcast(128), (), ["rden"])
            cp("dve", t32a, posi, ["rden"], ["t32a"])
            ts("dve", t32a, t32a, invf, None, ALU.mult, None, ["t32a", "pcol"], ["t32a"])
            for which, dst, dres in ((0, sinT, "sinT"), (1, cosT, "cosT")):
                if which == 1:
                    ts("dve", t32a, t32a, math.pi / 2, None, ALU.add, None, ["t32a"], ["t32a"])
                ts("dve", posi, t32a, 1.0 / (2 * math.pi), None, ALU.mult, None, ["t32a"], ["rden"])
                cp("dve", t32b, posi, ["rden"], ["t32b"])
                stt(t32c, t32b, -6.28125, t32a, ALU.mult, ALU.add, ["t32b", "t32a"], ["t32c"])
                stt(t32c, t32b, -(2 * math.pi - 6.28125), t32c, ALU.mult, ALU.add, ["t32b", "t32c"], ["t32c"])
                ts("dve", t32b, t32c, math.pi, None, ALU.is_gt, None, ["t32c"], ["t32b"])
                stt(t32c, t32b, -2 * math.pi, t32c, ALU.mult, ALU.add, ["t32b", "t32c"], ["t32c"])
                ts("dve", t32b, t32c, -math.pi, None, ALU.is_lt, None, ["t32c"], ["t32b"])
                stt(t32c, t32b, 2 * math.pi, t32c, ALU.mult, ALU.add, ["t32b", "t32c"], ["t32c"])
                ts("dve", t32c, t32c, math.pi, -math.pi, ALU.min, ALU.max, ["t32c"], ["t32c"])
                act(dst, t32c, AF.Sin, ["t32c"], [dres])

            def fm_group(wt_, wr_, j, M=128):
                pb, pr = bank()
                for kc in range(8):
                    mm(pb[0:M, :], wt_[:, kc, j * 128:j * 128 + M], xT[:, kc, :], kc == 0, kc == 7, [wr_, RxT], [pr])
                return pb, pr

            def rope_to(pb, pr, dst, dres):
                cp("act", tb16, pb, [pr], ["tb16"])
                p2, pr2 = bank()
                mm(p2, rmb, tb16, True, True, ["rmb", "tb16"], [pr2])
                tt("pool", t32a, tb16, cosT, ALU.mult, ["tb16", "cosT"], ["t32a"])
                tt("dve", t32b, p2, sinT, ALU.mult, [pr2, "sinT"], ["t32b"])
                tt("dve", dst, t32a, t32b, ALU.add, ["t32a", "t32b"], [dres])

            for which in range(2):
                wt_, wr_ = wload(3 + which)
                for h in range(4):
                    pb, pr = fm_group(wt_, wr_, h)
                    cp("act", pre[:, which * 4 + h, 3:3 + ST], pb, [pr], ["pre"])
            for which, dstt, dres in ((0, sga, Rsga), (1, sgm, Rsgm)):
                for n in range(2):
                    wt_, wr_ = wload(5 + which * 2 + n)
                    for c in range(4):
                        pb, pr = fm_group(wt_, wr_, c)
                        act(dstt[:, n * 4 + c, :], pb, AF.Sigmoid, [pr], [dres])
            for n in range(2):
                wt_, wr_ = wload(9 + n)
                for tti in range(4):
                    pb, pr = bank()
                    for kc in range(8):
                        mm(pb, xT[:, kc, tti * 128:(tti + 1) * 128], wt_[:, kc, :], kc == 0, kc == 7, [wr_, RxT], [pr])
                    cp("act", vext[:, tti, 2 * n:2 * n + 2, 0:256], pb.rearrange("p (a b) -> p a b", a=2), [pr], ["vext"])
            for n in range(2):
                wt_, wr_ = wload(11 + n)
                for tti in range(4):
                    pb, pr = bank()
                    for kc in range(8):
                        mm(pb, xT[:, kc, tti * 128:(tti + 1) * 128], wt_[:, kc, :], kc == 0, kc == 7, [wr_, RxT], [pr])
                    act(sigmo[:, tti, n * 512:(n + 1) * 512], pb, AF.Sigmoid, [pr], ["sigmo"])
            wt_, wr_ = wload(13)
            for tti in range(4):
                pb, pr = bank()
                for kc in range(8):
                    mm(pb[:, 0:128], xT[:, kc, tti * 128:(tti + 1) * 128], wt_[:, kc, 0:128], kc == 0, kc == 7, [wr_, RxT], [pr])
                cp("act", vatt[:, 1 + tti, :], pb[:, 0:128], [pr], ["vatt"])

            for g in range(2):
                wt_, wr_ = wload(g)
                for c in range(4):
                    pb, pr = fm_group(wt_, wr_, c)
                    rope_to(pb, pr, qT[:, g * 4 + c, :], RqT)
            wt_, wr_ = wload(2)
            for g in range(2):
                pb, pr = fm_group(wt_, wr_, g)
                rope_to(pb, pr, kdT[:, g, 1:5, :].rearrange("p a b -> p (a b)"), "kdT")
            pb, pr = fm_group(wt_, wr_, 2, M=4)
            act(rowi, pb[0:4, :], AF.Identity, [pr, "pcol"], ["rowi"], bias=pcol[0:4, 49:50])
            pb, pr = fm_group(wt_, wr_, 3, M=4)
            act(rowsp, pb[0:4, :], AF.Identity, [pr, "pcol"], ["rowsp"], bias=pcol[0:4, 50:51])
            act(rowsp, rowsp, AF.Exp, ["rowsp"], ["rowsp"], scale=-1.0)
            act(rowsp, rowsp, AF.Ln, ["rowsp"], ["rowsp"], bias=1.0)
            sch.op("dve", lambda e: e.tensor_tensor_scan(rowF, ones4, rowsp, carry[:, 0:1], ALU.mult, ALU.subtract),
                   ["ones4", "rowsp", "carry"], ["rowF"])
            tt("dve", rowa, rowi, rowF, ALU.subtract, ["rowi", "rowF"], ["rowi"])
            sch.op("dve", lambda e: e.tensor_tensor_scan(rowG, rowa, rowa, carry[:, 1:2], ALU.max, ALU.max),
                   ["rowi", "carry"], ["rowG"])
            cp("dve", carry[:, 0:1], rowF[:, ST - 1:ST], ["rowF"], ["carry"])
            cp("dve", carry[:, 1:2], rowG[:, ST - 1:ST], ["rowG"], ["carry"])
            stt(rowm, rowF, -1.0, rowG, ALU.mult, ALU.subtract, ["rowF", "rowG"], ["rowsp"])
            ts("dve", rowa, rowa, -0.5 * math.log(128.0), None, ALU.add, None, ["rowi"], ["rowi"])
            if late_pending[0]:
                late_pending[0] = False
                for b_ in late:
                    convert_block(b_)
            def att_A(u_):
                tti, g = u_ // 2, u_ % 2
                js = [1] if (first and tti == 0) else [0, 1]
                PQ = {}
                for j in js:
                    slot = tti + j
                    for half in range(2):
                        q_ = (4 * u_ + 2 * j + half) % 8
                        PQ[(j, half)] = (PT6[:, q_, :], ("PTq", q_))
                        pt_, ptr_ = PQ[(j, half)]
                        pb, pr = bank()
                        mm(pb, kdT[half * 64:(half + 1) * 64, g, slot, :],
                           qT[half * 64:(half + 1) * 64, g * 4:(g + 1) * 4, tti * 128:(tti + 1) * 128],
                           True, True, ["kdT", RqT], [pr])
                        act(pt_, pb, AF.Exp, [pr], [ptr_], scale=0.125)
                        tt("pool", pt_, pt_, (mprevb if j == 0 else mcurb), ALU.mult, [ptr_, "mprevb", "mcurb"], [ptr_])
                return js, PQ

            def att_B(u_, js, PQ):
                tti, g = u_ // 2, u_ % 2
                po, pro = bank()
                pd, prd = bank()
                for half in range(2):
                    hs = slice(half * 64, (half + 1) * 64)
                    for ji, j in enumerate(js):
                        pt_, ptr_ = PQ[(j, half)]
                        mm(po[hs, :], vatt[:, tti + j, g * 64:(g + 1) * 64], pt_, ji == 0, ji == len(js) - 1, ["vatt", ptr_], [pro])
                    for ji, j in enumerate(js):
                        pt_, ptr_ = PQ[(j, half)]
                        mm(pd[hs, :], onesb[:, 0:64], pt_, ji == 0, False, ["onesb", ptr_], [prd])
                    mm(pd[hs, :], ones2, esk[:, g * 2 + half, :], False, True, ["ones2", "esk"], [prd])
                rd_ = rden if u_ % 2 == 0 else t32c
                rr_ = "rden" if u_ % 2 == 0 else "t32c"
                act(rd_, pd, AF.Ln, [prd], [rr_])
                act(rd_, rd_, AF.Exp, [rr_], [rr_], scale=-1.0)
                tt("dve", attT[:, g * 4:(g + 1) * 4, tti * 128:(tti + 1) * 128],
                   po.rearrange("p (a b) -> p a b", a=4), rd_.rearrange("p (a b) -> p a b", a=4), ALU.mult,
                   [pro, rr_], [RattT])

            pend_ = att_A(0)
            for u_ in range(8):
                nxt_ = att_A(u_ + 1) if u_ + 1 < 8 else None
                att_B(u_, *pend_)
                pend_ = nxt_
            cp("pool", kdT[:, :, 0, :], kdT[:, :, 4, :], ["kdT"], ["kdT"])
            cp("pool", vatt[:, 0, :], vatt[:, 4, :], ["vatt"], ["vatt"])

            pb, pr = bank()
            for c in range(4):
                tr(pb[:, c * 4:(c + 1) * 4], rowa[:, c * 128:(c + 1) * 128], cst[0:4, io:io + 4], ["rowi", "cst"], [pr])
                tr(pb[:, 16 + c * 4:16 + (c + 1) * 4], rowm[:, c * 128:(c + 1) * 128], cst[0:4, io:io + 4], ["rowsp", "cst"], [pr])
            cp("dve", aT.rearrange("p a b -> p (a b)"), pb[:, 0:16], [pr], ["aT"])
            act(emt.rearrange("p a b -> p (a b)"), pb[:, 16:32], AF.Exp, [pr], ["emt"])
            for h in range(4):
                pb, pr = bank()
                mm(pb, cst[0:4, so + h * 128:so + (h + 1) * 128], rowG, True, True, ["cst", "rowG"], [pr])
                cp("act", Gb[:, h, :], pb, [pr], ["Gb"] + (ROUTE_RES if h == 0 else []))
            for ch in range(8):
                tc_, tr_ = (t32a, "t32a") if ch % 2 == 0 else (t32b, "t32b")
                ts("dve", tc_, pre[:, ch, 3:3 + ST], cwt(ch, 3), cb[:, ch:ch + 1], ALU.mult, ALU.add, ["pre", "pcol"], [tr_])
                for j in range(3):
                    stt(tc_, pre[:, ch, j:j + ST], cwt(ch, j), tc_, ALU.mult, ALU.add, ["pre", "pcol", tr_], [tr_])
                act(qkc[:, ch, :], tc_, AF.Silu, [tr_], [Rqkc])
            cp("pool", pre[:, :, 0:3], pre[:, :, ST:ST + 3], ["pre"], ["pre"])
            for n in range(2):
                wt_, wr_ = wload(B_ATT + n)
                for oc in range(4):
                    pb, pr = bank()
                    for c in range(8):
                        mm(pb, wt_[:, c, oc * 128:(oc + 1) * 128], attT[:, c, :], c == 0, c == 7, [wr_, RattT], [pr])
                    tt("dve", sga[:, n * 4 + oc, :], pb, sga[:, n * 4 + oc, :], ALU.mult, [pr, Rsga], [Rsga])
            H4 = range(4)

            def fbank(i):
                return ps_t[:, i, :], ("ps", i)

            def ml_P(c):
                cs = slice(c * 128, (c + 1) * 128)
                gps = [gprev[:, h:h + 1] if c == 0 else Gb[:, h, c * 128 - 1:c * 128] for h in H4]
                gpr = "gprev" if c == 0 else "Gb"
                gcs = [Gb[:, h, c * 128 + 127:c * 128 + 128] for h in H4]
                for h in H4:
                    tt("pool", Gm4[:, h, :], Gb[:, h, cs], C("bigm"), ALU.add, ["Gb", "cst"], [("Gm", h)])
                for h in H4:
                    act(Wt4[:, h, :], Gm4[:, h, :], AF.Exp, [("Gm", h), "aT"], [("Wt", h)], bias=aT[:, c, h:h + 1], scale=-1.0)
                pS, prS = fbank(4)
                for h in H4:
                    mm(pS[:, h * 128:(h + 1) * 128], qkc[:, 4 + h, cs], qkc[:, h, cs], True, True, [Rqkc], [prS])
                tt("dve", PTm4.rearrange("p a b -> p (a b)"), pS, Wt4.rearrange("p a b -> p (a b)"), ALU.mult,
                   [prS] + [("Wt", h) for h in H4], ["PTm4"])
                for h in H4:
                    act(wint4[:, h, :], Gb[:, h, cs], AF.Exp, ["Gb", gpr], [("wint", h)], bias=gps[h], scale=-1.0)
                tt("dve", qtil4, qkc[:, 0:4, cs], wint4, ALU.mult, [Rqkc] + [("wint", h) for h in H4], ["qtil4"])
                gp4 = gprev if c == 0 else Gb[:, :, c * 128 - 1]
                gc4 = Gb[:, :, c * 128 + 127]
                tt("pool", sm4[:, 8, :], gc4, aT[:, c, :], ALU.subtract, ["Gb", "aT"], ["sm89"])
                tt("pool", sm4[:, 9, :], gc4, gp4, ALU.subtract, ["Gb", gpr, "sm89"], ["sm89"])
                act(sm4[:, 8:10, :].rearrange("p a b -> p (a b)"), sm4[:, 8:10, :].rearrange("p a b -> p (a b)"), AF.Exp,
                    ["sm89"], ["sm89"], scale=-1.0)
                pK, prK = fbank(4)
                pKb = pK.bitcast(BF16)
                for h in H4:
                    tr(pKb[:, h * 128:(h + 1) * 128], qkc[:, 4 + h, cs], identb, [Rqkc, "identb"], [prK])
                tt("dve", ktil4, pKb[:, 0:512].rearrange("p (a b) -> p a b", a=4), sm4[:, 8, :].unsqueeze(2).to_broadcast([128, 4, 128]),
                   ALU.mult, [prK, "sm89"], ["ktil4"])
                if c > 0:
                    cp("pool", Sbf[:, :, 0:257], S32[:, :, 0:257], ["S32"], ["Sbf"])
                for h in H4:
                    pD, prD = fbank(5 if h % 2 == 0 else 7)
                    mm(pD[:, 0:257], ktil4[:, h, :], vext[:, c, h, 0:257], True, True, ["ktil4", "vext"], [prD])
                    stt(S32[:, h, 0:257], S32[:, h, 0:257], sm4[:, 9, h:h + 1], pD[:, 0:257], ALU.mult, ALU.add,
                        ["S32", "sm89", prD], ["S32"])

            def ml_Q(c):
                pNs = []
                for h in H4:
                    pN, prN = fbank(h)
                    pNs.append((pN, prN))
                    mm(pN[:, 0:257], qtil4[:, h, :], Sbf[:, h, 0:257], True, False, ["qtil4", "Sbf"], [prN])
                    mm(pN[:, 0:257], PTm4[:, h, :], vext[:, c, h, 0:257], False, True, ["PTm4", "vext"], [prN])
                return pNs

            def ml_R(c, pNs):
                cs = slice(c * 128, (c + 1) * 128)
                act(sm4[:, 0, :], ps_t[:, 0:4, 256], AF.Abs, [pNs[h][1] for h in H4], ["sm0"])
                tt("dve", sm4[:, 0, :], sm4[:, 0, :], emt[:, c, :], ALU.max, ["sm0", "emt"], ["sm0m"])
                recip(sm4[:, 1, :], sm4[:, 0, :], ["sm0m"], ["sm1"])
                for h in H4:
                    act(sqj, pNs[h][0][:, 0:256], AF.Square, [pNs[h][1], "sm1"], ["rden", ("sm2", h)], scale=sm4[:, 1, h:h + 1],
                        accum_out=sm4[:, 2, h:h + 1])
                ts("dve", sm4[:, 3, :], sm4[:, 2, :], 1.0 / 256.0, RMS_EPS, ALU.mult, ALU.add, [("sm2", h) for h in H4], ["sm3"])
                act(sm4[:, 4, :], sm4[:, 3, :], AF.Sqrt, ["sm3"], ["sm4"])
                recip(sm4[:, 5, :], sm4[:, 4, :], ["sm4"], ["sm5"])
                tt("dve", sm4[:, 6, :], sm4[:, 5, :], sm4[:, 1, :], ALU.mult, ["sm5", "sm1"], ["sm6"])
                for h in H4:
                    stt(hmtok4[:, h, :], pNs[h][0][:, 0:256], sm4[:, 6, h:h + 1], sigmo[:, c, h * 256:(h + 1) * 256], ALU.mult, ALU.mult,
                        [pNs[h][1], "sm6", "sigmo"], [("hmtok", h)])
                pT_, prT = fbank(6)
                pTb = pT_.bitcast(BF16)
                for h in H4:
                    for hf in range(2):
                        tr(pTb[:, (2 * h + hf) * 128:(2 * h + hf + 1) * 128], hmtok4[:, h, hf * 128:(hf + 1) * 128], identb,
                           [("hmtok", h), "identb"], [prT])
                cp("act", hmT[:, :, cs], pTb.rearrange("p (a b) -> p a b", a=8), [prT], [RhmT])

            ml_P(0)
            for c in range(4):
                pNs_ = ml_Q(c)
                if c + 1 < 4:
                    ml_P(c + 1)
                else:
                    cp("pool", Sbf[:, :, 0:257], S32[:, :, 0:257], ["S32"], ["Sbf"])
                ml_R(c, pNs_)
            for h in range(4):
                cp("pool", gprev[:, h:h + 1], Gb[:, h, ST - 1:ST], ["Gb"], ["gprev"])

            for n in range(2):
                wt_, wr_ = wload(B_ML + n)
                for oc in range(4):
                    pb, pr = bank()
                    for c in range(8):
                        mm(pb, wt_[:, c, oc * 128:(oc + 1) * 128], hmT[:, c, :], c == 0, c == 7, [wr_, RhmT], [pr])
                    tt("dve", t32c, pb, sgm[:, n * 4 + oc, :], ALU.mult, [pr, Rsgm], ["t32c"])
                    tt("dve", yT[:, n * 4 + oc, :], t32c, sga[:, n * 4 + oc, :], ALU.add, ["t32c", Rsga], [RyT])
            wmix = [wload(B_MIX + n) for n in range(2)]
            for tti in range(4):
                for n in range(2):
                    wt_, wr_ = wmix[n]
                    pb, pr = bank()
                    for c in range(8):
                        mm(pb, yT[:, c, tti * 128:(tti + 1) * 128], wt_[:, c, :], c == 0, c == 7, [wr_, RyT], [pr])
                    stt(xsb[:, tti, n * 512:(n + 1) * 512], xsb[:, tti, n * 512:(n + 1) * 512], ALPHA, pb, ALU.mult, ALU.add,
                        [("xsb", tti), pr], [("xsb", tti)])
                layer_norm(tti, 0)

            if stage >= 2:
                for tti in range(4):
                    for hb in range(2):
                        pb, pr = bank()
                        for k4 in range(4):
                            kc = hb * 4 + k4
                            tr(pb[:, k4 * 128:(k4 + 1) * 128], xsb[:, tti, kc * 128:(kc + 1) * 128], C("ident"),
                               [("xsb", tti), "cst"], [pr])
                        cp("act" if hb else "dve", h1T[:, hb * 4:(hb + 1) * 4, tti * 128:(tti + 1) * 128],
                           pb.rearrange("p (a b) -> p a b", a=4), [pr], [Rh1T])
                for n in range(2):
                    wt_, wr_ = wload(B_XQ + n)
                    for oc in range(4):
                        pb, pr = bank()
                        for c in range(8):
                            mm(pb, wt_[:, c, oc * 128:(oc + 1) * 128], h1T[:, c, :], c == 0, c == 7, [wr_, Rh1T], [pr])
                        cp("act", qxT[:, n * 4 + oc, :], pb, [pr], [RqxT])
                PXs = [PxT, PxT2]
                RDs = [rden, rden2]

                def xa_A(hd):
                    P_ = PXs[hd % 2]
                    for mt in range(2):
                        pb, pr = bank()
                        for dc in range(2):
                            mm(pb, KmT[:, 2 * hd + dc, mt * 128:(mt + 1) * 128], qxT[:, 2 * hd + dc, :], dc == 0, dc == 1,
                               ["KmT", RqxT], [pr])
                        act(P_[:, mt, :], pb, AF.Exp, [pr], [("PxT", hd % 2, mt)], scale=1.0 / 16.0)

                def xa_B(hd):
                    P_ = PXs[hd % 2]
                    rd = RDs[hd % 2]
                    rr = "t32c" if hd % 2 else "rden"
                    pd, prd = bank()
                    for mt in range(2):
                        mm(pd, onesb, P_[:, mt, :], mt == 0, mt == 1, ["onesb", ("PxT", hd % 2, mt)], [prd])
                    act(rd, pd, AF.Ln, [prd], [rr])
                    act(rd, rd, AF.Exp, [rr], [rr], scale=-1.0)
                    for dc in range(2):
                        po, pro = bank()
                        for mt in range(2):
                            mm(po, Vm[:, mt, (2 * hd + dc) * 128:(2 * hd + dc + 1) * 128], P_[:, mt, :], mt == 0, mt == 1,
                               ["Vm", ("PxT", hd % 2, mt)], [pro])
                        tt("dve", oxT[:, 2 * hd + dc, :], po, rd, ALU.mult, [pro, rr], [RoxT])

                xa_A(0)
                for hd in range(4):
                    if hd + 1 < 4:
                        xa_A(hd + 1)
                    xa_B(hd)
                wxo = [wload(B_XO + n) for n in range(2)]
                for tti in range(4):
                    for n in range(2):
                        wt_, wr_ = wxo[n]
                        pb, pr = bank()
                        for c in range(8):
                            mm(pb, oxT[:, c, tti * 128:(tti + 1) * 128], wt_[:, c, :], c == 0, c == 7, [wr_, RoxT], [pr])
                        stt(xsb[:, tti, n * 512:(n + 1) * 512], xsb[:, tti, n * 512:(n + 1) * 512], ALPHA, pb, ALU.mult, ALU.add,
                            [("xsb", tti), pr], [("xsb", tti)])
                    layer_norm(tti, 1)

            if stage < 3:
                for tti in range(4):
                    tk = dma("pool", dout[b, t0 + tti * 128:t0 + (tti + 1) * 128, :], xsb[:, tti, :], [("xsb", tti)], ["dout"])
                    final_tokens.append(tk)
            else:
                T4 = slice(tile_no, tile_no + 4)
                for tti in range(4):
                    tno = tile_no + tti
                    res = ("xsb", tti)
                    h2T = h2Ts[tti % 2]; hres = H2R[tti % 2]
                    dma("pool", H32[tno * 128:(tno + 1) * 128, :], xsb[:, tti, :], [res], ["H32"])
                    for hb in range(2):
                        pb, pr = bank()
                        for k4 in range(4):
                            kc = hb * 4 + k4
                            tr(pb[:, k4 * 128:(k4 + 1) * 128], xsb[:, tti, kc * 128:(kc + 1) * 128], C("ident"), [res, "cst"], [pr])
                        cp("act", h2T[:, hb * 4:(hb + 1) * 4, :], pb.rearrange("p (a b) -> p a b", a=4), [pr], hres)
                    pb, pr = bank()
                    for kc in range(8):
                        mm(pb[:, 0:72], h2T[:, kc, :], wrs[:, kc, :], kc == 0, kc == 7, hres + ["wrs"], [pr])
                    tt("dve", lg4[:, tti, :], pb[:, 0:72], smb[:, 24:96], ALU.add, [pr, "smb"], ["lg4"])
                G8 = lg4[:, :, 0:8]
                red(rq[:, 0, :], G8, ALU.max, ["lg4"], ["rq0"])
                tt("dve", d4, G8, rq[:, 0, :].unsqueeze(2).to_broadcast([128, 4, 8]), ALU.subtract, ["lg4", "rq0"], ["d4"])
                ts("dve", pen4, d4, 0.0, None, ALU.is_equal, None, ["d4"], ["pen4"])
                ts("dve", pen4, pen4, 1e30, -1e30, ALU.mult, ALU.add, ["pen4"], ["pen4"])
                act(d4, d4, AF.Exp, ["d4"], ["d4"])
                red(rq[:, 1, :], d4, ALU.add, ["d4"], ["rq1"])
                recip(rq[:, 2, :], rq[:, 1, :], ["rq1"], ["rq2"])
                tt("dve", lem4.rearrange("p t (g e) -> p t g e", g=8), lg4[:, :, 8:72].rearrange("p t (g e) -> p t g e", g=8),
                   pen4.unsqueeze(3).to_broadcast([128, 4, 8, 8]), ALU.add, ["lg4", "pen4"], ["lem4"])
                for tti in range(4):
                    sch.op("dve", (lambda e, tti=tti: e.max(top84[:, tti, :], lem4[:, tti, :])), ["lem4"], ["top84"])
                for s_ in range(2):
                    tt("dve", oh4[:, :, s_, :], lem4, top84[:, :, s_:s_ + 1].to_broadcast([128, 4, 64]), ALU.is_equal,
                       ["lem4", "top84"], [("oh4", s_)])
                    tt("dve", j4, oh4[:, :, s_, :], C("iota64").unsqueeze(1).to_broadcast([128, 4, 64]), ALU.mult,
                       [("oh4", s_), "cst"], ["j4"])
                    red(RW[:, T4, 2 + s_], j4, ALU.add, ["j4"], ["RW"])
                tt("dve", rq[:, 3, :], top84[:, :, 1], top84[:, :, 0], ALU.subtract, ["top84"], ["rq3"])
                act(rq[:, 3, :], rq[:, 3, :], AF.Exp, ["rq3"], ["rq3"])
                ts("dve", rq[:, 3, :], rq[:, 3, :], 1.0, None, ALU.add, None, ["rq3"], ["rq3"])
                recip(rq[:, 4, :], rq[:, 3, :], ["rq3"], ["rq4"])
                tt("dve", RW[:, T4, 0], rq[:, 4, :], rq[:, 2, :], ALU.mult, ["rq4", "rq2"], ["RW"])
                tt("dve", RW[:, T4, 1], rq[:, 2, :], RW[:, T4, 0], ALU.subtract, ["rq2", "RW"], ["RW"])
                tt("dve", ohb4, oh4[:, :, 0, :], oh4[:, :, 1, :], ALU.add, [("oh4", 0), ("oh4", 1)], ["ohb4"])
                pR, prR = bank()
                for tti in range(4):
                    o_ = pR[:, tti * 64:(tti + 1) * 64]
                    mm(o_, onesb, ohcum, True, False, ["onesb", "ohcum"], [prR])
                    for t2 in range(tti):
                        mm(o_, onesb, ohb4[:, t2, :], False, False, ["onesb", "ohb4"], [prR])
                    mm(o_, ustrb, ohb4[:, tti, :], False, True, ["ustrb", "ohb4"], [prR])
                for s_ in range(2):
                    tt("dve", j4, pR[:, 0:256].rearrange("p (a b) -> p a b", a=4), oh4[:, :, s_, :], ALU.mult, [prR, ("oh4", s_)], ["j4"])
                    red(RW[:, T4, 4 + s_], j4, ALU.add, ["j4"], ["RW"])
                red(ohs, ohb4.rearrange("p t e -> p e t"), ALU.add, ["ohb4"], ["ohs"])
                tt("dve", ohcum, ohcum, ohs, ALU.add, ["ohcum", "ohs"], ["ohcum"])
            tile_no += 4

    if stage >= 3:
        sch.fence()
        ptr[0] = p1_base
        cnt = carve([128, 64]); cnti = carve([128, 64], I32); pend = carve([128, 64]); pstart = carve([128, 64])
        bef = carve([128, 256]); bei = carve([128, 256], I32)
        NWG, NWD, DEP = 4, 6, 3
        wg16 = [carve([128, 8, 512], BF16) for i in range(NWG)]
        wd16 = [carve([128, 2, D], BF16) for i in range(NWD)]
        xblk = [carve([128, D], BF16) for i in range(DEP)]
        xTb = [carve([128, 8, 128], BF16) for i in range(DEP)]
        sgl = [carve([128, 256]) for i in range(DEP)]
        actb = [carve([128, 256], BF16) for i in range(DEP)]
        actT = [carve([128, 2, 128], BF16) for i in range(DEP)]
        ysb = [carve([128, D], BF16) for i in range(DEP)]
        y0s = [carve([128, D], BF16) for i in range(3)]; y1s = [carve([128, D], BF16) for i in range(3)]
        yfs = [carve([128, D]) for i in range(2)]
        x16s = [carve([128, D], BF16) for i in range(4)]
        hls = [carve([128, D]) for i in range(4)]
        pb, pr = bank()
        mm(pb[:, 0:64], onesb, ohcum, True, True, ["onesb", "ohcum"], [pr])
        cp("dve", cnt, pb[:, 0:64], [pr], ["cnt"])
        ts("dve", cnti, cnt, 127.0, None, ALU.add, None, ["cnt"], ["cnti"])
        ts("dve", cnti, cnti, 7, None, ALU.arith_shift_right, None, ["cnti"], ["cnti"])
        ts("dve", cnti, cnti, 7, None, ALU.logical_shift_left, None, ["cnti"], ["cnti"])
        cp("dve", cnt, cnti, ["cnti"], ["cnt"])
        sch.op("dve", lambda e: e.tensor_tensor_scan(pend, C("ones")[:, 0:64], cnt, 0.0, ALU.mult, ALU.add),
               ["cst", "cnt"], ["pend"])
        tt("dve", pstart, pend, cnt, ALU.subtract, ["pend", "cnt"], ["pstart"])
        ohall = carve([128, NTT, 64])
        posf = carve([128, NTT])
        for s_ in range(2):
            tt("dve", ohall, C("iota64").unsqueeze(1).to_broadcast([128, NTT, 64]),
               RW[:, :, 2 + s_:3 + s_].to_broadcast([128, NTT, 64]), ALU.is_equal, ["cst", "RW"], ["ohall"])
            tt("dve", ohall, ohall, pstart.unsqueeze(1).to_broadcast([128, NTT, 64]), ALU.mult, ["ohall", "pstart"], ["ohall"])
            red(posf, ohall, ALU.add, ["ohall"], ["posf"])
            tt("dve", posf, posf, RW[:, :, 4 + s_], ALU.add, ["posf", "RW"], ["posf"])
            cp("dve", posI[:, :, s_], posf, ["posf"], ["posI"])
        memset("dve", bef, 0.0, ["bef"])
        for e_ in range(NEXP):
            stt(bef, C("blk128"), pend[:, e_:e_ + 1], bef, ALU.is_ge, ALU.add, ["cst", "pend", "bef"], ["bef"])
        ts("dve", bef, bef, 128.0, None, ALU.mult, None, ["bef"], ["bef"])
        ts("dve", bef, bef, C("pidx")[:, 0:1], None, ALU.add, None, ["bef", "cst"], ["bef"])
        cp("dve", bei, bef, ["bef"], ["bei"])
        for tno in range(NTT):
            hl = hls[tno % 4]; x16 = x16s[tno % 4]
            dma("sp", hl, H32[tno * 128:(tno + 1) * 128, :], ["H32"], [("hl", tno % 4)])
            cp("act", x16, hl, [("hl", tno % 4)], [("x16", tno % 4)])
            for s_ in range(2):
                sch.dma("pool", (lambda e, tno=tno, s_=s_, x16=x16: e.indirect_dma_start(
                    out=XS, out_offset=bass.IndirectOffsetOnAxis(ap=posI[:, tno, s_:s_ + 1], axis=0),
                    in_=x16, in_offset=None)), [("x16", tno % 4), "posI"], ["XS"])
        sch.fence()
        bc_ = {}

        def bcreg(e):
            if "r" not in bc_:
                bc_["r"] = e.alloc_register("bcreg")
                e.reg_mov(bc_["r"], NEXP * 128 - 1)
            return bc_["r"]

        def moe_A(blk):
            g_, d_, p_ = blk % NWG, blk % NWD, blk % DEP
            sch.dma("pool", (lambda e: e.indirect_dma_start(
                out=wg16[g_].rearrange("p a b -> p (a b)"), out_offset=None, in_=WGB,
                in_offset=bass.IndirectOffsetOnAxis(ap=bei[:, blk:blk + 1], axis=0),
                bounds_check=bcreg(e), oob_is_err=False)), ["bei", "WGB"], [("wg16", g_)])
            sch.dma("pool", (lambda e: e.indirect_dma_start(
                out=wd16[d_].rearrange("p a b -> p (a b)"), out_offset=None, in_=WDB,
                in_offset=bass.IndirectOffsetOnAxis(ap=bei[:, blk:blk + 1], axis=0),
                bounds_check=bcreg(e), oob_is_err=False)), ["bei", "WDB"], [("wd16", d_)])
            dma("act", xblk[p_], XS[blk * 128:(blk + 1) * 128, :], ["XS"], [("xblk", p_)])
            pb, pr = bank()
            pbb = pb.bitcast(BF16)
            for kc in range(8):
                tr(pbb[:, kc * 128:(kc + 1) * 128], xblk[p_][:, kc * 128:(kc + 1) * 128], identb, [("xblk", p_), "identb"], [pr])
            cp("dve", xTb[p_].rearrange("p a b -> p (a b)"), pbb, [pr], [("xTb", p_)])

        def moe_B(blk):
            g_, p_ = blk % NWG, blk % DEP
            pH, prH = bank()
            for kc in range(8):
                mm(pH, xTb[p_][:, kc, :], wg16[g_][:, kc, :], kc == 0, kc == 7, [("xTb", p_), ("wg16", g_)], [prH])
            act(sgl[p_], pH[:, 0:256], AF.Silu, [prH], [("sgl", p_)])
            tt("dve", actb[p_], pH[:, 256:512], sgl[p_], ALU.mult, [prH, ("sgl", p_)], [("actb", p_)])

        def moe_C(blk):
            p_ = blk % DEP
            pb, pr = bank()
            pbb = pb.bitcast(BF16)
            for fc in range(2):
                tr(pbb[:, fc * 128:(fc + 1) * 128], actb[p_][:, fc * 128:(fc + 1) * 128], identb, [("actb", p_), "identb"], [pr])
            cp("dve", actT[p_].rearrange("p a b -> p (a b)"), pbb[:, 0:256], [pr], [("actT", p_)])

        def moe_D(blk):
            d_, p_ = blk % NWD, blk % DEP
            for n in range(2):
                pY, prY = bank()
                for fc in range(2):
                    mm(pY, actT[p_][:, fc, :], wd16[d_][:, fc, n * 512:(n + 1) * 512], fc == 0, fc == 1,
                       [("actT", p_), ("wd16", d_)], [prY])
                cp("act" if n == 0 else "dve", ysb[p_][:, n * 512:(n + 1) * 512], pY, [prY], [("ysb", p_)])
            dma("sp", YS[blk * 128:(blk + 1) * 128, :], ysb[p_], [("ysb", p_)], ["YS"])

        for step in range(NBLKM + 3):
            if step < NBLKM:
                moe_A(step)
            if 0 <= step - 1 < NBLKM:
                moe_B(step - 1)
            if 0 <= step - 2 < NBLKM:
                moe_C(step - 2)
            if 0 <= step - 3 < NBLKM:
                moe_D(step - 3)
        sch.fence()
        for i in range(2):
            dma("sp", lnp[:, i, :], dvec[4 + i, :].partition_broadcast(128), (), ["lnp"])
        def p3_fetch(tno):
            tti = tno % 4
            k3 = tno % 3
            dma("act", xsb[:, tti, :], H32[tno * 128:(tno + 1) * 128, :], ["H32"], [("xsb", tti)])
            for s_, yy, yr in ((0, y0s[k3], ("y0", k3)), (1, y1s[k3], ("y1", k3))):
                sch.dma("pool", (lambda e, s_=s_, yy=yy: e.indirect_dma_start(
                    out=yy, out_offset=None, in_=YS,
                    in_offset=bass.IndirectOffsetOnAxis(ap=posI[:, tno, s_:s_ + 1], axis=0))), ["YS", "posI"], [yr])

        p3_fetch(0)
        if NTT > 1:
            p3_fetch(1)
        for tno in range(NTT):
            tti = tno % 4
            b_ = (tno * 128) // S
            tok0 = tno * 128 - b_ * S
            res = ("xsb", tti)
            if tno + 2 < NTT:
                p3_fetch(tno + 2)
            k3 = tno % 3
            y0 = y0s[k3]; y1 = y1s[k3]
            yf = yfs[tno % 2]; yfr = ("yf", tno % 2)
            act(yf, y0, AF.Copy, [("y0", k3), "RW"], [yfr], scale=RW[:, tno, 0:1])
            stt(yf, y1, RW[:, tno, 1:2], yf, ALU.mult, ALU.add, [("y1", k3), "RW", yfr], [yfr])
            stt(xsb[:, tti, :], xsb[:, tti, :], ALPHA, yf, ALU.mult, ALU.add, [res, yfr], [res])
            layer_norm(tti, 0, add_eng="dve")
            tk = dma("sp", dout[b_, tok0:tok0 + 128, :], xsb[:, tti, :], [res], ["dout"])
            final_tokens.append(tk)

    sch.emit(final_tokens)
    es.close()
    return nc


def make_in_maps(inp, S, NB, ncores):
    f = lambda a: np.ascontiguousarray(np.asarray(a, dtype=np.float32))
    wall = build_wall(f(inp["w_in"][0]), f(inp["w_att_branch"][0]), f(inp["w_ml_branch"][0]), f(inp["w_mix_out"][0]),
                      f(inp["w_xq"][0]), f(inp["w_xo"][0]), f(inp["w_xkv"][0]))
    vecs = np.stack([f(inp[k][0]) for k in ("ln1_g", "ln1_b", "ln2_g", "ln2_b", "ln3_g", "ln3_b")])
    pcol = np.zeros((128, 56), np.float32)
    pcol[:, 0:8] = f(inp["ml_norm_g"][0]).reshape(8, 128).T
    pcol[:, 8:16] = f(inp["conv_b"][0]).reshape(8, 128).T
    cwv = f(inp["conv_w"][0])
    for ch in range(8):
        for j in range(4):
            pcol[:, 16 + ch * 4 + j] = cwv[j, ch * 128:(ch + 1) * 128]
    pcol[:, 48] = (10000.0 ** (-(np.arange(128) % 32).astype(np.float32) / np.float32(32))).astype(np.float32)
    pcol[0:4, 49] = f(inp["b_igate"][0])
    pcol[0:4, 50] = f(inp["b_fgate"][0])
    smalls = np.zeros((128,), np.float32)
    smalls[0:16] = f(inp["attn_sinks"][0])
    smalls[24:32] = f(inp["b_router_group"][0])
    smalls[32:96] = f(inp["b_router_expert"][0])
    wr = np.concatenate([f(inp["w_router_group"][0]), f(inp["w_router_expert"][0])], axis=1)
    wr = np.ascontiguousarray(wr.reshape(8, 128, 72).transpose(1, 0, 2))
    wgu = np.concatenate([f(inp["w_gate"][0]), f(inp["w_up"][0])], axis=2)
    wgu = np.ascontiguousarray(wgu.reshape(NEXP, 8, 128, 512).transpose(0, 2, 1, 3)).reshape(NEXP * 128, 4096)
    wd = f(inp["w_down"][0])
    wd = np.ascontiguousarray(wd.reshape(NEXP, 2, 128, D).transpose(0, 2, 1, 3)).reshape(NEXP * 128, 2048)
    x = f(inp["x"]); mem = f(inp["mem"]); pos = np.ascontiguousarray(np.asarray(inp["positions"], dtype=np.int32))
    maps = []
    for c in range(ncores):
        maps.append({
            "x": x[c * NB:(c + 1) * NB], "mem": mem[c * NB:(c + 1) * NB], "pos": pos[c * NB:(c + 1) * NB],
            "wall": wall, "consts": _CONST_ARR, "vecs": vecs, "pcol": pcol, "smalls": smalls,
            "wr": wr, "wgu": wgu, "wd": wd,
        })
    return maps


_NC_CACHE = {}


def kernel(**inp):
    B, S = inp["x"].shape[0], inp["x"].shape[1]
    NB = B // NCORES
    key = (S, NB)
    if key not in _NC_CACHE:
        _NC_CACHE[key] = build(S, NB)
    nc = _NC_CACHE[key]
    maps = make_in_maps(inp, S, NB, NCORES)
    res = run_bass_kernel_spmd(nc, maps, core_ids=list(range(NCORES)))
    return np.concatenate([r["out"] for r in res.results], axis=0).astype(np.float32)
```
